# Optimizing a Trainium2 kernel written in Bass

```python
import math
import jax
import jax.numpy as jnp
from jax import lax
import numpy as np

D_MODEL = 1024
BATCH = 4
SEQ = 8192
DEPTH = 4

GRID_W = 64
CTX_LEN = 256
N_EVEN = (DEPTH + 1) // 2
N_ODD = DEPTH // 2
N_MOD = 6
EPS = 1e-6

LRU_WIDTH = D_MODEL // 2
LRU_BLOCKS = 8
LRU_BLOCK = LRU_WIDTH // LRU_BLOCKS
CONV_W = 4
CONV_LEFT = 2
LRU_C = 8.0
ATT_HEADS = 8
ATT_KV_HEADS = 2
ATT_GROUP = ATT_HEADS // ATT_KV_HEADS
HEAD_DIM = 64
ATT_WIDTH = ATT_HEADS * HEAD_DIM
KV_WIDTH = ATT_KV_HEADS * HEAD_DIM
WINDOW = 128
BLOCK_Q = 128
ROPE_FREQS = HEAD_DIM // 4
ROPE_BASE = 10000.0
S5_WIDTH = D_MODEL // 2
S5_GROUP = 16
S5_GROUPS = S5_WIDTH // S5_GROUP
S5_STATE = 64
HG_HEADS = 4
HG_DK = 128
HG_DV = 128
HG_WIDTH = HG_HEADS * HG_DK
HG_CHUNK = 64
FFN_DIM = 2816
N_EXPERTS = 8
TOP_K = 2
EXPERT_DIM = 1408

IN_AB = 2 * LRU_WIDTH + ATT_WIDTH + 2 * KV_WIDTH
MIX_AB = LRU_WIDTH + ATT_WIDTH
IN_CD = S5_WIDTH + 5 * HG_WIDTH
MIX_CD = S5_WIDTH + HG_WIDTH

kernel_name = 'hybrid_rglru_swa_s5_hgrn2_moe_dit'


def rms_norm(x, g):
    xf = x.astype(jnp.float32)
    y = xf * lax.rsqrt(jnp.mean(xf * xf, axis=-1, keepdims=True) + EPS)
    return (y * g.astype(jnp.float32)).astype(x.dtype)


def axial_rope_tables(rows):
    row = jnp.repeat(jnp.arange(rows, dtype=jnp.float32), GRID_W)
    col = jnp.tile(jnp.arange(GRID_W, dtype=jnp.float32), rows)
    inv = ROPE_BASE ** (-jnp.arange(ROPE_FREQS, dtype=jnp.float32) / ROPE_FREQS)
    ang = jnp.stack([row[:, None] * inv, col[:, None] * inv], axis=1)
    return jnp.cos(ang), jnp.sin(ang)


def apply_axial_rope(x, cos, sin):
    shp = x.shape
    xr = x.reshape(shp[:-1] + (2, 2, ROPE_FREQS))
    bshape = (shp[1],) + (1,) * (x.ndim - 3) + (2, ROPE_FREQS)
    c = cos.reshape(bshape).astype(x.dtype)
    s = sin.reshape(bshape).astype(x.dtype)
    x1, x2 = xr[..., 0, :], xr[..., 1, :]
    return jnp.stack([x1 * c - x2 * s, x2 * c + x1 * s], axis=-2).reshape(shp)


def centred_depthwise_conv(x, w, b):
    T = x.shape[1]
    xp = jnp.pad(x, ((0, 0), (CONV_LEFT, CONV_W - 1 - CONV_LEFT), (0, 0)))
    y = b + xp[:, 0:T] * w[0]
    for j in range(1, CONV_W):
        y = y + xp[:, j:j + T] * w[j]
    return y


def block_diag(x, w):
    B, T, _ = x.shape
    xb = x.reshape(B, T, w.shape[0], w.shape[1])
    return jnp.einsum('btnh,nhk->btnk', xb, w).reshape(B, T, -1)


def linear_scan(a, b, h0, reverse):
    def combine(e1, e2):
        a1, b1 = e1
        a2, b2 = e2
        return a1 * a2, a2 * b1 + b2
    a_cum, h = lax.associative_scan(combine, (a, b), axis=1, reverse=reverse)
    if h0 is not None:
        h = h + a_cum * h0[:, None]
    return h


def rglru_coeffs(u, wa, ba, wx, bx, lam):
    r = jax.nn.sigmoid(block_diag(u, wa) + ba)
    i = jax.nn.sigmoid(block_diag(u, wx) + bx)
    log_a = -LRU_C * r * jax.nn.softplus(-lam)
    a = jnp.exp(log_a)
    mult = jnp.sqrt(-jnp.expm1(2.0 * log_a))
    return a, mult * (i * u)


def rglru_bidir(u_x, u_c, need_ctx, wa, ba, wx, bx, lam):
    hx_dirs, hc_dirs = [], []
    for d, rev in enumerate((False, True)):
        a_c, b_c = rglru_coeffs(u_c, wa[d], ba[d], wx[d], bx[d], lam[d])
        h_c = linear_scan(a_c, b_c, None, rev)
        h0 = h_c[:, 0] if rev else h_c[:, -1]
        a_x, b_x = rglru_coeffs(u_x, wa[d], ba[d], wx[d], bx[d], lam[d])
        hx_dirs.append(linear_scan(a_x, b_x, h0, rev))
        hc_dirs.append(h_c)
    h_x = hx_dirs[0] + hx_dirs[1]
    h_c = (hc_dirs[0] + hc_dirs[1]) if need_ctx else None
    return h_x, h_c


def band_mask(nb, T):
    n = jnp.arange(nb)[:, None, None]
    qpos = n * BLOCK_Q + jnp.arange(BLOCK_Q)[None, :, None]
    kpos = (n - 1) * BLOCK_Q + jnp.arange(3 * BLOCK_Q)[None, None, :]
    return (jnp.abs(qpos - kpos) <= WINDOW) & (kpos >= 0) & (kpos < T)


def banded(t):
    pad = [(0, 0), (1, 1)] + [(0, 0)] * (t.ndim - 2)
    tp = jnp.pad(t, pad)
    return jnp.concatenate([tp[:, :-2], tp[:, 1:-1], tp[:, 2:]], axis=2)


def window_gqa_with_sink(q_x, k_x, v_x, q_c, k_c, v_c, sink, need_ctx):
    B, T = q_x.shape[:2]
    nb = T // BLOCK_Q
    n_loc = 3 * BLOCK_Q
    n_ctx = k_c.shape[1]
    scale = HEAD_DIM ** -0.5
    qb = q_x.reshape(B, nb, BLOCK_Q, ATT_KV_HEADS, ATT_GROUP, HEAD_DIM)
    kb = banded(k_x.reshape(B, nb, BLOCK_Q, ATT_KV_HEADS, HEAD_DIM))
    vb = banded(v_x.reshape(B, nb, BLOCK_Q, ATT_KV_HEADS, HEAD_DIM))
    s_loc = jnp.einsum('bnqhgd,bnkhd->bnhgqk', qb, kb).astype(jnp.float32) * scale
    s_loc = jnp.where(band_mask(nb, T)[None, :, None, None], s_loc, -jnp.inf)
    s_ctx = jnp.einsum('bnqhgd,bchd->bnhgqc', qb, k_c).astype(jnp.float32) * scale
    sink_hg = sink.astype(jnp.float32).reshape(ATT_KV_HEADS, ATT_GROUP, 1, 1)
    s_sink = jnp.broadcast_to(sink_hg, s_loc.shape[:-1] + (1,))
    p = jax.nn.softmax(jnp.concatenate([s_loc, s_ctx, s_sink], axis=-1), axis=-1).astype(v_x.dtype)
    o = (jnp.einsum('bnhgqk,bnkhd->bnqhgd', p[..., :n_loc], vb)
         + jnp.einsum('bnhgqc,bchd->bnqhgd', p[..., n_loc:n_loc + n_ctx], v_c))
    o_x = o.reshape(B, T, ATT_WIDTH)
    if not need_ctx:
        return o_x, None
    s_cc = jnp.einsum('bqhgd,bchd->bhgqc', q_c, k_c).astype(jnp.float32) * scale
    s_sink_c = jnp.broadcast_to(sink_hg, s_cc.shape[:-1] + (1,))
    p_c = jax.nn.softmax(jnp.concatenate([s_cc, s_sink_c], axis=-1), axis=-1).astype(v_c.dtype)
    o_c = jnp.einsum('bhgqc,bchd->bqhgd', p_c[..., :n_ctx], v_c).reshape(B, n_ctx, ATT_WIDTH)
    return o_x, o_c


def mixer_ab(hx, hc, need_ctx, rope, w_in, conv_w, conv_b, wa, ba, wx, bx, lam, sink, w_out):
    B, T, _ = hx.shape
    Tc = hc.shape[1]
    splits = [LRU_WIDTH, 2 * LRU_WIDTH, 2 * LRU_WIDTH + ATT_WIDTH, 2 * LRU_WIDTH + ATT_WIDTH + KV_WIDTH]
    gx, ux, qx, kx, vx = jnp.split(hx @ w_in, splits, axis=-1)
    gc, uc, qc, kc, vc = jnp.split(hc @ w_in, splits, axis=-1)
    ux = centred_depthwise_conv(ux, conv_w, conv_b)
    uc = centred_depthwise_conv(uc, conv_w, conv_b)
    lru_x, lru_c = rglru_bidir(ux, uc, need_ctx, wa, ba, wx, bx, lam)
    cos, sin = rope
    qx = apply_axial_rope(qx.reshape(B, T, ATT_KV_HEADS, ATT_GROUP, HEAD_DIM), cos, sin)
    kx = apply_axial_rope(kx.reshape(B, T, ATT_KV_HEADS, HEAD_DIM), cos, sin)
    vx = vx.reshape(B, T, ATT_KV_HEADS, HEAD_DIM)
    qc = qc.reshape(B, Tc, ATT_KV_HEADS, ATT_GROUP, HEAD_DIM)
    kc = kc.reshape(B, Tc, ATT_KV_HEADS, HEAD_DIM)
    vc = vc.reshape(B, Tc, ATT_KV_HEADS, HEAD_DIM)
    att_x, att_c = window_gqa_with_sink(qx, kx, vx, qc, kc, vc, sink, need_ctx)
    dx = jnp.concatenate([lru_x * jax.nn.gelu(gx), att_x], axis=-1) @ w_out
    dc = (jnp.concatenate([lru_c * jax.nn.gelu(gc), att_c], axis=-1) @ w_out) if need_ctx else None
    return dx, dc


def s5_discretise(a_re, a_im, log_step, b_re, b_im):
    lr = jnp.minimum(a_re, -1e-4)
    li = a_im
    dt = jnp.exp(log_step)[:, None]
    mag = jnp.exp(lr * dt)
    ang = li * dt
    lbr, lbi = mag * jnp.cos(ang), mag * jnp.sin(ang)
    zr, zi = lbr - 1.0, lbi
    den = lr * lr + li * li
    fr = (zr * lr + zi * li) / den
    fi = (zi * lr - zr * li) / den
    bbr = fr[..., None] * b_re - fi[..., None] * b_im
    bbi = fr[..., None] * b_im + fi[..., None] * b_re
    return lbr, lbi, bbr, bbi


def _complex_combine(e1, e2):
    a1r, a1i, b1r, b1i = e1
    a2r, a2i, b2r, b2i = e2
    return (a2r * a1r - a2i * a1i, a2r * a1i + a2i * a1r,
            a2r * b1r - a2i * b1i + b2r, a2r * b1i + a2i * b1r + b2i)


def s5_scan(u, disc, h0, reverse):
    lbr, lbi, bbr, bbi = disc
    B, T, _ = u.shape
    ug = u.reshape(B, T, S5_GROUPS, S5_GROUP)
    br = jnp.einsum('btgc,gnc->btgn', ug, bbr)
    bi = jnp.einsum('btgc,gnc->btgn', ug, bbi)
    ar = jnp.broadcast_to(lbr, br.shape)
    ai = jnp.broadcast_to(lbi, bi.shape)
    a_r, a_i, hr, hi = lax.associative_scan(_complex_combine, (ar, ai, br, bi), axis=1, reverse=reverse)
    if h0 is not None:
        h0r, h0i = h0[0][:, None], h0[1][:, None]
        hr, hi = hr + a_r * h0r - a_i * h0i, hi + a_r * h0i + a_i * h0r
    return hr, hi


def s5_readout(hr, hi, c_re, c_im):
    y = jnp.einsum('btgn,gcn->btgc', hr, c_re) - jnp.einsum('btgn,gcn->btgc', hi, c_im)
    return y.reshape(hr.shape[0], hr.shape[1], S5_WIDTH)


def s5_bidir(u_x, u_c, need_ctx, a_re, a_im, log_step, b_re, b_im, c_re, c_im, d, w_glu, b_glu):
    y_x = d * u_x
    y_c = d * u_c if need_ctx else None
    for dr, rev in enumerate((False, True)):
        disc = s5_discretise(a_re[dr], a_im[dr], log_step[dr], b_re[dr], b_im[dr])
        hr_c, hi_c = s5_scan(u_c, disc, None, rev)
        edge = 0 if rev else -1
        hr_x, hi_x = s5_scan(u_x, disc, (hr_c[:, edge], hi_c[:, edge]), rev)
        y_x = y_x + s5_readout(hr_x, hi_x, c_re[dr], c_im[dr])
        if need_ctx:
            y_c = y_c + s5_readout(hr_c, hi_c, c_re[dr], c_im[dr])

    def glu(y):
        y = jax.nn.gelu(y)
        return y * jax.nn.sigmoid(y @ w_glu + b_glu)

    return glu(y_x), (glu(y_c) if need_ctx else None)


def gla_chunk_scan(q, k, v, log_f, s0):
    B, T, H, K = q.shape
    V = v.shape[-1]
    nc = T // HG_CHUNK

    def to_chunks(t):
        return t.reshape(B, nc, HG_CHUNK, H, t.shape[-1]).transpose(1, 0, 3, 2, 4)

    lower = jnp.tril(jnp.ones((HG_CHUNK, HG_CHUNK), dtype=bool))

    def step(s, inp):
        qc, kc, vc, gc = inp
        bcum = jnp.cumsum(gc, axis=2)
        o_inter = jnp.einsum('bhtk,bhkv->bhtv', qc * jnp.exp(bcum), s)
        diff = jnp.where(lower[:, :, None], bcum[:, :, :, None, :] - bcum[:, :, None, :, :], -jnp.inf)
        attn = jnp.einsum('bhtk,bhsk,bhtsk->bhts', qc, kc, jnp.exp(diff))
        o = o_inter + jnp.einsum('bhts,bhsv->bhtv', attn, vc)
        b_last = bcum[:, :, -1:, :]
        s = (jnp.exp(b_last[:, :, 0, :, None]) * s
             + jnp.einsum('bhsk,bhsv->bhkv', kc * jnp.exp(b_last - bcum), vc))
        return s, o

    s_fin, o = lax.scan(step, s0, (to_chunks(q), to_chunks(k), to_chunks(v), to_chunks(log_f)))
    return o.transpose(1, 0, 3, 2, 4).reshape(B, T, H, V), s_fin


def _heads(t):
    return t.reshape(t.shape[:2] + (HG_HEADS, -1)).astype(jnp.float32)


def hgrn_dir(q, v, zf, lbh, s0, rev):
    f = lbh + (1.0 - lbh) * jax.nn.sigmoid(zf)
    log_f = jnp.log(f)
    k = 1.0 - f
    if rev:
        q, k, v, log_f = (jnp.flip(t, axis=1) for t in (q, k, v, log_f))
    o, s = gla_chunk_scan(q, k, v, log_f, s0)
    if rev:
        o = jnp.flip(o, axis=1)
    return o, s


def hgrn2_bidir(zx, zc, need_ctx, lb, norm_g):
    qx, ffx, fbx, ix, gx = zx
    qc, ffc, fbc, ic, gc = zc
    q_x, v_x = _heads(jax.nn.silu(qx)), _heads(ix)
    q_c, v_c = _heads(jax.nn.silu(qc)), _heads(ic)
    lbh = lb.reshape(HG_HEADS, HG_DK)
    s0 = jnp.zeros((qx.shape[0], HG_HEADS, HG_DK, HG_DV), jnp.float32)
    ox, oc = [], []
    for zf_x, zf_c, rev in ((ffx, ffc, False), (fbx, fbc, True)):
        o_c, s_c = hgrn_dir(q_c, v_c, _heads(zf_c), lbh, s0, rev)
        o_x, _ = hgrn_dir(q_x, v_x, _heads(zf_x), lbh, s_c, rev)
        ox.append(o_x)
        oc.append(o_c)

    def readout(o, g):
        return (rms_norm(o, norm_g) * jax.nn.silu(_heads(g))).reshape(g.shape).astype(g.dtype)

    out_x = readout(ox[0] + ox[1], gx)
    out_c = readout(oc[0] + oc[1], gc) if need_ctx else None
    return out_x, out_c


def mixer_cd(hx, hc, need_ctx, w_in, a_re, a_im, log_step, b_re, b_im, c_re, c_im, d, w_glu, b_glu,
             lb, hg_g, w_out):
    splits = [S5_WIDTH + j * HG_WIDTH for j in range(5)]
    ux, qx, ffx, fbx, ix, gx = jnp.split(hx @ w_in, splits, axis=-1)
    uc, qc, ffc, fbc, ic, gc = jnp.split(hc @ w_in, splits, axis=-1)
    s5_x, s5_c = s5_bidir(ux, uc, need_ctx, a_re, a_im, log_step, b_re, b_im, c_re, c_im, d, w_glu, b_glu)
    hg_x, hg_c = hgrn2_bidir((qx, ffx, fbx, ix, gx), (qc, ffc, fbc, ic, gc), need_ctx, lb, hg_g)
    dx = jnp.concatenate([s5_x, hg_x], axis=-1) @ w_out
    dc = (jnp.concatenate([s5_c, hg_c], axis=-1) @ w_out) if need_ctx else None
    return dx, dc


def swiglu(h, w1, w3, w2):
    return (jax.nn.silu(h @ w1) * (h @ w3)) @ w2


def moe_swiglu(h, router, w1, w3, w2):
    logits = (h @ router).astype(jnp.float32)
    top_v, top_i = lax.top_k(logits, TOP_K)
    gates = jax.nn.softmax(top_v, axis=-1)
    weight = jnp.sum(jax.nn.one_hot(top_i, N_EXPERTS, dtype=jnp.float32) * gates[..., None], axis=-2)
    weight = weight.astype(h.dtype)
    out = jnp.zeros_like(h)
    for e in range(N_EXPERTS):
        out = out + weight[..., e:e + 1] * swiglu(h, w1[e], w3[e], w2[e])
    return out


def setup_inputs(seed: int = 0) -> dict:
    key = jax.random.key(seed)
    ks = iter(jax.random.split(key, 64))
    D = D_MODEL

    def nrm(shape, scale):
        return scale * jax.random.normal(next(ks), shape, jnp.float32)

    def gain(shape):
        return 1.0 + nrm(shape, 0.01)

    a8 = jax.random.uniform(next(ks), (N_EVEN, 2, LRU_WIDTH), jnp.float32, 0.9, 0.999)
    s = a8 ** (1.0 / LRU_C)
    lru_lam = jnp.log(s) - jnp.log1p(-s)
    s5_log_step = jax.random.uniform(next(ks), (N_ODD, 2, S5_GROUPS), jnp.float32,
                                     math.log(1e-3), math.log(1e-1))
    s5_a_re = -0.5 + nrm((N_ODD, 2, S5_GROUPS, S5_STATE), 0.01)
    s5_a_im = math.pi * jnp.arange(S5_STATE, dtype=jnp.float32) + nrm((N_ODD, 2, S5_GROUPS, S5_STATE), 0.01)
    return {
        'x': nrm((BATCH, SEQ, D), 1.0),
        'c': nrm((BATCH, D), 1.0),
        'ctx': nrm((BATCH, CTX_LEN, D), 1.0),
        'c_ctx': nrm((D,), 1.0),
        'w_mod': nrm((DEPTH, D, N_MOD * D), 0.5 * D ** -0.5),
        'b_mod': nrm((DEPTH, N_MOD * D), 0.01),
        'norm_mix': gain((DEPTH, D)),
        'norm_ffn': gain((DEPTH, D)),
        'final_norm': gain((D,)),
        'w_in_ab': nrm((N_EVEN, D, IN_AB), D ** -0.5),
        'lru_conv_w': nrm((N_EVEN, CONV_W, LRU_WIDTH), CONV_W ** -0.5),
        'lru_conv_b': nrm((N_EVEN, LRU_WIDTH), 0.01),
        'lru_wa': nrm((N_EVEN, 2, LRU_BLOCKS, LRU_BLOCK, LRU_BLOCK), LRU_BLOCK ** -0.5),
        'lru_ba': nrm((N_EVEN, 2, LRU_WIDTH), 0.01),
        'lru_wx': nrm((N_EVEN, 2, LRU_BLOCKS, LRU_BLOCK, LRU_BLOCK), LRU_BLOCK ** -0.5),
        'lru_bx': nrm((N_EVEN, 2, LRU_WIDTH), 0.01),
        'lru_lam': lru_lam,
        'attn_sink': nrm((N_EVEN, ATT_HEADS), 1.0),
        'w_out_ab': nrm((N_EVEN, MIX_AB, D), MIX_AB ** -0.5),
        'ffn_w1': nrm((N_EVEN, D, FFN_DIM), D ** -0.5),
        'ffn_w3': nrm((N_EVEN, D, FFN_DIM), D ** -0.5),
        'ffn_w2': nrm((N_EVEN, FFN_DIM, D), FFN_DIM ** -0.5),
        'w_in_cd': nrm((N_ODD, D, IN_CD), D ** -0.5),
        's5_a_re': s5_a_re,
        's5_a_im': s5_a_im,
        's5_log_step': s5_log_step,
        's5_b_re': nrm((N_ODD, 2, S5_GROUPS, S5_STATE, S5_GROUP), S5_GROUP ** -0.5),
        's5_b_im': nrm((N_ODD, 2, S5_GROUPS, S5_STATE, S5_GROUP), S5_GROUP ** -0.5),
        's5_c_re': nrm((N_ODD, 2, S5_GROUPS, S5_GROUP, S5_STATE), S5_STATE ** -0.5),
        's5_c_im': nrm((N_ODD, 2, S5_GROUPS, S5_GROUP, S5_STATE), S5_STATE ** -0.5),
        's5_d': nrm((N_ODD, S5_WIDTH), 1.0),
        's5_w_glu': nrm((N_ODD, S5_WIDTH, S5_WIDTH), S5_WIDTH ** -0.5),
        's5_b_glu': nrm((N_ODD, S5_WIDTH), 0.01),
        'hg_lb_raw': nrm((N_ODD, HG_WIDTH), 1.0),
        'hg_norm': gain((N_ODD, HG_DV)),
        'w_out_cd': nrm((N_ODD, MIX_CD, D), MIX_CD ** -0.5),
        'moe_router': nrm((N_ODD, D, N_EXPERTS), D ** -0.5),
        'moe_w1': nrm((N_ODD, N_EXPERTS, D, EXPERT_DIM), D ** -0.5),
        'moe_w3': nrm((N_ODD, N_EXPERTS, D, EXPERT_DIM), D ** -0.5),
        'moe_w2': nrm((N_ODD, N_EXPERTS, EXPERT_DIM, D), EXPERT_DIM ** -0.5),
    }


def reference(x, c, ctx, c_ctx, w_mod, b_mod, norm_mix, norm_ffn, final_norm,
              w_in_ab, lru_conv_w, lru_conv_b, lru_wa, lru_ba, lru_wx, lru_bx, lru_lam, attn_sink, w_out_ab,
              ffn_w1, ffn_w3, ffn_w2,
              w_in_cd, s5_a_re, s5_a_im, s5_log_step, s5_b_re, s5_b_im, s5_c_re, s5_c_im, s5_d, s5_w_glu, s5_b_glu,
              hg_lb_raw, hg_norm, w_out_cd,
              moe_router, moe_w1, moe_w3, moe_w2):
    rows = x.shape[1] // GRID_W
    rope = axial_rope_tables(rows)
    lb_soft = jax.nn.softmax(hg_lb_raw.astype(jnp.float32), axis=0)
    lb_table = jnp.cumsum(lb_soft, axis=0) - lb_soft[0:1]
    silu_c = jax.nn.silu(c)
    silu_cc = jax.nn.silu(c_ctx)
    xc = ctx
    for l in range(DEPTH):
        need_ctx = l < DEPTH - 1
        j = l // 2
        mod_x = jnp.split((silu_c @ w_mod[l] + b_mod[l])[:, None, :], N_MOD, axis=-1)
        mod_c = jnp.split(silu_cc @ w_mod[l] + b_mod[l], N_MOD, axis=-1)
        hx = rms_norm(x, norm_mix[l]) * (1.0 + mod_x[1]) + mod_x[0]
        hc = rms_norm(xc, norm_mix[l]) * (1.0 + mod_c[1]) + mod_c[0]
        if l % 2 == 0:
            dx, dc = mixer_ab(hx, hc, need_ctx, rope, w_in_ab[j], lru_conv_w[j], lru_conv_b[j],
                              lru_wa[j], lru_ba[j], lru_wx[j], lru_bx[j], lru_lam[j], attn_sink[j], w_out_ab[j])
        else:
            dx, dc = mixer_cd(hx, hc, need_ctx, w_in_cd[j], s5_a_re[j], s5_a_im[j], s5_log_step[j],
                              s5_b_re[j], s5_b_im[j], s5_c_re[j], s5_c_im[j], s5_d[j], s5_w_glu[j], s5_b_glu[j],
                              lb_table[j], hg_norm[j], w_out_cd[j])
        x = x + mod_x[2] * dx
        hx = rms_norm(x, norm_ffn[l]) * (1.0 + mod_x[4]) + mod_x[3]
        if l % 2 == 0:
            x = x + mod_x[5] * swiglu(hx, ffn_w1[j], ffn_w3[j], ffn_w2[j])
        else:
            x = x + mod_x[5] * moe_swiglu(hx, moe_router[j], moe_w1[j], moe_w3[j], moe_w2[j])
        if need_ctx:
            xc = xc + mod_c[2] * dc
            hc = rms_norm(xc, norm_ffn[l]) * (1.0 + mod_c[4]) + mod_c[3]
            if l % 2 == 0:
                xc = xc + mod_c[5] * swiglu(hc, ffn_w1[j], ffn_w3[j], ffn_w2[j])
            else:
                xc = xc + mod_c[5] * moe_swiglu(hc, moe_router[j], moe_w1[j], moe_w3[j], moe_w2[j])
    return rms_norm(x, final_norm)
```

```python
import contextlib
import math
import numpy as np
import concourse.bass as bass
import concourse.mybir as mybir
from concourse.bass_utils import run_bass_kernel_spmd

F32 = mybir.dt.float32
BF16 = mybir.dt.bfloat16
AF = mybir.ActivationFunctionType
ALU = mybir.AluOpType

D = 1024
T = 8192
TC = 256
TA = T + TC
DEPTH = 4
EPS = 1e-6
NDS = 24
SAME_SYNC = True
FFD = 2816
EXD = 1408
NEXP = 8


class Reg:
    __slots__ = ("w", "r")

    def __init__(self):
        self.w = None
        self.r = {}


class TT:
    def __init__(self, t, reg=None):
        self.t = t
        self.g = reg if reg is not None else Reg()

    def __getitem__(self, k):
        return self.t[k]


class KB:
    def __init__(self, nc):
        self.nc = nc
        self.E = {"pe": nc.tensor, "act": nc.scalar, "dve": nc.vector, "pool": nc.gpsimd, "sp": nc.sync}
        self.es = contextlib.ExitStack()
        self.sem = {e: self.es.enter_context(nc.semaphore("s_" + e)) for e in self.E}
        self.cnt = {e: 0 for e in self.E}
        self.known = {e: {} for e in self.E}
        self.dsem = [self.es.enter_context(nc.semaphore("d%d" % i)) for i in range(NDS)]
        self.dcnt = [0] * NDS
        self.dnext = 0
        self.ps = [TT(self.es.enter_context(nc.psum_tensor("ps%d" % i, [128, 512], F32))) for i in range(8)]
        self.psn = 0
        self.uid = 0
        self.ninst = 0

    def _need(self, eng, tok):
        if tok is None:
            return
        kind, key, val = tok
        if kind == "e" and key == eng and (eng == "pe" or not SAME_SYNC):
            return
        kk = (kind, key)
        if self.known[eng].get(kk, 0) >= val:
            return
        sem = self.sem[key] if kind == "e" else self.dsem[key]
        self.E[eng].wait_ge(sem, val)
        self.known[eng][kk] = val

    def _deps(self, eng, R, W):
        for r in R:
            self._need(eng, r.g.w)
        for w in W:
            self._need(eng, w.g.w)
            for t in list(w.g.r.values()):
                self._need(eng, t)

    def _commit(self, tok, R, W):
        for r in R:
            r.g.r[(tok[0], tok[1])] = tok
        for w in W:
            w.g.w = tok
            w.g.r = {}

    def op(self, eng, fn, R=(), W=()):
        self._deps(eng, R, W)
        inst = fn(self.E[eng])
        self.cnt[eng] += 1
        inst.then_inc(self.sem[eng], 1)
        tok = ("e", eng, self.cnt[eng])
        self._commit(tok, R, W)
        self.ninst += 1
        return tok

    def dma(self, q, out, in_, R=(), W=(), **kw):
        i = self.dnext
        self.dnext = (self.dnext + 1) % NDS
        if self.dcnt[i] > 0:
            self._need(q, ("d", i, 16 * self.dcnt[i]))
        self._deps(q, R, W)
        inst = self.E[q].dma_start(out=out, in_=in_, **kw)
        inst.then_inc(self.dsem[i], 16)
        self.dcnt[i] += 1
        tok = ("d", i, 16 * self.dcnt[i])
        self._commit(tok, R, W)
        self.ninst += 1
        return tok

    def barrier(self):
        for e in self.E:
            for o in self.E:
                if o != e and self.cnt[o] > 0:
                    self._need(e, ("e", o, self.cnt[o]))
            for i in range(NDS):
                if self.dcnt[i] > 0:
                    self._need(e, ("d", i, 16 * self.dcnt[i]))

    @contextlib.contextmanager
    def phase(self):
        ph = Phase(self)
        with ph.es:
            yield ph
            self.barrier()

    def bank(self):
        p = self.ps[self.psn]
        self.psn = (self.psn + 1) % 8
        return p

    def dram(self, name, shape, dt):
        return TT(self.nc.dram_tensor(name, shape, dt, kind="Internal").ap())

    def mm(self, out, lhsT, rhs, start, stop, R, W):
        return self.op("pe", lambda e: e.matmul(out, lhsT=lhsT, rhs=rhs, start=start, stop=stop), R=R, W=W)

    def act(self, out, in_, func, R, W, bias=None, scale=None):
        kw = {}
        if bias is not None:
            kw["bias"] = bias
        if scale is not None:
            kw["scale"] = scale
        return self.op("act", lambda e: e.activation(out=out, in_=in_, func=func, **kw), R=R, W=W)

    def ts(self, out, in0, s1, s2, op0, op1, R, W, eng="dve"):
        if op1 is None:
            return self.op(eng, lambda e: e.tensor_scalar(out=out, in0=in0, scalar1=s1, scalar2=None, op0=op0), R=R, W=W)
        return self.op(eng, lambda e: e.tensor_scalar(out=out, in0=in0, scalar1=s1, scalar2=s2, op0=op0, op1=op1), R=R, W=W)

    def stt(self, out, in0, scalar, in1, op0, op1, R, W):
        return self.op("dve", lambda e: e.scalar_tensor_tensor(out=out, in0=in0, scalar=scalar, in1=in1, op0=op0, op1=op1), R=R, W=W)

    def tt(self, out, in0, in1, op, R, W, eng="dve"):
        return self.op(eng, lambda e: e.tensor_tensor(out=out, in0=in0, in1=in1, op=op), R=R, W=W)

    def cp(self, out, in_, R, W, eng="dve"):
        if eng == "act":
            return self.op("act", lambda e: e.copy(out=out, in_=in_), R=R, W=W)
        return self.op(eng, lambda e: e.tensor_copy(out=out, in_=in_), R=R, W=W)

    def recip(self, out, in_, R, W):
        return self.op("dve", lambda e: e.reciprocal(out=out, in_=in_), R=R, W=W)

    def memset(self, t, val, eng="pool"):
        return self.op(eng, lambda e: e.memset(t.t[:], val), W=[t])


class Phase:
    def __init__(self, kb):
        self.kb = kb
        self.es = contextlib.ExitStack()

    def sb(self, name, shape, dt):
        self.kb.uid += 1
        return TT(self.es.enter_context(self.kb.nc.sbuf_tensor("%s_%d" % (name, self.kb.uid), list(shape), dt)))


def tok_chunks():
    return [(0, TC)] + [(TC + 512 * i, 512) for i in range(T // 512)]


class Prog:
    def __init__(self, nc, depth_run=DEPTH, debug=False, layers=None, skip=()):
        self.skip = set(skip)
        self.nc = nc
        self.kb = KB(nc)
        self.depth_run = depth_run
        self.layers = list(range(depth_run)) if layers is None else layers
        self.debug = debug
        self.inp = {}

    def din(self, name, shape, dt=F32):
        a = self.nc.dram_tensor(name, list(shape), dt, kind="ExternalInput").ap()
        self.inp[name] = a
        return a

    def declare(self):
        d = self.din
        d("x", [T, D]); d("c", [D]); d("ctx", [TC, D]); d("c_ctx", [D])
        d("w_mod", [DEPTH, D, 6 * D]); d("b_mod", [DEPTH, 6 * D])
        d("norm_mix", [DEPTH, D]); d("norm_ffn", [DEPTH, D]); d("final_norm", [D])
        d("w_in_ab", [2, D, 1792]); d("lru_conv_w", [2, 4, 512]); d("lru_conv_b", [2, 512])
        d("lru_wa", [2, 2, 8, 64, 64]); d("lru_ba", [2, 2, 512]); d("lru_wx", [2, 2, 8, 64, 64]); d("lru_bx", [2, 2, 512])
        d("lru_lam", [2, 2, 512]); d("attn_sink", [2, 8]); d("w_out_ab", [2, D, D])
        d("ffn_w1", [2, D, FFD]); d("ffn_w3", [2, D, FFD]); d("ffn_w2", [2, FFD, D])
        d("w_in_cd", [2, D, 3072])
        d("s5_a_re", [2, 2, 32, 64]); d("s5_a_im", [2, 2, 32, 64]); d("s5_log_step", [2, 2, 32])
        d("s5_b_re", [2, 2, 32, 64, 16]); d("s5_b_im", [2, 2, 32, 64, 16])
        d("s5_c_re", [2, 2, 32, 16, 64]); d("s5_c_im", [2, 2, 32, 16, 64])
        d("s5_d", [2, 512]); d("s5_w_glu", [2, 512, 512]); d("s5_b_glu", [2, 512])
        d("hg_lb_raw", [2, 512]); d("hg_norm", [2, 128]); d("w_out_cd", [2, D, D])
        d("moe_router", [2, D, 8]); d("moe_w1", [2, 8, D, EXD]); d("moe_w3", [2, 8, D, EXD]); d("moe_w2", [2, 8, EXD, D])
        d("k_ident", [128, 128]); d("k_perm", [128, 128]); d("k_ropeC", [128, T]); d("k_ropeS", [128, T])
        d("k_mprev", [128, 512]); d("k_mnext", [128, 512]); d("k_sel", [8, 8 * 128])
        d("k_j1", [128, TA // 16]); d("k_m01", [128, 1024]); d("k_mask128", [128, 128])
        self.y = self.nc.dram_tensor("y", [T, D], F32, kind="ExternalOutput").ap()
        if self.debug:
            self.dbg = self.nc.dram_tensor("dbg", [D, TA], F32, kind="ExternalOutput").ap()
            self.dbgm = self.nc.dram_tensor("dbgm", [D, TA], BF16, kind="ExternalOutput").ap()
        kb = self.kb
        self.XT = kb.dram("XT", [D, TA], F32)
        self.ZT = kb.dram("ZT", [3072, TA], F32)
        self.MT = kb.dram("MT", [D, TA], BF16)
        self.VT = kb.dram("VT", [TA, 128], BF16)
        self.U = kb.dram("U", [20, 128, 11, 3072], BF16)
        self.YG = kb.dram("YG", [512, TA], BF16)

    def build(self):
        nc = self.nc
        kb = self.kb
        self.declare()
        with contextlib.ExitStack() as gs:
            gs.enter_context(nc.allow_non_contiguous_dma(reason="small strided parameter loads"))
            gs.enter_context(nc.allow_low_precision(reason="bf16 matmul operands, fp32 accumulation"))
            self.G = Phase(kb)
            gs.enter_context(self.G.es)
            self.setup_globals()
            self.phase0_mod()
            self.convert_weights()
            self.ingest()
            for l in self.layers:
                if l % 2 == 0:
                    self.layer_ab(l)
                else:
                    self.layer_cd(l)
            if self.debug:
                self.dump_xt()
            self.final()
            kb.barrier()
        kb.es.close()
        return nc

    def setup_globals(self):
        kb = self.kb
        G = self.G
        I = self.inp
        self.ident = G.sb("ident", [128, 128], F32)
        kb.dma("sp", self.ident[:], I["k_ident"], W=[self.ident])
        self.identb = G.sb("identb", [128, 128], BF16)
        kb.cp(self.identb[:], self.ident[:], R=[self.ident], W=[self.identb])
        self.onesb = G.sb("onesb", [128, 128], BF16)
        kb.memset(self.onesb, 1.0)
        self.MOD = G.sb("MOD", [128, DEPTH, 48, 2], F32)
        self.G1 = G.sb("G1", [128, DEPTH, 8, 2], F32)
        self.G4 = G.sb("G4", [128, DEPTH, 8, 2], F32)
        self.oneb = G.sb("oneb", [128, 1], F32)
        kb.memset(self.oneb, 1.0)
        self.epsb = G.sb("epsb", [128, 1], F32)
        kb.memset(self.epsb, EPS)
        self.fn = G.sb("fn", [128, 8], F32)
        kb.dma("sp", self.fn[:], I["final_norm"].rearrange("(k p) -> p k", p=128), W=[self.fn])

    def phase0_mod(self):
        kb = self.kb
        I = self.inp
        with kb.phase() as ph:
            cs = ph.sb("cs", [128, 8, 2], F32)
            kb.dma("sp", cs[:, :, 0], I["c"].rearrange("(k p) -> p k", p=128), W=[cs])
            kb.dma("sp", cs[:, :, 1], I["c_ctx"].rearrange("(k p) -> p k", p=128), W=[cs])
            sc = ph.sb("sc", [128, 8, 2], F32)
            kb.act(sc[:], cs[:], AF.Silu, R=[cs], W=[sc])
            bm = ph.sb("bm", [128, DEPTH, 48], F32)
            for l_ in range(DEPTH):
                kb.dma("sp", bm[:, l_, :], I["b_mod"][l_].rearrange("(j p) -> p j", p=128), W=[bm])
            nm = ph.sb("nm", [128, DEPTH, 8], F32)
            for l_ in range(DEPTH):
                kb.dma("sp", nm[:, l_, :], I["norm_mix"][l_].rearrange("(k p) -> p k", p=128), W=[nm])
            nf = ph.sb("nf", [128, DEPTH, 8], F32)
            for l_ in range(DEPTH):
                kb.dma("sp", nf[:, l_, :], I["norm_ffn"][l_].rearrange("(k p) -> p k", p=128), W=[nf])
            wm = [ph.sb("wm%d" % i, [128, 8, 1024], F32) for i in range(2)]
            it = 0
            for l in range(DEPTH):
                for m in range(6):
                    w = wm[it % 2]
                    it += 1
                    kb.dma("sp", w[:], I["w_mod"][l].rearrange("(k p) n -> p k n", p=128)[:, :, m * 1024:(m + 1) * 1024], W=[w])
                    for dc in range(8):
                        pb = kb.bank()
                        for k in range(8):
                            kb.mm(pb[:, 0:2], w[:, k, dc * 128:(dc + 1) * 128], sc[:, k, :], k == 0, k == 7, R=[w, sc], W=[pb])
                        j = m * 8 + dc
                        kb.ts(self.MOD[:, l, j, :], pb[:, 0:2], bm[:, l, j:j + 1], None, ALU.add, None, R=[pb, bm], W=[self.MOD])
            for l in range(DEPTH):
                for s in range(2):
                    kb.stt(self.G1[:, l, :, s], self.MOD[:, l, 8:16, s], 1.0, nm[:, l, :], ALU.add, ALU.mult, R=[self.MOD, nm], W=[self.G1])
                    kb.stt(self.G4[:, l, :, s], self.MOD[:, l, 32:40, s], 1.0, nf[:, l, :], ALU.add, ALU.mult, R=[self.MOD, nf], W=[self.G4])

    def convert_weights(self):
        kb = self.kb
        I = self.inp
        need = []
        for l in self.layers:
            j = l // 2
            if l % 2 == 0:
                for pe in range(2):
                    need.append((j * 2 + pe, I["ffn_w1"][j][:, pe * EXD:(pe + 1) * EXD], I["ffn_w3"][j][:, pe * EXD:(pe + 1) * EXD],
                                 I["ffn_w2"][j][pe * EXD:(pe + 1) * EXD, :]))
            else:
                for e in range(NEXP):
                    need.append((4 + j * 8 + e, I["moe_w1"][j, e], I["moe_w3"][j, e], I["moe_w2"][j, e]))
        with kb.phase() as ph:
            sf = [ph.sb("cvf%d" % i, [128, 8, EXD], F32) for i in range(3)]
            sbb = [ph.sb("cvb%d" % i, [128, 11, 1024], BF16) for i in range(2)]
            it = 0
            engs = ["pool", "dve", "act"]
            for (u, w1, w3, w2) in need:
                for mi, w in enumerate((w1, w3)):
                    f = sf[it % 3]; b = sbb[it % 2]
                    kb.dma("sp", f[:], w.rearrange("(k p) n -> p k n", p=128), W=[f])
                    eng = engs[it % 3]
                    it += 1
                    for k in range(8):
                        kb.cp(b[:, :, k * 128:(k + 1) * 128], f[:, k, :].rearrange("p (f c) -> p f c", c=128), R=[f], W=[b], eng=eng)
                    kb.dma("sp", self.U[u, :, :, mi * 1024:(mi + 1) * 1024], b[:], R=[b], W=[self.U])
                f = sf[it % 3]; b = sbb[it % 2]
                fv = f.t[:].rearrange("p k n -> p (k n)")[:, 0:11 * 1024].rearrange("p (f n) -> p f n", n=1024)
                kb.dma("sp", fv, w2.rearrange("(f p) n -> p f n", p=128), W=[f])
                eng = engs[it % 3]
                it += 1
                kb.cp(b[:], fv, R=[f], W=[b], eng=eng)
                kb.dma("sp", self.U[u, :, :, 2048:3072], b[:], R=[b], W=[self.U])

    def ingest(self):
        kb = self.kb
        I = self.inp
        with kb.phase() as ph:
            tin = [ph.sb("tin%d" % i, [128, D], F32) for i in range(3)]
            st = [ph.sb("tst%d" % i, [128, 8, 512], F32) for i in range(2)]
            ci = 0
            ti = 0
            for (t0, n) in tok_chunks():
                s = st[ci % 2]
                ci += 1
                for tl in range(n // 128):
                    a = tin[ti % 3]
                    ti += 1
                    tt0 = t0 + tl * 128
                    src = I["ctx"][tt0:tt0 + 128, :] if tt0 < TC else I["x"][tt0 - TC:tt0 - TC + 128, :]
                    kb.dma("sp", a[:], src, W=[a])
                    for h in range(2):
                        pb = kb.bank()
                        for q in range(4):
                            k = h * 4 + q
                            kb.op("pe", lambda e, pb=pb, q=q, k=k, a=a: e.transpose(pb[:, q * 128:(q + 1) * 128], a[:, k * 128:(k + 1) * 128], self.ident[:]),
                                  R=[a, self.ident], W=[pb])
                        kb.cp(s[:, h * 4:(h + 1) * 4, tl * 128:(tl + 1) * 128], pb[:].rearrange("p (q t) -> p q t", t=128), R=[pb], W=[s],
                              eng=("act" if h else "dve"))
                kb.dma("sp", self.XT.t.rearrange("(k p) t -> p k t", p=128)[:, :, t0:t0 + n], s[:, :, 0:n], R=[s], W=[self.XT])

    def normmod(self, ph, xin, n, Gt, M0, sidx, hT, tmp, fp32out=False):
        kb = self.kb
        sq = tmp["sq"]
        xr = getattr(xin, "regs", None) or [xin]
        kb.act(sq[:, :, 0:n], xin[:, :, 0:n], AF.Square, R=xr, W=[sq])
        pb = kb.bank()
        for k in range(8):
            kb.mm(pb[:, 0:n], self.onesb[:], sq[:, k, 0:n], k == 0, k == 7, R=[sq, self.onesb], W=[pb])
        rs = tmp["rs"]
        kb.act(rs[:, 0:n], pb[:, 0:n], AF.Sqrt, R=[pb], W=[rs], bias=self.epsb[:, 0:1], scale=1.0 / D)
        kb.recip(rs[:, 0:n], rs[:, 0:n], R=[rs], W=[rs])
        hf = tmp["hf"]
        for k in range(8):
            kb.stt(hf[:, k, 0:n], xin[:, k, 0:n], Gt(k), rs[:, 0:n], ALU.mult, ALU.mult, R=[xr[k] if len(xr) == 8 else xr[0], rs, self.G1, self.G4], W=[hf])
            if fp32out:
                kb.act(hf[:, k, 0:n], hf[:, k, 0:n], AF.Identity, R=[hf, self.MOD], W=[hf], bias=M0(k))
                kb.cp(hT[:, k, 0:n], hf[:, k, 0:n], R=[hf], W=[hT], eng="pool")
            else:
                kb.act(hT[:, k, 0:n], hf[:, k, 0:n], AF.Identity, R=[hf, self.MOD], W=[hT], bias=M0(k))

    def layer_ab(self, l):
        kb = self.kb
        I = self.inp
        j = l // 2
        need_ctx = l < DEPTH - 1
        with kb.phase() as ph:
            W = ph.sb("Win", [128, 8, 1792], BF16)
            wv = I["w_in_ab"][j].rearrange("(k p) n -> p k n", p=128)
            kb.dma("pool", W[:, :, 0:1024], wv[:, :, 0:1024], W=[W])
            for jj in range(4):
                for hh in range(2):
                    kb.dma("pool", W[:, :, 1024 + jj * 128 + hh * 64:1024 + jj * 128 + hh * 64 + 64],
                           wv[:, :, 1024 + (hh * 4 + jj) * 64:1024 + (hh * 4 + jj) * 64 + 64], W=[W])
            kb.dma("pool", W[:, :, 1536:1792], wv[:, :, 1536:1792], W=[W])
            self.p1_generic(ph, l, W, 13, lambda oc: (oc * 128, oc * 128), vcol=1664)
        self.lru(l)
        self.attn(l)
        self.p3(l, I["w_out_ab"][j], [(j * 2 + pe) for pe in range(2)], moe=None)

    def p1_generic(self, ph, l, W, nch, colrow, vcol=None):
        kb = self.kb
        xs = [ph.sb("xin%d" % i, [128, 8, 512], F32) for i in range(2)]
        hs = [ph.sb("hT%d" % i, [128, 8, 512], BF16) for i in range(2)]
        tmp = {"sq": ph.sb("sq", [128, 8, 512], BF16), "rs": ph.sb("rs", [128, 512], F32), "hf": ph.sb("hf", [128, 8, 512], F32)}
        zs = [ph.sb("zs%d" % i, [128, 512], F32) for i in range(4)]
        vs = [ph.sb("vs%d" % i, [128, 128], BF16) for i in range(2)]
        XTv = self.XT.t.rearrange("(k p) t -> p k t", p=128)
        zi = 0
        vi = 0
        for ci, (t0, n) in enumerate(tok_chunks()):
            s = 1 if t0 < TC else 0
            xin = xs[ci % 2]
            hT = hs[ci % 2]
            kb.dma("sp", xin[:, :, 0:n], XTv[:, :, t0:t0 + n], R=[self.XT], W=[xin])
            self.normmod(ph, xin, n, lambda k: self.G1[:, l, k, s:s + 1], lambda k: self.MOD[:, l, k, s:s + 1], s, hT, tmp)
            for oc in range(nch):
                c0, r0 = colrow(oc)
                pb = kb.bank()
                for k in range(8):
                    kb.mm(pb[:, 0:n], W[:, k, c0:c0 + 128], hT[:, k, 0:n], k == 0, k == 7, R=[W, hT], W=[pb])
                z = zs[zi % 4]
                zi += 1
                kb.cp(z[:, 0:n], pb[:, 0:n], R=[pb], W=[z], eng=("act" if zi % 2 else "dve"))
                kb.dma("sp", self.ZT[r0:r0 + 128, t0:t0 + n], z[:, 0:n], R=[z], W=[self.ZT])
            if vcol is not None:
                for tl in range(n // 128):
                    pb = kb.bank()
                    for k in range(8):
                        kb.mm(pb[:, 0:128], hT[:, k, tl * 128:(tl + 1) * 128], W[:, k, vcol:vcol + 128], k == 0, k == 7, R=[W, hT], W=[pb])
                    v = vs[vi % 2]
                    vi += 1
                    kb.cp(v[:], pb[:, 0:128], R=[pb], W=[v], eng="dve")
                    kb.dma("sp", self.VT[t0 + tl * 128:t0 + (tl + 1) * 128, :], v[:], R=[v], W=[self.VT])

    def lru(self, l):
        kb = self.kb
        I = self.inp
        j = l // 2
        with kb.phase() as ph:
            cw = ph.sb("cw", [128, 4, 4], F32)
            for tp in range(4):
                kb.dma("sp", cw[:, tp, :], I["lru_conv_w"][j, tp].rearrange("(c p) -> p c", p=128), W=[cw])
            cb = ph.sb("cb", [128, 4], F32)
            kb.dma("sp", cb[:], I["lru_conv_b"][j].rearrange("(c p) -> p c", p=128), W=[cb])
            ba = ph.sb("ba", [128, 2, 4], F32)
            for d_ in range(2):
                kb.dma("sp", ba[:, d_, :], I["lru_ba"][j, d_].rearrange("(c p) -> p c", p=128), W=[ba])
            bx = ph.sb("bx", [128, 2, 4], F32)
            for d_ in range(2):
                kb.dma("sp", bx[:, d_, :], I["lru_bx"][j, d_].rearrange("(c p) -> p c", p=128), W=[bx])
            lam = ph.sb("lam", [128, 2, 4], F32)
            for d_ in range(2):
                kb.dma("sp", lam[:, d_, :], I["lru_lam"][j, d_].rearrange("(c p) -> p c", p=128), W=[lam])
            cl = ph.sb("cl", [128, 2, 4], F32)
            kb.act(cl[:], lam[:], AF.Exp, R=[lam], W=[cl], scale=-1.0)
            kb.act(cl[:], cl[:], AF.Ln, R=[cl], W=[cl], bias=self.oneb[:, 0:1])
            kb.ts(cl[:], cl[:], -8.0, None, ALU.mult, None, R=[cl], W=[cl])
            uraw = ph.sb("uraw", [128, TA], F32)
            u = ph.sb("u", [128, TA], F32)
            ub = ph.sb("ub", [128, TA], BF16)
            H = ph.sb("H", [128, TA], F32)
            gg = ph.sb("gg", [128, TA], F32)
            ob = ph.sb("ob", [128, TA], BF16)
            wg = [[ph.sb("wg%d%d" % (d, a), [128, 128], BF16) for a in range(2)] for d in range(2)]
            tmpn = ["r", "i", "a", "m", "b", "hb"]
            tm = {nm: [ph.sb("l%s%d" % (nm, i), [128, 512], F32) for i in range(2)] for nm in tmpn}
            segs = [(0, TC), (TC, T)]
            for c in range(4):
                kb.dma("sp", uraw[:], self.ZT[(4 + c) * 128:(5 + c) * 128, :], R=[self.ZT], W=[uraw])
                kb.dma("sp", gg[:], self.ZT[c * 128:(c + 1) * 128, :], R=[self.ZT], W=[gg])
                for d in range(2):
                    for a, nmw in enumerate(("lru_wa", "lru_wx")):
                        kb.memset(wg[d][a], 0.0)
                        for b2 in range(2):
                            kb.dma("pool", wg[d][a][b2 * 64:(b2 + 1) * 64, b2 * 64:(b2 + 1) * 64], I[nmw][j, d, 2 * c + b2], W=[wg[d][a]])
                for (s0, ln) in segs:
                    kb.ts(u[:, s0:s0 + ln], uraw[:, s0:s0 + ln], cw[:, 2, c:c + 1], cb[:, c:c + 1], ALU.mult, ALU.add, R=[uraw, cw, cb], W=[u])
                    kb.stt(u[:, s0 + 2:s0 + ln], uraw[:, s0:s0 + ln - 2], cw[:, 0, c:c + 1], u[:, s0 + 2:s0 + ln], ALU.mult, ALU.add, R=[uraw, cw, u], W=[u])
                    kb.stt(u[:, s0 + 1:s0 + ln], uraw[:, s0:s0 + ln - 1], cw[:, 1, c:c + 1], u[:, s0 + 1:s0 + ln], ALU.mult, ALU.add, R=[uraw, cw, u], W=[u])
                    kb.stt(u[:, s0:s0 + ln - 1], uraw[:, s0 + 1:s0 + ln], cw[:, 3, c:c + 1], u[:, s0:s0 + ln - 1], ALU.mult, ALU.add, R=[uraw, cw, u], W=[u])
                kb.cp(ub[:], u[:], R=[u], W=[ub], eng="act")
                for d in range(2):
                    chunks = tok_chunks()
                    order = chunks if d == 0 else [chunks[0]] + chunks[:0:-1]
                    carry = None
                    for ci, (t0, n) in enumerate(order):
                        b_ = ci % 2
                        r, i_, a, m, b, hb = (tm[nm][b_] for nm in tmpn)
                        pa = kb.bank()
                        kb.mm(pa[:, 0:n], wg[d][0][:], ub[:, t0:t0 + n], True, True, R=[wg[d][0], ub], W=[pa])
                        px = kb.bank()
                        kb.mm(px[:, 0:n], wg[d][1][:], ub[:, t0:t0 + n], True, True, R=[wg[d][1], ub], W=[px])
                        kb.act(r[:, 0:n], pa[:, 0:n], AF.Sigmoid, R=[pa, ba], W=[r], bias=ba[:, d, c:c + 1])
                        kb.act(i_[:, 0:n], px[:, 0:n], AF.Sigmoid, R=[px, bx], W=[i_], bias=bx[:, d, c:c + 1])
                        kb.act(a[:, 0:n], r[:, 0:n], AF.Exp, R=[r, cl], W=[a], scale=cl[:, d, c:c + 1])
                        kb.act(m[:, 0:n], a[:, 0:n], AF.Square, R=[a], W=[m])
                        kb.act(m[:, 0:n], m[:, 0:n], AF.Sqrt, R=[m], W=[m], bias=self.oneb[:, 0:1], scale=-1.0)
                        kb.tt(b[:, 0:n], i_[:, 0:n], u[:, t0:t0 + n], ALU.mult, R=[i_, u], W=[b])
                        kb.tt(b[:, 0:n], b[:, 0:n], m[:, 0:n], ALU.mult, R=[b, m], W=[b])
                        if d == 0:
                            init = 0.0 if carry is None else H[:, t0 - 1:t0]
                            kb.op("dve", lambda e, n=n, t0=t0, a=a, b=b, init=init: e.tensor_tensor_scan(
                                out=H[:, t0:t0 + n], data0=a[:, 0:n], data1=b[:, 0:n], initial=init, op0=ALU.mult, op1=ALU.add),
                                R=[a, b, H], W=[H])
                            carry = True
                        else:
                            init = 0.0 if carry is None else carry[:, 0:1]
                            rd = [a, b] + ([tm["hb"][1 - b_]] if carry is not None else [])
                            kb.op("dve", lambda e, n=n, a=a, b=b, hb=hb, init=init: e.tensor_tensor_scan(
                                out=hb[:, 0:n][:, ::-1], data0=a[:, 0:n][:, ::-1], data1=b[:, 0:n][:, ::-1], initial=init,
                                op0=ALU.mult, op1=ALU.add), R=rd, W=[hb])
                            kb.tt(H[:, t0:t0 + n], H[:, t0:t0 + n], hb[:, 0:n], ALU.add, R=[H, hb], W=[H])
                            carry = hb
                kb.act(gg[:], gg[:], AF.Gelu_apprx_tanh, R=[gg], W=[gg])
                kb.tt(ob[:], H[:], gg[:], ALU.mult, R=[H, gg], W=[ob])
                kb.dma("sp", self.MT[c * 128:(c + 1) * 128, :], ob[:], R=[ob], W=[self.MT])

    def attn(self, l):
        kb = self.kb
        I = self.inp
        j = l // 2
        need_ctx = l < DEPTH - 1
        NT = TA // 128
        with kb.phase() as ph:
            QT = ph.sb("QT", [128, 4, TA], BF16)
            KT = ph.sb("KT", [128, TA], BF16)
            VK = ph.sb("VK", [128, NT, 128], BF16)
            kb.dma("sp", VK[:], self.VT.t.rearrange("(n p) d -> p n d", p=128), R=[self.VT], W=[VK])
            perm = ph.sb("perm", [128, 128], F32)
            kb.dma("sp", perm[:], I["k_perm"], W=[perm])
            mb = []
            for nm in ("k_mprev", "k_mnext"):
                mf = ph.sb(nm + "f", [128, 512], F32)
                kb.dma("sp", mf[:], I[nm], W=[mf])
                m_ = ph.sb(nm + "b", [128, 512], BF16)
                kb.cp(m_[:], mf[:], R=[mf], W=[m_])
                mb.append(m_)
            sk = ph.sb("sk", [64, 8], F32)
            kb.dma("sp", sk[:], I["attn_sink"][j].partition_broadcast(64), W=[sk])
            kb.act(sk[:], sk[:], AF.Exp, R=[sk], W=[sk])
            sinkE = ph.sb("sinkE", [64, 2, 4, 128], F32)
            for g in range(2):
                for hj in range(4):
                    kb.cp(sinkE[:, g, hj, :], sk[:, g * 4 + hj:g * 4 + hj + 1].to_broadcast([64, 128]), R=[sk], W=[sinkE])
            zq = [ph.sb("zq%d" % i, [128, 512], F32) for i in range(3)]
            rc = [ph.sb("rc%d" % i, [128, 512], F32) for i in range(2)]
            rs_ = [ph.sb("rsn%d" % i, [128, 512], F32) for i in range(2)]
            t1 = [ph.sb("t1%d" % i, [128, 512], F32) for i in range(2)]
            t2 = [ph.sb("t2%d" % i, [128, 512], F32) for i in range(2)]
            zi = 0
            for ci, (t0, n) in enumerate(tok_chunks()):
                if t0 >= TC:
                    C_ = rc[ci % 2]; S_ = rs_[ci % 2]
                    kb.dma("sp", C_[:], I["k_ropeC"][:, t0 - TC:t0 - TC + n], W=[C_])
                    kb.dma("sp", S_[:], I["k_ropeS"][:, t0 - TC:t0 - TC + n], W=[S_])
                for fc in range(5):
                    z = zq[zi % 3]
                    zi += 1
                    kb.dma("sp", z[:, 0:n], self.ZT[(8 + fc) * 128:(9 + fc) * 128, t0:t0 + n], R=[self.ZT], W=[z])
                    dst = QT[:, fc, t0:t0 + n] if fc < 4 else KT[:, t0:t0 + n]
                    dreg = QT if fc < 4 else KT
                    if t0 < TC:
                        kb.cp(dst, z[:, 0:n], R=[z], W=[dreg], eng="act")
                    else:
                        pb = kb.bank()
                        kb.mm(pb[:, 0:n], perm[:], z[:, 0:n], True, True, R=[perm, z], W=[pb])
                        a_ = t1[zi % 2]; b_ = t2[zi % 2]
                        kb.tt(a_[:, 0:n], z[:, 0:n], C_[:, 0:n], ALU.mult, R=[z, C_], W=[a_])
                        kb.tt(b_[:, 0:n], pb[:, 0:n], S_[:, 0:n], ALU.mult, R=[pb, S_], W=[b_])
                        kb.tt(dst, a_[:, 0:n], b_[:, 0:n], ALU.add, R=[a_, b_], W=[dreg])
            PT = [[ph.sb("PT%d_%d" % (i, k), [128, 512], BF16) for k in range(5)] for i in range(2)]
            den = [ph.sb("den%d" % i, [64, 512], F32) for i in range(2)]
            ot = [ph.sb("ot%d" % i, [64, 512], BF16) for i in range(2)]
            blocks = []
            if need_ctx:
                blocks += [(0, [(0, None), (128, None)]), (128, [(0, None), (128, None)])]
            for n_ in range(T // 128):
                t0 = TC + 128 * n_
                kts = []
                if n_ > 0:
                    kts.append((t0 - 128, 0))
                kts.append((t0, None))
                if n_ < T // 128 - 1:
                    kts.append((t0 + 128, 1))
                kts += [(0, None), (128, None)]
                blocks.append((t0, kts))
            bi = 0
            for (t0, kts) in blocks:
                for g in range(2):
                    pr = slice(g * 64, (g + 1) * 64)
                    pts = PT[bi % 2]
                    for ki, (tk, mk) in enumerate(kts):
                        pb = kb.bank()
                        kb.mm(pb[:], KT[pr, tk:tk + 128], QT[pr, :, t0:t0 + 128], True, mk is None, R=[KT, QT], W=[pb])
                        if mk is not None:
                            kb.mm(pb[:], self.identb[:], mb[mk][:], False, True, R=[self.identb, mb[mk]], W=[pb])
                        kb.act(pts[ki][:], pb[:], AF.Exp, R=[pb], W=[pts[ki]], scale=0.125)
                    po = kb.bank()
                    for ki, (tk, mk) in enumerate(kts):
                        kb.mm(po[0:64, :], VK[:, tk // 128, g * 64:(g + 1) * 64], pts[ki][:], ki == 0, ki == len(kts) - 1, R=[VK, pts[ki]], W=[po])
                    pd = kb.bank()
                    for ki, (tk, mk) in enumerate(kts):
                        kb.mm(pd[0:64, :], self.onesb[:, 0:64], pts[ki][:], ki == 0, ki == len(kts) - 1, R=[self.onesb, pts[ki]], W=[pd])
                    dn = den[bi % 2]; o_ = ot[bi % 2]
                    kb.tt(dn[:], pd[0:64, :], sinkE[:, g, :, :].rearrange("p j t -> p (j t)"), ALU.add, R=[pd, sinkE], W=[dn])
                    kb.recip(dn[:], dn[:], R=[dn], W=[dn])
                    kb.tt(o_[:], po[0:64, :], dn[:], ALU.mult, R=[po, dn], W=[o_])
                    kb.dma("sp", self.MT[512 + g * 256:512 + (g + 1) * 256, t0:t0 + 128].rearrange("(j d) t -> d j t", d=64),
                           o_[:].rearrange("p (j t) -> p j t", t=128), R=[o_], W=[self.MT])
                    bi += 1

    def p3(self, l, wout, units, moe):
        kb = self.kb
        I = self.inp
        need_ctx = l < DEPTH - 1
        j = l // 2
        with kb.phase() as ph:
            Wo = ph.sb("Wo", [128, 8, D], BF16)
            kb.dma("pool", Wo[:], wout.rearrange("(k p) n -> p k n", p=128), W=[Wo])
            x1 = ph.sb("x1", [128, 8, 1024], F32)
            x1r = [TT(x1.t, Reg()) for _ in range(8)]
            mt = [ph.sb("mt%d" % i, [128, 8, 512], BF16) for i in range(2)]
            h2 = ph.sb("h2", [128, 8, 1024], BF16)
            actb = ph.sb("actb", [128, 11, 1024], BF16)
            W2e = ph.sb("W2e", [128, 11, 1024], BF16)
            NR = 4
            ring = [ph.sb("ring%d" % i, [128, 2048], BF16) for i in range(NR)]
            tmp = {"sq": ph.sb("sq", [128, 8, 512], BF16), "rs": ph.sb("rs", [128, 512], F32), "hf": ph.sb("hf", [128, 8, 512], F32)}
            sa = [ph.sb("sa%d" % i, [128, 512], F32) for i in range(2)]
            if moe is not None:
                WE = ph.sb("WE", [128, 8, 1024], BF16)
                rt = ph.sb("rt", [128, 8, 8], F32)
                kb.dma("sp", rt[:], I["moe_router"][j].rearrange("(k p) e -> p k e", p=128), W=[rt])
                sel = ph.sb("sel", [8, 8 * 128], F32)
                kb.dma("sp", sel[:], I["k_sel"], W=[sel])
                selb = ph.sb("selb", [8, 8 * 128], BF16)
                kb.cp(selb[:], sel[:], R=[sel], W=[selb])
                lg = ph.sb("lg", [128, 8], F32)
                m8 = ph.sb("m8", [128, 8], F32)
                gt = ph.sb("gt", [128, 4], F32)
                wtk = ph.sb("wtk", [128, 8], F32)
                wtk2 = ph.sb("wtk2", [128, 8], F32)
                wT = ph.sb("wT", [8, 512], BF16)
            XTv = self.XT.t.rearrange("(k p) t -> p k t", p=128)
            MTv = self.MT.t.rearrange("(k p) t -> p k t", p=128)
            supers = [(0, TC)] if need_ctx else []
            supers += [(TC + 1024 * i, 1024) for i in range(T // 1024)]
            ri = 0
            mi = 0
            si = 0
            for (T0, SN) in supers:
                s = 1 if T0 < TC else 0
                subs = [(o, min(512, SN - o)) for o in range(0, SN, 512)]
                kb.dma("sp", x1[:, :, 0:SN], XTv[:, :, T0:T0 + SN], R=[self.XT], W=x1r)
                for (o, n) in subs:
                    m_ = mt[mi % 2]
                    mi += 1
                    kb.dma("sp", m_[:, :, 0:n], MTv[:, :, T0 + o:T0 + o + n], R=[self.MT], W=[m_])
                    for oc in range(8):
                        pb = kb.bank()
                        for k in range(8):
                            kb.mm(pb[:, 0:n], Wo[:, k, oc * 128:(oc + 1) * 128], m_[:, k, 0:n], k == 0, k == 7, R=[Wo, m_], W=[pb])
                        kb.stt(x1[:, oc, o:o + n], pb[:, 0:n], self.MOD[:, l, 16 + oc, s:s + 1], x1[:, oc, o:o + n], ALU.mult, ALU.add,
                               R=[pb, self.MOD, x1r[oc]], W=[x1r[oc]])
                    xv = TT(x1.t[:, :, o:o + n], None)
                    xv.regs = x1r
                    hv = TT(h2.t[:, :, o:o + n], h2.g)
                    self.normmod(ph, xv, n, lambda k: self.G4[:, l, k, s:s + 1], lambda k: self.MOD[:, l, 24 + k, s:s + 1], s, hv, tmp, fp32out=(moe is not None))
                    if moe is not None:
                        hf = tmp["hf"]
                        for tl in range(n // 128):
                            pb = kb.bank()
                            for k in range(8):
                                kb.mm(pb[:, 0:8], hf[:, k, tl * 128:(tl + 1) * 128], rt[:, k, :], k == 0, k == 7, R=[hf, rt], W=[pb])
                            kb.cp(lg[:], pb[:, 0:8], R=[pb], W=[lg])
                            kb.op("dve", lambda e: e.max(out=m8[:], in_=lg[:]), R=[lg], W=[m8])
                            kb.tt(gt[:, 0:1], m8[:, 1:2], m8[:, 0:1], ALU.subtract, R=[m8], W=[gt])
                            kb.act(gt[:, 1:2], gt[:, 0:1], AF.Exp, R=[gt], W=[gt])
                            kb.ts(gt[:, 2:3], gt[:, 1:2], 1.0, None, ALU.add, None, R=[gt], W=[gt])
                            kb.recip(gt[:, 2:3], gt[:, 2:3], R=[gt], W=[gt])
                            kb.tt(gt[:, 3:4], gt[:, 1:2], gt[:, 2:3], ALU.mult, R=[gt], W=[gt])
                            kb.ts(wtk[:], lg[:], m8[:, 0:1], gt[:, 2:3], ALU.is_equal, ALU.mult, R=[lg, m8, gt], W=[wtk])
                            kb.ts(wtk2[:], lg[:], m8[:, 1:2], gt[:, 3:4], ALU.is_equal, ALU.mult, R=[lg, m8, gt], W=[wtk2])
                            kb.tt(wtk[:], wtk[:], wtk2[:], ALU.add, R=[wtk, wtk2], W=[wtk])
                            pt_ = kb.bank()
                            kb.op("pe", lambda e, pt_=pt_: e.transpose(pt_[0:8, 0:128], wtk[:], self.ident[:]), R=[wtk, self.ident], W=[pt_])
                            kb.cp(wT[:, tl * 128:(tl + 1) * 128], pt_[0:8, 0:128], R=[pt_], W=[wT])
                        for e_ in range(NEXP):
                            pb = kb.bank()
                            kb.mm(pb[:, 0:n], selb[:, e_ * 128:(e_ + 1) * 128], wT[:, 0:n], True, True, R=[selb, wT], W=[pb])
                            kb.cp(WE[:, e_, o:o + n], pb[:, 0:n], R=[pb], W=[WE], eng="act")
                for ui, u in enumerate(units):
                    for fc in range(11):
                        rg = ring[ri % NR]
                        ri += 1
                        kb.dma("sp", rg[:], self.U[u, :, fc, 0:2048], R=[self.U], W=[rg])
                        for (o, n) in subs:
                            pa = kb.bank()
                            for k in range(8):
                                kb.mm(pa[:, 0:n], rg[:, k * 128:(k + 1) * 128], h2[:, k, o:o + n], k == 0, k == 7, R=[rg, h2], W=[pa])
                            pb = kb.bank()
                            for k in range(8):
                                kb.mm(pb[:, 0:n], rg[:, 1024 + k * 128:1024 + (k + 1) * 128], h2[:, k, o:o + n], k == 0, k == 7, R=[rg, h2], W=[pb])
                            s_ = sa[si % 2]
                            si += 1
                            kb.act(s_[:, 0:n], pa[:, 0:n], AF.Silu, R=[pa], W=[s_])
                            kb.tt(actb[:, fc, o:o + n], s_[:, 0:n], pb[:, 0:n], ALU.mult, R=[s_, pb], W=[actb])
                            if moe is not None:
                                kb.tt(actb[:, fc, o:o + n], actb[:, fc, o:o + n], WE[:, ui, o:o + n], ALU.mult, R=[actb, WE], W=[actb], eng="pool")
                    kb.dma("sp", W2e[:], self.U[u, :, :, 2048:3072], R=[self.U], W=[W2e])
                    for oc in range(8):
                        for (o, n) in subs:
                            pb = kb.bank()
                            for fc in range(11):
                                kb.mm(pb[:, 0:n], W2e[:, fc, oc * 128:(oc + 1) * 128], actb[:, fc, o:o + n], fc == 0, fc == 10, R=[W2e, actb], W=[pb])
                            kb.stt(x1[:, oc, o:o + n], pb[:, 0:n], self.MOD[:, l, 40 + oc, s:s + 1], x1[:, oc, o:o + n], ALU.mult, ALU.add,
                                   R=[pb, self.MOD, x1r[oc]], W=[x1r[oc]])
                        if ui == len(units) - 1:
                            kb.dma("sp", self.XT[oc * 128:(oc + 1) * 128, T0:T0 + SN], x1[:, oc, 0:SN], R=[x1r[oc]], W=[self.XT])

    def layer_cd(self, l):
        kb = self.kb
        I = self.inp
        j = l // 2
        with kb.phase() as ph:
            W = ph.sb("Wcd", [128, 8, 3072], BF16)
            wv = I["w_in_cd"][j].rearrange("(k p) n -> p k n", p=128)
            for h in range(2):
                kb.dma("pool", W[:, :, h * 1536:(h + 1) * 1536], wv[:, :, h * 1536:(h + 1) * 1536], W=[W])
            self.p1_generic(ph, l, W, 24, lambda oc: (oc * 128, oc * 128))
        if "s5" not in self.skip:
            self.s5(l)
            self.s5_glu(l)
        if "hg" not in self.skip:
            self.hgrn(l)
        if "moe" not in self.skip:
            self.p3(l, I["w_out_cd"][j], [4 + j * 8 + e for e in range(NEXP)], moe=True)

    def s5(self, l):
        kb = self.kb
        I = self.inp
        j = l // 2
        NJ = TA // 16
        HJ = NJ // 2
        MAGIC = 12582912.0
        TWO_PI = 2.0 * math.pi
        with kb.phase() as ph:
            smr = Reg()
            SM = TT(None, smr)

            def T_(name, shape, dt=F32):
                t = ph.sb(name, shape, dt)
                t.g = smr
                return t

            def tt_(o, a, b, op):
                kb.tt(o, a, b, op, R=[SM], W=[SM])

            def ts_(o, a, s1, s2, op0, op1=None):
                kb.ts(o, a, s1, s2, op0, op1, R=[SM], W=[SM])

            def act_(o, a, f, **kw):
                kb.act(o, a, f, R=[SM], W=[SM], **kw)

            def frac_(o, x, tmp):
                ts_(tmp, x, MAGIC, None, ALU.add)
                ts_(tmp, tmp, -MAGIC, None, ALU.add)
                tt_(o, x, tmp, ALU.subtract)

            PBpad = ph.sb("PBpad", [128, 4, 16, 2, 128], BF16)
            CPpad = ph.sb("CPpad", [128, 4, 17, 2, 128], BF16)
            kb.memset(PBpad, 0.0)
            kb.memset(CPpad, 0.0)
            J1 = ph.sb("J1", [128, NJ], F32)
            kb.dma("sp", J1[:], I["k_j1"], W=[J1])
            Dv = T_("Dv", [128, 4])
            kb.dma("sp", Dv[:], I["s5_d"][j].rearrange("(q p) -> p q", p=128), W=[SM])
            Cnat = ph.sb("Cnat", [16, 32, 64], F32)
            P_ = []
            for d in range(2):
                p = {}
                are = T_("are%d" % d, [128, 16]); aim = T_("aim%d" % d, [128, 16]); ls = T_("ls%d" % d, [128, 16])
                Br = T_("Br%d" % d, [128, 16, 16]); Bi = T_("Bi%d" % d, [128, 16, 16])
                Cr = T_("Cr%d" % d, [128, 16, 16]); Ci = T_("Ci%d" % d, [128, 16, 16])
                for a in range(2):
                    pr = slice(a * 64, (a + 1) * 64)
                    kb.dma("sp", are[pr, :], I["s5_a_re"][j, d].rearrange("(P a) n -> a n P", a=2)[a], W=[SM])
                    kb.dma("sp", aim[pr, :], I["s5_a_im"][j, d].rearrange("(P a) n -> a n P", a=2)[a], W=[SM])
                    kb.dma("sp", ls[pr, :], I["s5_log_step"][j, d].rearrange("(P a) -> a P", a=2)[a].partition_broadcast(64), W=[SM])
                    kb.dma("sp", Br[pr, :, :], I["s5_b_re"][j, d].rearrange("(P a) n c -> a n P c", a=2)[a], W=[SM])
                    kb.dma("sp", Bi[pr, :, :], I["s5_b_im"][j, d].rearrange("(P a) n c -> a n P c", a=2)[a], W=[SM])
                for (nm, dst) in (("s5_c_re", Cr), ("s5_c_im", Ci)):
                    kb.dma("sp", Cnat[:], I[nm][j, d].rearrange("g c n -> c g n"), W=[Cnat])
                    pb = kb.bank()
                    for P in range(16):
                        kb.op("pe", lambda e, pb=pb, P=P: e.transpose(pb[:, P * 16:(P + 1) * 16],
                              Cnat[:, 2 * P:2 * P + 2, :].rearrange("c a n -> c (a n)"), self.ident[0:16, 0:16]),
                              R=[Cnat, self.ident], W=[pb])
                    kb.cp(dst[:].rearrange("p a c -> p (a c)"), pb[:, 0:256], R=[pb], W=[SM])
                lr = T_("lr%d" % d, [128, 16]); dt = T_("dt%d" % d, [128, 16]); tq = T_("tq%d" % d, [128, 16])
                mag = T_("mag%d" % d, [128, 16]); mg16 = T_("mg16%d" % d, [128, 16]); trn = T_("trn%d" % d, [128, 16])
                tmp = T_("tmp%d" % d, [128, 16]); fx = T_("fx%d" % d, [128, 16]); sv = T_("sv%d" % d, [128, 16]); cv = T_("cv%d" % d, [128, 16])
                lbr = T_("lbr%d" % d, [128, 16]); lbi = T_("lbi%d" % d, [128, 16]); zr = T_("zr%d" % d, [128, 16])
                den = T_("den%d" % d, [128, 16]); fr = T_("fr%d" % d, [128, 16]); fi = T_("fi%d" % d, [128, 16]); t16 = T_("t16%d" % d, [128, 16])
                u1 = T_("u1%d" % d, [128, 16]); u2 = T_("u2%d" % d, [128, 16])
                ts_(lr[:], are[:], -1e-4, None, ALU.min)
                act_(dt[:], ls[:], AF.Exp)
                tt_(tq[:], lr[:], dt[:], ALU.mult)
                act_(mag[:], tq[:], AF.Exp)
                act_(mg16[:], tq[:], AF.Exp, scale=16.0)
                tt_(trn[:], aim[:], dt[:], ALU.mult)
                ts_(trn[:], trn[:], 1.0 / TWO_PI, None, ALU.mult)
                frac_(fx[:], trn[:], tmp[:])
                act_(sv[:], fx[:], AF.Sin, scale=TWO_PI)
                ts_(u1[:], trn[:], 0.25, None, ALU.add)
                frac_(fx[:], u1[:], tmp[:])
                act_(cv[:], fx[:], AF.Sin, scale=TWO_PI)
                tt_(lbr[:], mag[:], cv[:], ALU.mult)
                tt_(lbi[:], mag[:], sv[:], ALU.mult)
                ts_(zr[:], lbr[:], -1.0, None, ALU.add)
                tt_(den[:], lr[:], lr[:], ALU.mult)
                tt_(u1[:], aim[:], aim[:], ALU.mult)
                tt_(den[:], den[:], u1[:], ALU.add)
                kb.recip(den[:], den[:], R=[SM], W=[SM])
                tt_(u1[:], zr[:], lr[:], ALU.mult)
                tt_(u2[:], lbi[:], aim[:], ALU.mult)
                tt_(u1[:], u1[:], u2[:], ALU.add)
                tt_(fr[:], u1[:], den[:], ALU.mult)
                tt_(u1[:], lbi[:], lr[:], ALU.mult)
                tt_(u2[:], zr[:], aim[:], ALU.mult)
                tt_(u1[:], u1[:], u2[:], ALU.subtract)
                tt_(fi[:], u1[:], den[:], ALU.mult)
                ts_(u1[:], trn[:], 16.0, None, ALU.mult)
                frac_(t16[:], u1[:], tmp[:])
                bbr = T_("bbr%d" % d, [128, 16, 16]); bbi = T_("bbi%d" % d, [128, 16, 16])
                w1 = T_("w1%d" % d, [128, 16, 16]); w2 = T_("w2%d" % d, [128, 16, 16])
                frb = fr[:].unsqueeze(2).to_broadcast([128, 16, 16])
                fib = fi[:].unsqueeze(2).to_broadcast([128, 16, 16])
                tt_(w1[:], Br[:], frb, ALU.mult)
                tt_(w2[:], Bi[:], fib, ALU.mult)
                tt_(bbr[:], w1[:], w2[:], ALU.subtract)
                tt_(w1[:], Bi[:], frb, ALU.mult)
                tt_(w2[:], Br[:], fib, ALU.mult)
                tt_(bbi[:], w1[:], w2[:], ALU.add)
                pwr = T_("pwr%d" % d, [128, 17, 16]); pwi = T_("pwi%d" % d, [128, 17, 16])
                kb.memset(TT(pwr.t[:, 0, :], smr), 1.0, eng="dve") if False else kb.op("dve", lambda e: e.memset(pwr[:, 0, :], 1.0), W=[SM])
                kb.op("dve", lambda e: e.memset(pwi[:, 0, :], 0.0), W=[SM])
                for tau in range(16):
                    tt_(u1[:], pwr[:, tau, :], lbr[:], ALU.mult)
                    tt_(u2[:], pwi[:, tau, :], lbi[:], ALU.mult)
                    tt_(pwr[:, tau + 1, :], u1[:], u2[:], ALU.subtract)
                    tt_(u1[:], pwr[:, tau, :], lbi[:], ALU.mult)
                    tt_(u2[:], pwi[:, tau, :], lbr[:], ALU.mult)
                    tt_(pwi[:, tau + 1, :], u1[:], u2[:], ALU.add)
                p.update(bbr=bbr, bbi=bbi, Cr=Cr, Ci=Ci, pwr=pwr, pwi=pwi, mg16=mg16, t16=t16)
                P_.append(p)
            Y = ph.sb("Y", [128, TA], F32)
            up = [ph.sb("up%d" % d, [128, 16, NJ], BF16) for d in range(2)]
            Kt = ph.sb("Kt", [128, 16, 128], BF16)
            WGT = ph.sb("WGT", [128, 16, 2, 128], BF16)
            Hr = ph.sb("Hr", [128, 4, NJ], BF16)
            Hi = ph.sb("Hi", [128, 4, NJ], BF16)
            Ec = ph.sb("Ec", [128, NJ], F32)
            Es = ph.sb("Es", [128, NJ], F32)
            tA, tB, tC, tD, tE, tF = [ph.sb("s5t%d" % i, [128, NJ], F32) for i in range(6)]
            x1 = T_("x1t", [128, 17, 16]); x2 = T_("x2t", [128, 17, 16])
            Yv = Y.t[:].rearrange("p (j t) -> p j t", t=16)
            Ycv = Y.t[:, 0:TC].rearrange("p (j t) -> p j t", t=16)
            Yxv = Y.t[:, TC:TA].rearrange("p (j t) -> p j t", t=16)
            for q in range(4):
                kb.dma("sp", Y[:], self.ZT[q * 128:(q + 1) * 128, :], R=[self.ZT], W=[Y])
                upf0 = up[0].t[:].rearrange("p s j -> p (s j)")
                upv0 = up[0].t[:].rearrange("p s j -> p j s")
                upv1 = up[1].t[:].rearrange("p s j -> p j s")
                kb.cp(upv0, Yv, R=[Y], W=[up[0]], eng="act")
                kb.cp(upv1[:, 0:16, :], Ycv[:, ::-1, ::-1], R=[Y], W=[up[1]])
                kb.cp(upv1[:, 16:NJ, :], Yxv[:, ::-1, ::-1], R=[Y], W=[up[1]])
                kb.ts(Y[:], Y[:], Dv[:, q:q + 1], None, ALU.mult, None, R=[Y, SM], W=[Y])
                for d in range(2):
                    p = P_[d]
                    for pi in range(4):
                        P = q * 4 + pi
                        oA = 32 * pi
                        for (src_r, src_i, pad, nt, t0_, negim) in ((p["bbr"], p["bbi"], PBpad, 16, 0, False), (p["Cr"], p["Ci"], CPpad, 17, 0, True)):
                            pr_b = p["pwr"][:, 0:nt, P].unsqueeze(2).to_broadcast([128, nt, 16])
                            pi_b = p["pwi"][:, 0:nt, P].unsqueeze(2).to_broadcast([128, nt, 16])
                            sr_b = src_r[:, P, :].unsqueeze(1).to_broadcast([128, nt, 16])
                            si_b = src_i[:, P, :].unsqueeze(1).to_broadcast([128, nt, 16])
                            tt_(x1[:, 0:nt, :], pr_b, sr_b, ALU.mult)
                            tt_(x2[:, 0:nt, :], pi_b, si_b, ALU.mult)
                            for a in range(2):
                                prt = slice(a * 64, (a + 1) * 64)
                                kb.tt(pad[prt, pi, :, 0, oA + 16 * a:oA + 16 * a + 16], x1[prt, 0:nt, :], x2[prt, 0:nt, :], ALU.subtract, R=[SM], W=[pad])
                            tt_(x1[:, 0:nt, :], pr_b, si_b, ALU.mult)
                            tt_(x2[:, 0:nt, :], pi_b, sr_b, ALU.mult)
                            for a in range(2):
                                prt = slice(a * 64, (a + 1) * 64)
                                if negim:
                                    kb.stt(pad[prt, pi, :, 1, oA + 16 * a:oA + 16 * a + 16], x1[prt, 0:nt, :], -1.0, x2[prt, 0:nt, :], ALU.mult, ALU.subtract, R=[SM], W=[pad])
                                else:
                                    kb.tt(pad[prt, pi, :, 1, oA + 16 * a:oA + 16 * a + 16], x1[prt, 0:nt, :], x2[prt, 0:nt, :], ALU.add, R=[SM], W=[pad])
                    for tau in range(16):
                        pb = kb.bank()
                        for pi in range(4):
                            kb.mm(pb[:, 0:128], PBpad[:, pi, tau, 0, :], CPpad[:, pi, 0, 0, :], pi == 0, False, R=[PBpad, CPpad], W=[pb])
                            kb.mm(pb[:, 0:128], PBpad[:, pi, tau, 1, :], CPpad[:, pi, 0, 1, :], False, pi == 3, R=[PBpad, CPpad], W=[pb])
                        kb.cp(Kt[:, tau, :], pb[:, 0:128], R=[pb], W=[Kt], eng=("act" if tau % 2 else "dve"))
                    for pi in range(4):
                        P = q * 4 + pi
                        for tb in range(4):
                            pb = kb.bank()
                            pbv = pb.t[:].bitcast(BF16)
                            for k8 in range(8):
                                tau = tb * 4 + k8 // 2
                                ri = k8 % 2
                                kb.op("pe", lambda e, pbv=pbv, k8=k8, tau=tau, ri=ri, pi=pi: e.transpose(pbv[:, k8 * 128:(k8 + 1) * 128], PBpad[:, pi, tau, ri, :], self.identb[:]),
                                      R=[PBpad, self.identb], W=[pb])
                            kb.cp(WGT[:, tb * 4:(tb + 1) * 4, :, :].rearrange("p a b c -> p (a b c)"), pbv[:, 0:1024], R=[pb], W=[WGT], eng=("act" if tb % 2 else "dve"))
                        kb.ts(tA[:], J1[:], p["t16"][:, P:P + 1], None, ALU.mult, None, R=[J1, SM], W=[tA])
                        kb.ts(tB[:], tA[:], MAGIC, None, ALU.add, None, R=[tA], W=[tB])
                        kb.ts(tB[:], tB[:], -MAGIC, None, ALU.add, None, R=[tB], W=[tB])
                        kb.tt(tB[:], tA[:], tB[:], ALU.subtract, R=[tA, tB], W=[tB])
                        kb.act(Es[:], tB[:], AF.Sin, R=[tB], W=[Es], scale=TWO_PI)
                        kb.ts(tA[:], tA[:], 0.25, None, ALU.add, None, R=[tA], W=[tA])
                        kb.ts(tB[:], tA[:], MAGIC, None, ALU.add, None, R=[tA], W=[tB])
                        kb.ts(tB[:], tB[:], -MAGIC, None, ALU.add, None, R=[tB], W=[tB])
                        kb.tt(tB[:], tA[:], tB[:], ALU.subtract, R=[tA, tB], W=[tB])
                        kb.act(Ec[:], tB[:], AF.Sin, R=[tB], W=[Ec], scale=TWO_PI)
                        G = [[None, None], [None, None]]
                        for ri in range(2):
                            for hh in range(2):
                                pb = kb.bank()
                                for s in range(16):
                                    kb.mm(pb[:, 0:HJ], WGT[:, 15 - s, ri, :], up[d][:, s, hh * HJ:(hh + 1) * HJ], s == 0, s == 15, R=[WGT, up[d]], W=[pb])
                                G[ri][hh] = pb
                        for hh in range(2):
                            cs = slice(hh * HJ, (hh + 1) * HJ)
                            Gr = G[0][hh]; Gi = G[1][hh]
                            kb.tt(tA[:, cs], Gr[:, 0:HJ], Ec[:, cs], ALU.mult, R=[Gr, Ec], W=[tA])
                            kb.tt(tB[:, cs], Gi[:, 0:HJ], Es[:, cs], ALU.mult, R=[Gi, Es], W=[tB])
                            kb.tt(tC[:, cs], tA[:, cs], tB[:, cs], ALU.add, R=[tA, tB], W=[tC])
                            kb.tt(tA[:, cs], Gi[:, 0:HJ], Ec[:, cs], ALU.mult, R=[Gi, Ec], W=[tA])
                            kb.tt(tB[:, cs], Gr[:, 0:HJ], Es[:, cs], ALU.mult, R=[Gr, Es], W=[tB])
                            kb.tt(tD[:, cs], tA[:, cs], tB[:, cs], ALU.subtract, R=[tA, tB], W=[tD])
                        mgb = p["mg16"][:, P:P + 1].to_broadcast([128, NJ])
                        kb.op("dve", lambda e, mgb=mgb: e.tensor_tensor_scan(out=tE[:], data0=mgb, data1=tC[:], initial=0.0, op0=ALU.mult, op1=ALU.add), R=[tC, SM], W=[tE])
                        kb.op("dve", lambda e, mgb=mgb: e.tensor_tensor_scan(out=tF[:], data0=mgb, data1=tD[:], initial=0.0, op0=ALU.mult, op1=ALU.add), R=[tD, SM], W=[tF])
                        kb.tt(tA[:], tE[:], Ec[:], ALU.mult, R=[tE, Ec], W=[tA])
                        kb.tt(tB[:], tF[:], Es[:], ALU.mult, R=[tF, Es], W=[tB])
                        kb.tt(Hr[:, pi, :], tA[:], tB[:], ALU.subtract, R=[tA, tB], W=[Hr])
                        kb.tt(tA[:], tF[:], Ec[:], ALU.mult, R=[tF, Ec], W=[tA])
                        kb.tt(tB[:], tE[:], Es[:], ALU.mult, R=[tE, Es], W=[tB])
                        kb.tt(Hi[:, pi, :], tA[:], tB[:], ALU.add, R=[tA, tB], W=[Hi])
                    for t in range(16):
                        for hh in range(2):
                            c0 = hh * HJ
                            pb = kb.bank()
                            for s in range(t + 1):
                                kb.mm(pb[:, 0:HJ], Kt[:, t - s, :], up[d][:, s, c0:c0 + HJ], s == 0, False, R=[Kt, up[d]], W=[pb])
                            lo = 1 if hh == 0 else 0
                            for pi in range(4):
                                kb.mm(pb[:, lo:HJ], CPpad[:, pi, t + 1, 0, :], Hr[:, pi, c0 + lo - 1:c0 + HJ - 1], False, False, R=[CPpad, Hr], W=[pb])
                                kb.mm(pb[:, lo:HJ], CPpad[:, pi, t + 1, 1, :], Hi[:, pi, c0 + lo - 1:c0 + HJ - 1], False, pi == 3, R=[CPpad, Hi], W=[pb])
                            if d == 0:
                                kb.tt(Yv[:, c0:c0 + HJ, t], pb[:, 0:HJ], Yv[:, c0:c0 + HJ, t], ALU.add, R=[pb, Y], W=[Y])
                            else:
                                tm = 15 - t
                                if hh == 0:
                                    kb.tt(Ycv[:, :, tm][:, ::-1], pb[:, 0:16], Ycv[:, :, tm][:, ::-1], ALU.add, R=[pb, Y], W=[Y])
                                    kb.tt(Yxv[:, 264:512, tm][:, ::-1], pb[:, 16:HJ], Yxv[:, 264:512, tm][:, ::-1], ALU.add, R=[pb, Y], W=[Y])
                                else:
                                    kb.tt(Yxv[:, 0:264, tm][:, ::-1], pb[:, 0:HJ], Yxv[:, 0:264, tm][:, ::-1], ALU.add, R=[pb, Y], W=[Y])
                kb.act(upf0, Y[:], AF.Gelu_apprx_tanh, R=[Y], W=[up[0]])
                kb.dma("sp", self.YG[q * 128:(q + 1) * 128, :], upf0, R=[up[0]], W=[self.YG])

    def s5_glu(self, l):
        kb = self.kb
        I = self.inp
        j = l // 2
        with kb.phase() as ph:
            Wg = ph.sb("Wg", [128, 4, 512], BF16)
            kb.dma("pool", Wg[:], I["s5_w_glu"][j].rearrange("(k p) n -> p k n", p=128), W=[Wg])
            bg = ph.sb("bg", [128, 4], F32)
            kb.dma("sp", bg[:], I["s5_b_glu"][j].rearrange("(q p) -> p q", p=128), W=[bg])
            yg = [ph.sb("yg%d" % i, [128, 4, 512], BF16) for i in range(2)]
            sg = [ph.sb("sg%d" % i, [128, 512], F32) for i in range(2)]
            ob = [ph.sb("gob%d" % i, [128, 512], BF16) for i in range(2)]
            YGv = self.YG.t.rearrange("(k p) t -> p k t", p=128)
            it = 0
            for ci, (t0, n) in enumerate(tok_chunks()):
                y_ = yg[ci % 2]
                kb.dma("sp", y_[:, :, 0:n], YGv[:, :, t0:t0 + n], R=[self.YG], W=[y_])
                for oc in range(4):
                    pb = kb.bank()
                    for k in range(4):
                        kb.mm(pb[:, 0:n], Wg[:, k, oc * 128:(oc + 1) * 128], y_[:, k, 0:n], k == 0, k == 3, R=[Wg, y_], W=[pb])
                    s_ = sg[it % 2]; o_ = ob[it % 2]
                    it += 1
                    kb.act(s_[:, 0:n], pb[:, 0:n], AF.Sigmoid, R=[pb, bg], W=[s_], bias=bg[:, oc:oc + 1])
                    kb.tt(o_[:, 0:n], y_[:, oc, 0:n], s_[:, 0:n], ALU.mult, R=[y_, s_], W=[o_])
                    kb.dma("sp", self.MT[oc * 128:(oc + 1) * 128, t0:t0 + n], o_[:, 0:n], R=[o_], W=[self.MT])

    def hgrn(self, l):
        kb = self.kb
        I = self.inp
        j = l // 2
        NBX = 1024
        blocks_mem = [(0, TC)] + [(TC + NBX * i, NBX) for i in range(T // NBX)]
        orders = [blocks_mem, [blocks_mem[0]] + blocks_mem[:0:-1]]
        with kb.phase() as ph:
            smr = Reg()
            SM = TT(None, smr)

            def T_(name, shape, dt=F32):
                t = ph.sb(name, shape, dt)
                t.g = smr
                return t
            raw = T_("raw", [128, 2, 4])
            for ly in range(2):
                kb.dma("sp", raw[:, ly, :], I["hg_lb_raw"][ly].rearrange("(h p) -> p h", p=128), W=[SM])
            lb = T_("lb", [128, 4]); oml = T_("oml", [128, 4])
            if j == 0:
                kb.op("dve", lambda e: e.memset(lb[:], 0.0), W=[SM])
                kb.op("dve", lambda e: e.memset(oml[:], 1.0), W=[SM])
            else:
                mx = T_("mx", [128, 4]); e0 = T_("e0", [128, 4]); e1 = T_("e1", [128, 4])
                kb.tt(mx[:], raw[:, 0, :], raw[:, 1, :], ALU.max, R=[SM], W=[SM])
                kb.tt(e0[:], raw[:, 0, :], mx[:], ALU.subtract, R=[SM], W=[SM])
                kb.tt(e1[:], raw[:, 1, :], mx[:], ALU.subtract, R=[SM], W=[SM])
                kb.act(e0[:], e0[:], AF.Exp, R=[SM], W=[SM])
                kb.act(e1[:], e1[:], AF.Exp, R=[SM], W=[SM])
                kb.tt(e0[:], e0[:], e1[:], ALU.add, R=[SM], W=[SM])
                kb.recip(e0[:], e0[:], R=[SM], W=[SM])
                kb.tt(lb[:], e1[:], e0[:], ALU.mult, R=[SM], W=[SM])
                kb.ts(oml[:], lb[:], -1.0, 1.0, ALU.mult, ALU.add, R=[SM], W=[SM])
            hgn = T_("hgn", [128, 1])
            kb.dma("sp", hgn[:], I["hg_norm"][j].rearrange("(p o) -> p o", o=1), W=[SM])
            M01 = ph.sb("M01", [128, NBX], F32)
            kb.dma("sp", M01[:], I["k_m01"], W=[M01])
            mk = ph.sb("mk128", [128, 128], F32)
            kb.dma("sp", mk[:], I["k_mask128"], W=[mk])
            O = [ph.sb("O%d" % d, [128, TA], F32) for d in range(2)]
            C_ = []
            for d in range(2):
                c = {}
                for nm in ("A", "B", "C", "Dd", "Ee", "Q", "V32"):
                    c[nm] = ph.sb("h%s%d" % (nm, d), [128, NBX], F32)
                for nm in ("q1b", "qmb", "kmb"):
                    c[nm] = ph.sb("h%s%d" % (nm, d), [128, NBX], BF16)
                c["khT"] = ph.sb("khT%d" % d, [128, NBX // 128, 128], BF16)
                c["vT"] = ph.sb("vT%d" % d, [128, NBX // 128, 128], BF16)
                c["S32"] = ph.sb("S32%d" % d, [128, 128], F32)
                c["As"] = ph.sb("As%d" % d, [128, 128], F32)
                c["Sb"] = ph.sb("Sb%d" % d, [128, 128], BF16)
                c["eb"] = ph.sb("eb%d" % d, [128, NBX // 64], F32)
                c["AT"] = [ph.sb("AT%d%d" % (d, i), [128, 128], BF16) for i in range(2)]
                C_.append(c)
            os_ = ph.sb("os_", [128, 512], F32); sqh = ph.sb("sqh", [128, 512], BF16); rsh = ph.sb("rsh", [128, 512], F32)
            gq = ph.sb("gq", [128, 512], F32); ohb = ph.sb("ohb", [128, 512], BF16)

            def prepass(d, hd, m0, NB):
                c = C_[d]
                rv = (lambda ap: ap[:, ::-1]) if d else (lambda ap: ap)
                nch = NB // 64
                A, B, C, Dd, Ee, Q = (c[nm] for nm in ("A", "B", "C", "Dd", "Ee", "Q"))
                zrow = (1024 if d == 0 else 1536) + hd * 128
                kb.dma("sp", A[:, 0:NB], self.ZT[zrow:zrow + 128, m0:m0 + NB], R=[self.ZT], W=[A])
                kb.dma("sp", Q[:, 0:NB], self.ZT[512 + hd * 128:512 + (hd + 1) * 128, m0:m0 + NB], R=[self.ZT], W=[Q])
                kb.dma("sp", Ee[:, 0:NB], self.ZT[2048 + hd * 128:2048 + (hd + 1) * 128, m0:m0 + NB], R=[self.ZT], W=[Ee])
                kb.cp(c["V32"][:, 0:NB], rv(Ee[:, 0:NB]), R=[Ee], W=[c["V32"]])
                if d:
                    kb.cp(Dd[:, 0:NB], rv(A[:, 0:NB]), R=[A], W=[Dd])
                    kb.act(B[:, 0:NB], Dd[:, 0:NB], AF.Sigmoid, R=[Dd], W=[B])
                else:
                    kb.act(B[:, 0:NB], A[:, 0:NB], AF.Sigmoid, R=[A], W=[B])
                kb.ts(B[:, 0:NB], B[:, 0:NB], oml[:, hd:hd + 1], lb[:, hd:hd + 1], ALU.mult, ALU.add, R=[B, SM], W=[B])
                kb.act(C[:, 0:NB], B[:, 0:NB], AF.Ln, R=[B], W=[C])
                kb.ts(B[:, 0:NB], B[:, 0:NB], -1.0, 1.0, ALU.mult, ALU.add, R=[B], W=[B])
                kb.op("dve", lambda e: e.tensor_tensor_scan(out=A[:, 0:NB], data0=M01[:, 0:NB], data1=C[:, 0:NB], initial=0.0, op0=ALU.mult, op1=ALU.add),
                      R=[M01, C], W=[A])
                Av = A.t[:, 0:NB].rearrange("p (n c) -> p n c", c=64)
                Dv3 = Dd.t[:, 0:NB].rearrange("p (n c) -> p n c", c=64)
                kb.act(c["eb"][:, 0:nch], Av[:, :, 63], AF.Exp, R=[A], W=[c["eb"]])
                if d:
                    kb.cp(C[:, 0:NB], rv(Q[:, 0:NB]), R=[Q], W=[C])
                    kb.act(C[:, 0:NB], C[:, 0:NB], AF.Silu, R=[C], W=[C])
                else:
                    kb.act(C[:, 0:NB], Q[:, 0:NB], AF.Silu, R=[Q], W=[C])
                kb.act(Ee[:, 0:NB], A[:, 0:NB], AF.Exp, R=[A], W=[Ee])
                kb.tt(c["q1b"][:, 0:NB], C[:, 0:NB], Ee[:, 0:NB], ALU.mult, R=[C, Ee], W=[c["q1b"]])
                kb.tt(Dv3, Av, Av[:, :, 31:32].to_broadcast([128, nch, 64]), ALU.subtract, R=[A], W=[Dd])
                kb.ts(Dd[:, 0:NB], Dd[:, 0:NB], -80.0, 80.0, ALU.max, ALU.min, R=[Dd], W=[Dd])
                kb.act(Ee[:, 0:NB], Dd[:, 0:NB], AF.Exp, R=[Dd], W=[Ee])
                kb.tt(c["qmb"][:, 0:NB], C[:, 0:NB], Ee[:, 0:NB], ALU.mult, R=[C, Ee], W=[c["qmb"]])
                kb.act(Ee[:, 0:NB], Dd[:, 0:NB], AF.Exp, R=[Dd], W=[Ee], scale=-1.0)
                kb.tt(c["kmb"][:, 0:NB], B[:, 0:NB], Ee[:, 0:NB], ALU.mult, R=[B, Ee], W=[c["kmb"]])
                kb.tt(Dv3, Av[:, :, 63:64].to_broadcast([128, nch, 64]), Av, ALU.subtract, R=[A], W=[Dd])
                kb.act(Ee[:, 0:NB], Dd[:, 0:NB], AF.Exp, R=[Dd], W=[Ee])
                kb.tt(Dd[:, 0:NB], B[:, 0:NB], Ee[:, 0:NB], ALU.mult, R=[B, Ee, Dd], W=[Dd])
                for dc in range(NB // 128):
                    if "hgT" in self.skip:
                        break
                    pb = kb.bank()
                    pb2 = kb.bank()
                    kb.op("pe", lambda e, pb=pb, dc=dc: e.transpose(pb[:, 0:128], Dd[:, dc * 128:(dc + 1) * 128], self.ident[:]), R=[Dd, self.ident], W=[pb])
                    kb.op("pe", lambda e, pb2=pb2, dc=dc: e.transpose(pb2[:, 0:128], c["V32"][:, dc * 128:(dc + 1) * 128], self.ident[:]), R=[c["V32"], self.ident], W=[pb2])
                    kb.cp(c["khT"][:, dc, :], pb[:, 0:128], R=[pb], W=[c["khT"]], eng="act")
                    kb.cp(c["vT"][:, dc, :], pb2[:, 0:128], R=[pb2], W=[c["vT"]], eng="dve")

            st = {}

            def stepA(d, dc):
                c = C_[d]
                cols = slice(dc * 128, (dc + 1) * 128)
                pA = kb.bank()
                kb.mm(pA[:, 0:128], c["kmb"][:, cols], c["qmb"][:, cols], True, True, R=[c["kmb"], c["qmb"]], W=[pA])
                AT = c["AT"][dc % 2]
                As = c["As"]
                kb.ts(As[:], pA[:, 0:128], -1e30, 1e30, ALU.max, ALU.min, R=[pA], W=[As])
                kb.tt(AT[:], As[:], mk[:], ALU.mult, R=[As, mk], W=[AT])
                pO = kb.bank()
                kb.mm(pO[:, 0:128], c["vT"][:, dc, :], AT[:], True, False, R=[c["vT"], AT], W=[pO])
                st[d] = pO
                half(d, dc, 0, pO)

            def half(d, dc, hf, pO):
                c = C_[d]
                ch = 2 * dc + hf
                c64 = slice(dc * 128 + hf * 64, dc * 128 + hf * 64 + 64)
                prt = slice(hf * 64, hf * 64 + 64)
                kb.mm(pO[:, hf * 64:(hf + 1) * 64], c["Sb"][:], c["q1b"][:, c64], False, hf == 1, R=[c["Sb"], c["q1b"]], W=[pO])
                pU = kb.bank()
                kb.mm(pU[:, 0:128], c["khT"][prt, dc, :], c["vT"][prt, dc, :], True, True, R=[c["khT"], c["vT"]], W=[pU])
                kb.stt(c["Sb"][:], c["S32"][:], c["eb"][:, ch:ch + 1], pU[:, 0:128], ALU.mult, ALU.add, R=[c["S32"], c["eb"], pU], W=[c["Sb"]])
                kb.stt(c["S32"][:], c["S32"][:], c["eb"][:, ch:ch + 1], pU[:, 0:128], ALU.mult, ALU.add, R=[c["S32"], c["eb"], pU], W=[c["S32"]])

            def stepB(d, dc, m0, NB, single):
                pO = st[d]
                if not single:
                    half(d, dc, 1, pO)
                if d == 0:
                    kb.cp(O[0][:, m0 + dc * 128:m0 + (dc + 1) * 128], pO[:, 0:128], R=[pO], W=[O[0]], eng="act")
                else:
                    a = m0 + NB - 128 * (dc + 1)
                    kb.cp(O[1][:, a:a + 128][:, ::-1], pO[:, 0:128], R=[pO], W=[O[1]], eng="dve")

            for hd in range(4):
                for d in range(2):
                    kb.memset(C_[d]["S32"], 0.0)
                    kb.memset(C_[d]["Sb"], 0.0)
                for bi in range(len(blocks_mem)):
                    NB = orders[0][bi][1]
                    for d in range(2):
                        prepass(d, hd, orders[d][bi][0], NB)
                    for dc in range(NB // 128):
                        if "hgA" in self.skip:
                            break
                        for d in range(2):
                            stepA(d, dc)
                        for d in range(2):
                            stepB(d, dc, orders[d][bi][0], NB, False)
                for (t0, n) in tok_chunks():
                    if "hgR" in self.skip:
                        break
                    kb.tt(os_[:, 0:n], O[0][:, t0:t0 + n], O[1][:, t0:t0 + n], ALU.add, R=[O[0], O[1]], W=[os_])
                    kb.act(sqh[:, 0:n], os_[:, 0:n], AF.Square, R=[os_], W=[sqh])
                    pb = kb.bank()
                    kb.mm(pb[:, 0:n], self.onesb[:], sqh[:, 0:n], True, True, R=[self.onesb, sqh], W=[pb])
                    kb.act(rsh[:, 0:n], pb[:, 0:n], AF.Sqrt, R=[pb], W=[rsh], bias=self.epsb[:, 0:1], scale=1.0 / 128.0)
                    kb.recip(rsh[:, 0:n], rsh[:, 0:n], R=[rsh], W=[rsh])
                    kb.dma("sp", gq[:, 0:n], self.ZT[2560 + hd * 128:2560 + (hd + 1) * 128, t0:t0 + n], R=[self.ZT], W=[gq])
                    kb.act(gq[:, 0:n], gq[:, 0:n], AF.Silu, R=[gq], W=[gq])
                    kb.stt(os_[:, 0:n], os_[:, 0:n], hgn[:, 0:1], rsh[:, 0:n], ALU.mult, ALU.mult, R=[os_, SM, rsh], W=[os_])
                    kb.tt(ohb[:, 0:n], os_[:, 0:n], gq[:, 0:n], ALU.mult, R=[os_, gq], W=[ohb])
                    kb.dma("sp", self.MT[512 + hd * 128:512 + (hd + 1) * 128, t0:t0 + n], ohb[:, 0:n], R=[ohb], W=[self.MT])

    def dump_xt(self):
        kb = self.kb
        with kb.phase() as ph:
            for k in range(8):
                if "nodbgm" in self.skip:
                    break
                kb.dma("sp", self.dbgm[k * 128:(k + 1) * 128, :], self.MT[k * 128:(k + 1) * 128, :], R=[self.MT])
            st = [ph.sb("dm%d" % i, [128, 8, 512], F32) for i in range(2)]
            XTv = self.XT.t.rearrange("(k p) t -> p k t", p=128)
            Dv = self.dbg.rearrange("(k p) t -> p k t", p=128)
            for ci, (t0, n) in enumerate(tok_chunks()):
                s = st[ci % 2]
                kb.dma("sp", s[:, :, 0:n], XTv[:, :, t0:t0 + n], R=[self.XT], W=[s])
                kb.dma("sp", Dv[:, :, t0:t0 + n], s[:, :, 0:n], R=[s])

    def final(self):
        kb = self.kb
        with kb.phase() as ph:
            xs = [ph.sb("fx%d" % i, [128, 8, 512], F32) for i in range(2)]
            sq = ph.sb("fsq", [128, 8, 512], BF16)
            rs = ph.sb("frs", [128, 512], F32)
            yf = [ph.sb("fy%d" % i, [128, 8, 512], F32) for i in range(2)]
            yo = [ph.sb("fo%d" % i, [128, D], F32) for i in range(3)]
            XTv = self.XT.t.rearrange("(k p) t -> p k t", p=128)
            oi = 0
            for ci, (t0, n) in enumerate(tok_chunks()[1:]):
                xin = xs[ci % 2]
                y_ = yf[ci % 2]
                kb.dma("sp", xin[:], XTv[:, :, t0:t0 + n], R=[self.XT], W=[xin])
                kb.act(sq[:], xin[:], AF.Square, R=[xin], W=[sq])
                pb = kb.bank()
                for k in range(8):
                    kb.mm(pb[:], self.onesb[:], sq[:, k, :], k == 0, k == 7, R=[sq, self.onesb], W=[pb])
                kb.act(rs[:], pb[:], AF.Sqrt, R=[pb], W=[rs], bias=self.epsb[:, 0:1], scale=1.0 / D)
                kb.recip(rs[:], rs[:], R=[rs], W=[rs])
                for k in range(8):
                    kb.stt(y_[:, k, :], xin[:, k, :], self.fn[:, k:k + 1], rs[:], ALU.mult, ALU.mult, R=[xin, rs, self.fn], W=[y_])
                for tl in range(4):
                    o_ = yo[oi % 3]
                    oi += 1
                    for h in range(2):
                        pb = kb.bank()
                        for q in range(4):
                            k = h * 4 + q
                            kb.op("pe", lambda e, pb=pb, q=q, k=k, y_=y_, tl=tl: e.transpose(pb[:, q * 128:(q + 1) * 128], y_[:, k, tl * 128:(tl + 1) * 128], self.ident[:]),
                                  R=[y_, self.ident], W=[pb])
                        kb.cp(o_[:, h * 512:(h + 1) * 512], pb[:], R=[pb], W=[o_], eng=("act" if h else "dve"))
                    r0 = t0 - TC + tl * 128
                    kb.dma("sp", self.y[r0:r0 + 128, :], o_[:], R=[o_])


def host_consts():
    c = {}
    c["k_ident"] = np.eye(128, dtype=np.float32)
    pm = np.zeros((128, 128), np.float32)
    for m in range(128):
        pm[m ^ 16, m] = 1.0
    c["k_perm"] = pm
    rows = T // 64
    t = np.arange(T)
    row = (t // 64).astype(np.float32)
    col = (t % 64).astype(np.float32)
    inv = (10000.0 ** (-np.arange(16, dtype=np.float32) / 16.0)).astype(np.float32)
    C = np.zeros((128, T), np.float32)
    S = np.zeros((128, T), np.float32)
    for m in range(128):
        i = m % 16
        axis = (m % 64) // 32
        half = (m % 32) // 16
        pos = row if axis == 0 else col
        ang = (pos * inv[i]).astype(np.float32)
        C[m] = np.cos(ang)
        S[m] = np.sin(ang) * (-1.0 if half == 0 else 1.0)
    c["k_ropeC"] = C
    c["k_ropeS"] = S
    kl = np.arange(128)[:, None]
    ql = np.arange(128)[None, :]
    mp = np.where(kl >= ql, 0.0, -30000.0).astype(np.float32)
    mn = np.where(kl <= ql, 0.0, -30000.0).astype(np.float32)
    c["k_mprev"] = np.tile(mp, (1, 4))
    c["k_mnext"] = np.tile(mn, (1, 4))
    sel = np.zeros((8, 8, 128), np.float32)
    for e in range(8):
        sel[e, e, :] = 1.0
    c["k_sel"] = sel.reshape(8, 8 * 128)
    c["k_j1"] = np.tile(np.arange(1, TA // 16 + 1, dtype=np.float32)[None, :], (128, 1))
    m01 = np.ones((128, 1024), np.float32)
    m01[:, ::64] = 0.0
    c["k_m01"] = m01
    s_ = np.arange(128)[:, None]
    t_ = np.arange(128)[None, :]
    c["k_mask128"] = (((s_ // 64) == (t_ // 64)) & (s_ <= t_)).astype(np.float32)
    return c


_CACHE = {}


def run(inputs, depth_run=DEPTH, debug=False, layers=None, build_only=False, skip=(), ncores=8):
    nc = bass.Bass("TRN2", target_bir_lowering=False)
    prog = Prog(nc, depth_run=depth_run, debug=debug, layers=layers, skip=skip)
    prog.build()
    if build_only:
        return prog
    consts = host_consts()
    shared = {k: np.ascontiguousarray(v) for k, v in inputs.items() if k not in ("x", "c", "ctx")}
    shared.update(consts)
    in_maps = []
    for core in range(ncores):
        b = core % 4
        m = dict(shared)
        m["x"] = np.ascontiguousarray(inputs["x"][b])
        m["c"] = np.ascontiguousarray(inputs["c"][b])
        m["ctx"] = np.ascontiguousarray(inputs["ctx"][b])
        in_maps.append(m)
    res = run_bass_kernel_spmd(nc, in_maps, core_ids=list(range(ncores)))
    return res


def kernel(**inputs):
    inputs = {k: np.asarray(v) for k, v in inputs.items()}
    res = run(inputs)
    out = np.stack([np.asarray(res.results[b]["y"]) for b in range(4)], axis=0)
    return out.astype(np.float32)
```

```python
import contextlib
import math
import numpy as np
import concourse.bass as bass
import concourse.mybir as mybir
from concourse.bass_utils import run_bass_kernel_spmd

F32 = mybir.dt.float32
BF16 = mybir.dt.bfloat16
AF = mybir.ActivationFunctionType
ALU = mybir.AluOpType

D = 1024
T = 8192
TC = 256
TA = T + TC
DEPTH = 4
EPS = 1e-6
NDS = 24
SAME_SYNC = True
FFD = 2816
EXD = 1408
NEXP = 8


class Reg:
    __slots__ = ("w", "r")

    def __init__(self):
        self.w = None
        self.r = {}


class TT:
    def __init__(self, t, reg=None):
        self.t = t
        self.g = reg if reg is not None else Reg()

    def __getitem__(self, k):
        return self.t[k]


class KB:
    def __init__(self, nc):
        self.nc = nc
        self.E = {"pe": nc.tensor, "act": nc.scalar, "dve": nc.vector, "pool": nc.gpsimd, "sp": nc.sync}
        self.es = contextlib.ExitStack()
        self.sem = {e: self.es.enter_context(nc.semaphore("s_" + e)) for e in self.E}
        self.cnt = {e: 0 for e in self.E}
        self.known = {e: {} for e in self.E}
        self.dsem = [self.es.enter_context(nc.semaphore("d%d" % i)) for i in range(NDS)]
        self.dcnt = [0] * NDS
        self.dnext = 0
        self.ps = [TT(self.es.enter_context(nc.psum_tensor("ps%d" % i, [128, 512], F32))) for i in range(8)]
        self.psn = 0
        self.uid = 0
        self.ninst = 0

    def _need(self, eng, tok):
        if tok is None:
            return
        kind, key, val = tok
        if kind == "e" and key == eng and (eng == "pe" or not SAME_SYNC):
            return
        kk = (kind, key)
        if self.known[eng].get(kk, 0) >= val:
            return
        sem = self.sem[key] if kind == "e" else self.dsem[key]
        self.E[eng].wait_ge(sem, val)
        self.known[eng][kk] = val

    def _deps(self, eng, R, W):
        for r in R:
            self._need(eng, r.g.w)
        for w in W:
            self._need(eng, w.g.w)
            for t in list(w.g.r.values()):
                self._need(eng, t)

    def _commit(self, tok, R, W):
        for r in R:
            r.g.r[(tok[0], tok[1])] = tok
        for w in W:
            w.g.w = tok
            w.g.r = {}

    def op(self, eng, fn, R=(), W=()):
        self._deps(eng, R, W)
        inst = fn(self.E[eng])
        self.cnt[eng] += 1
        inst.then_inc(self.sem[eng], 1)
        tok = ("e", eng, self.cnt[eng])
        self._commit(tok, R, W)
        self.ninst += 1
        return tok

    def dma(self, q, out, in_, R=(), W=(), **kw):
        i = self.dnext
        self.dnext = (self.dnext + 1) % NDS
        if self.dcnt[i] > 0:
            self._need(q, ("d", i, 16 * self.dcnt[i]))
        self._deps(q, R, W)
        inst = self.E[q].dma_start(out=out, in_=in_, **kw)
        inst.then_inc(self.dsem[i], 16)
        self.dcnt[i] += 1
        tok = ("d", i, 16 * self.dcnt[i])
        self._commit(tok, R, W)
        self.ninst += 1
        return tok

    def barrier(self):
        for e in self.E:
            for o in self.E:
                if o != e and self.cnt[o] > 0:
                    self._need(e, ("e", o, self.cnt[o]))
            for i in range(NDS):
                if self.dcnt[i] > 0:
                    self._need(e, ("d", i, 16 * self.dcnt[i]))

    @contextlib.contextmanager
    def phase(self):
        ph = Phase(self)
        with ph.es:
            yield ph
            self.barrier()

    def bank(self):
        p = self.ps[self.psn]
        self.psn = (self.psn + 1) % 8
        return p

    def dram(self, name, shape, dt):
        return TT(self.nc.dram_tensor(name, shape, dt, kind="Internal").ap())

    def mm(self, out, lhsT, rhs, start, stop, R, W):
        return self.op("pe", lambda e: e.matmul(out, lhsT=lhsT, rhs=rhs, start=start, stop=stop), R=R, W=W)

    def act(self, out, in_, func, R, W, bias=None, scale=None):
        kw = {}
        if bias is not None:
            kw["bias"] = bias
        if scale is not None:
            kw["scale"] = scale
        return self.op("act", lambda e: e.activation(out=out, in_=in_, func=func, **kw), R=R, W=W)

    def ts(self, out, in0, s1, s2, op0, op1, R, W, eng="dve"):
        if op1 is None:
            return self.op(eng, lambda e: e.tensor_scalar(out=out, in0=in0, scalar1=s1, scalar2=None, op0=op0), R=R, W=W)
        return self.op(eng, lambda e: e.tensor_scalar(out=out, in0=in0, scalar1=s1, scalar2=s2, op0=op0, op1=op1), R=R, W=W)

    def stt(self, out, in0, scalar, in1, op0, op1, R, W):
        return self.op("dve", lambda e: e.scalar_tensor_tensor(out=out, in0=in0, scalar=scalar, in1=in1, op0=op0, op1=op1), R=R, W=W)

    def tt(self, out, in0, in1, op, R, W, eng="dve"):
        return self.op(eng, lambda e: e.tensor_tensor(out=out, in0=in0, in1=in1, op=op), R=R, W=W)

    def cp(self, out, in_, R, W, eng="dve"):
        if eng == "act":
            return self.op("act", lambda e: e.copy(out=out, in_=in_), R=R, W=W)
        return self.op(eng, lambda e: e.tensor_copy(out=out, in_=in_), R=R, W=W)

    def recip(self, out, in_, R, W):
        return self.op("dve", lambda e: e.reciprocal(out=out, in_=in_), R=R, W=W)

    def memset(self, t, val, eng="pool"):
        return self.op(eng, lambda e: e.memset(t.t[:], val), W=[t])


class Phase:
    def __init__(self, kb):
        self.kb = kb
        self.es = contextlib.ExitStack()

    def sb(self, name, shape, dt):
        self.kb.uid += 1
        return TT(self.es.enter_context(self.kb.nc.sbuf_tensor("%s_%d" % (name, self.kb.uid), list(shape), dt)))


def tok_chunks():
    return [(0, TC)] + [(TC + 512 * i, 512) for i in range(T // 512)]


class Prog:
    def __init__(self, nc, depth_run=DEPTH, debug=False, layers=None, skip=()):
        self.skip = set(skip)
        self.nc = nc
        self.kb = KB(nc)
        self.depth_run = depth_run
        self.layers = list(range(depth_run)) if layers is None else layers
        self.debug = debug
        self.inp = {}

    def din(self, name, shape, dt=F32):
        a = self.nc.dram_tensor(name, list(shape), dt, kind="ExternalInput").ap()
        self.inp[name] = a
        return a

    def declare(self):
        d = self.din
        d("x", [T, D]); d("c", [D]); d("ctx", [TC, D]); d("c_ctx", [D])
        d("w_mod", [DEPTH, D, 6 * D]); d("b_mod", [DEPTH, 6 * D])
        d("norm_mix", [DEPTH, D]); d("norm_ffn", [DEPTH, D]); d("final_norm", [D])
        d("w_in_ab", [2, D, 1792]); d("lru_conv_w", [2, 4, 512]); d("lru_conv_b", [2, 512])
        d("lru_wa", [2, 2, 8, 64, 64]); d("lru_ba", [2, 2, 512]); d("lru_wx", [2, 2, 8, 64, 64]); d("lru_bx", [2, 2, 512])
        d("lru_lam", [2, 2, 512]); d("attn_sink", [2, 8]); d("w_out_ab", [2, D, D])
        d("ffn_w1", [2, D, FFD]); d("ffn_w3", [2, D, FFD]); d("ffn_w2", [2, FFD, D])
        d("w_in_cd", [2, D, 3072])
        d("s5_a_re", [2, 2, 32, 64]); d("s5_a_im", [2, 2, 32, 64]); d("s5_log_step", [2, 2, 32])
        d("s5_b_re", [2, 2, 32, 64, 16]); d("s5_b_im", [2, 2, 32, 64, 16])
        d("s5_c_re", [2, 2, 32, 16, 64]); d("s5_c_im", [2, 2, 32, 16, 64])
        d("s5_d", [2, 512]); d("s5_w_glu", [2, 512, 512]); d("s5_b_glu", [2, 512])
        d("hg_lb_raw", [2, 512]); d("hg_norm", [2, 128]); d("w_out_cd", [2, D, D])
        d("moe_router", [2, D, 8]); d("moe_w1", [2, 8, D, EXD]); d("moe_w3", [2, 8, D, EXD]); d("moe_w2", [2, 8, EXD, D])
        d("k_ident", [128, 128]); d("k_perm", [128, 128]); d("k_ropeC", [128, T]); d("k_ropeS", [128, T])
        d("k_mprev", [128, 512]); d("k_mnext", [128, 512]); d("k_sel", [8, 8 * 128])
        d("k_j1", [128, TA // 16]); d("k_m01", [128, 1024]); d("k_mask128", [128, 128])
        self.y = self.nc.dram_tensor("y", [T, D], F32, kind="ExternalOutput").ap()
        if self.debug:
            self.dbg = self.nc.dram_tensor("dbg", [D, TA], F32, kind="ExternalOutput").ap()
            self.dbgm = self.nc.dram_tensor("dbgm", [D, TA], BF16, kind="ExternalOutput").ap()
        kb = self.kb
        self.XT = kb.dram("XT", [D, TA], F32)
        self.ZT = kb.dram("ZT", [3072, TA], F32)
        self.MT = kb.dram("MT", [D, TA], BF16)
        self.VT = kb.dram("VT", [TA, 128], BF16)
        self.U = kb.dram("U", [20, 128, 11, 3072], BF16)
        self.YG = kb.dram("YG", [512, TA], BF16)
        self.OD = self.nc.dram_tensor("OD", [2, 512, TA], F32, kind="Internal").ap()
        self.ODr = TT(None)

    def build(self):
        nc = self.nc
        kb = self.kb
        self.declare()
        with contextlib.ExitStack() as gs:
            gs.enter_context(nc.allow_non_contiguous_dma(reason="small strided parameter loads"))
            gs.enter_context(nc.allow_low_precision(reason="bf16 matmul operands, fp32 accumulation"))
            self.G = Phase(kb)
            gs.enter_context(self.G.es)
            self.setup_globals()
            self.phase0_mod()
            self.convert_weights()
            self.ingest()
            for l in self.layers:
                if l % 2 == 0:
                    self.layer_ab(l)
                else:
                    self.layer_cd(l)
            if self.debug:
                self.dump_xt()
            self.final()
            kb.barrier()
        kb.es.close()
        return nc

    def setup_globals(self):
        kb = self.kb
        G = self.G
        I = self.inp
        self.ident = G.sb("ident", [128, 128], F32)
        kb.dma("sp", self.ident[:], I["k_ident"], W=[self.ident])
        self.identb = G.sb("identb", [128, 128], BF16)
        kb.cp(self.identb[:], self.ident[:], R=[self.ident], W=[self.identb])
        self.onesb = G.sb("onesb", [128, 128], BF16)
        kb.memset(self.onesb, 1.0)
        self.MOD = G.sb("MOD", [128, DEPTH, 48, 2], F32)
        self.G1 = G.sb("G1", [128, DEPTH, 8, 2], F32)
        self.G4 = G.sb("G4", [128, DEPTH, 8, 2], F32)
        self.oneb = G.sb("oneb", [128, 1], F32)
        kb.memset(self.oneb, 1.0)
        self.epsb = G.sb("epsb", [128, 1], F32)
        kb.memset(self.epsb, EPS)
        self.fn = G.sb("fn", [128, 8], F32)
        kb.dma("sp", self.fn[:], I["final_norm"].rearrange("(k p) -> p k", p=128), W=[self.fn])

    def phase0_mod(self):
        kb = self.kb
        I = self.inp
        with kb.phase() as ph:
            cs = ph.sb("cs", [128, 8, 2], F32)
            kb.dma("sp", cs[:, :, 0], I["c"].rearrange("(k p) -> p k", p=128), W=[cs])
            kb.dma("sp", cs[:, :, 1], I["c_ctx"].rearrange("(k p) -> p k", p=128), W=[cs])
            sc = ph.sb("sc", [128, 8, 2], F32)
            kb.act(sc[:], cs[:], AF.Silu, R=[cs], W=[sc])
            bm = ph.sb("bm", [128, DEPTH, 48], F32)
            for l_ in range(DEPTH):
                kb.dma("sp", bm[:, l_, :], I["b_mod"][l_].rearrange("(j p) -> p j", p=128), W=[bm])
            nm = ph.sb("nm", [128, DEPTH, 8], F32)
            for l_ in range(DEPTH):
                kb.dma("sp", nm[:, l_, :], I["norm_mix"][l_].rearrange("(k p) -> p k", p=128), W=[nm])
            nf = ph.sb("nf", [128, DEPTH, 8], F32)
            for l_ in range(DEPTH):
                kb.dma("sp", nf[:, l_, :], I["norm_ffn"][l_].rearrange("(k p) -> p k", p=128), W=[nf])
            wm = [ph.sb("wm%d" % i, [128, 8, 1024], F32) for i in range(2)]
            it = 0
            for l in range(DEPTH):
                for m in range(6):
                    w = wm[it % 2]
                    it += 1
                    kb.dma("sp", w[:], I["w_mod"][l].rearrange("(k p) n -> p k n", p=128)[:, :, m * 1024:(m + 1) * 1024], W=[w])
                    for dc in range(8):
                        pb = kb.bank()
                        for k in range(8):
                            kb.mm(pb[:, 0:2], w[:, k, dc * 128:(dc + 1) * 128], sc[:, k, :], k == 0, k == 7, R=[w, sc], W=[pb])
                        j = m * 8 + dc
                        kb.ts(self.MOD[:, l, j, :], pb[:, 0:2], bm[:, l, j:j + 1], None, ALU.add, None, R=[pb, bm], W=[self.MOD])
            for l in range(DEPTH):
                for s in range(2):
                    kb.stt(self.G1[:, l, :, s], self.MOD[:, l, 8:16, s], 1.0, nm[:, l, :], ALU.add, ALU.mult, R=[self.MOD, nm], W=[self.G1])
                    kb.stt(self.G4[:, l, :, s], self.MOD[:, l, 32:40, s], 1.0, nf[:, l, :], ALU.add, ALU.mult, R=[self.MOD, nf], W=[self.G4])

    def convert_weights(self):
        kb = self.kb
        I = self.inp
        need = []
        for l in self.layers:
            j = l // 2
            if l % 2 == 0:
                for pe in range(2):
                    need.append((j * 2 + pe, I["ffn_w1"][j][:, pe * EXD:(pe + 1) * EXD], I["ffn_w3"][j][:, pe * EXD:(pe + 1) * EXD],
                                 I["ffn_w2"][j][pe * EXD:(pe + 1) * EXD, :]))
            else:
                for e in range(NEXP):
                    need.append((4 + j * 8 + e, I["moe_w1"][j, e], I["moe_w3"][j, e], I["moe_w2"][j, e]))
        with kb.phase() as ph:
            sf = [ph.sb("cvf%d" % i, [128, 8, EXD], F32) for i in range(3)]
            sbb = [ph.sb("cvb%d" % i, [128, 11, 1024], BF16) for i in range(2)]
            engs = ["pool", "dve", "act"]
            jobs = []
            for (u, w1, w3, w2) in need:
                jobs.append(("a", u, 0, w1))
                jobs.append(("a", u, 1, w3))
                jobs.append(("b", u, 2, w2))

            def fview(f):
                return f.t[:].rearrange("p k n -> p (k n)")[:, 0:11 * 1024].rearrange("p (f n) -> p f n", n=1024)

            def load(i):
                kind, u, mi, w = jobs[i]
                f = sf[i % 3]
                if kind == "a":
                    kb.dma("sp", f[:], w.rearrange("(k p) n -> p k n", p=128), W=[f])
                else:
                    kb.dma("sp", fview(f), w.rearrange("(f p) n -> p f n", p=128), W=[f])

            def cast_store(i):
                kind, u, mi, w = jobs[i]
                f = sf[i % 3]; b = sbb[i % 2]
                eng = engs[i % 3]
                if kind == "a":
                    for k in range(8):
                        kb.cp(b[:, :, k * 128:(k + 1) * 128], f[:, k, :].rearrange("p (f c) -> p f c", c=128), R=[f], W=[b], eng=eng)
                else:
                    kb.cp(b[:], fview(f), R=[f], W=[b], eng=eng)
                kb.dma("sp", self.U[u, :, :, mi * 1024:(mi + 1) * 1024], b[:], R=[b], W=[self.U])

            if jobs:
                load(0)
            for i in range(len(jobs)):
                if i + 1 < len(jobs):
                    load(i + 1)
                cast_store(i)

    def ingest(self):
        kb = self.kb
        I = self.inp
        with kb.phase() as ph:
            tin = [ph.sb("tin%d" % i, [128, D], F32) for i in range(3)]
            st = [ph.sb("tst%d" % i, [128, 8, 512], F32) for i in range(2)]
            ci = 0
            ti = 0
            for (t0, n) in tok_chunks():
                s = st[ci % 2]
                ci += 1
                for tl in range(n // 128):
                    a = tin[ti % 3]
                    ti += 1
                    tt0 = t0 + tl * 128
                    src = I["ctx"][tt0:tt0 + 128, :] if tt0 < TC else I["x"][tt0 - TC:tt0 - TC + 128, :]
                    kb.dma("sp", a[:], src, W=[a])
                    for h in range(2):
                        pb = kb.bank()
                        for q in range(4):
                            k = h * 4 + q
                            kb.op("pe", lambda e, pb=pb, q=q, k=k, a=a: e.transpose(pb[:, q * 128:(q + 1) * 128], a[:, k * 128:(k + 1) * 128], self.ident[:]),
                                  R=[a, self.ident], W=[pb])
                        kb.cp(s[:, h * 4:(h + 1) * 4, tl * 128:(tl + 1) * 128], pb[:].rearrange("p (q t) -> p q t", t=128), R=[pb], W=[s],
                              eng=("act" if h else "dve"))
                kb.dma("sp", self.XT.t.rearrange("(k p) t -> p k t", p=128)[:, :, t0:t0 + n], s[:, :, 0:n], R=[s], W=[self.XT])

    def normmod(self, ph, xin, n, Gt, M0, sidx, hT, tmp, fp32out=False):
        kb = self.kb
        sq = tmp["sq"]
        xr = getattr(xin, "regs", None) or [xin]
        kb.act(sq[:, :, 0:n], xin[:, :, 0:n], AF.Square, R=xr, W=[sq])
        pb = kb.bank()
        for k in range(8):
            kb.mm(pb[:, 0:n], self.onesb[:], sq[:, k, 0:n], k == 0, k == 7, R=[sq, self.onesb], W=[pb])
        rs = tmp["rs"]
        kb.act(rs[:, 0:n], pb[:, 0:n], AF.Sqrt, R=[pb], W=[rs], bias=self.epsb[:, 0:1], scale=1.0 / D)
        kb.recip(rs[:, 0:n], rs[:, 0:n], R=[rs], W=[rs])
        hf = tmp["hf"]
        for k in range(8):
            kb.stt(hf[:, k, 0:n], xin[:, k, 0:n], Gt(k), rs[:, 0:n], ALU.mult, ALU.mult, R=[xr[k] if len(xr) == 8 else xr[0], rs, self.G1, self.G4], W=[hf])
            if fp32out:
                kb.act(hf[:, k, 0:n], hf[:, k, 0:n], AF.Identity, R=[hf, self.MOD], W=[hf], bias=M0(k))
                kb.cp(hT[:, k, 0:n], hf[:, k, 0:n], R=[hf], W=[hT], eng="pool")
            else:
                kb.act(hT[:, k, 0:n], hf[:, k, 0:n], AF.Identity, R=[hf, self.MOD], W=[hT], bias=M0(k))

    def layer_ab(self, l):
        kb = self.kb
        I = self.inp
        j = l // 2
        need_ctx = l < DEPTH - 1
        with kb.phase() as ph:
            W = ph.sb("Win", [128, 8, 1792], BF16)
            wv = I["w_in_ab"][j].rearrange("(k p) n -> p k n", p=128)
            kb.dma("pool", W[:, :, 0:1024], wv[:, :, 0:1024], W=[W])
            for jj in range(4):
                for hh in range(2):
                    kb.dma("pool", W[:, :, 1024 + jj * 128 + hh * 64:1024 + jj * 128 + hh * 64 + 64],
                           wv[:, :, 1024 + (hh * 4 + jj) * 64:1024 + (hh * 4 + jj) * 64 + 64], W=[W])
            kb.dma("pool", W[:, :, 1536:1792], wv[:, :, 1536:1792], W=[W])
            self.p1_generic(ph, l, W, 13, lambda oc: (oc * 128, oc * 128), vcol=1664)
        self.lru(l)
        self.attn(l)
        self.p3(l, I["w_out_ab"][j], [(j * 2 + pe) for pe in range(2)], moe=None)

    def p1_generic(self, ph, l, W, nch, colrow, vcol=None):
        kb = self.kb
        xs = [ph.sb("xin%d" % i, [128, 8, 512], F32) for i in range(2)]
        hs = [ph.sb("hT%d" % i, [128, 8, 512], BF16) for i in range(2)]
        tmp = {"sq": ph.sb("sq", [128, 8, 512], BF16), "rs": ph.sb("rs", [128, 512], F32), "hf": ph.sb("hf", [128, 8, 512], F32)}
        zs = [ph.sb("zs%d" % i, [128, 512], F32) for i in range(4)]
        vs = [ph.sb("vs%d" % i, [128, 128], BF16) for i in range(2)]
        XTv = self.XT.t.rearrange("(k p) t -> p k t", p=128)
        zi = 0
        vi = 0
        for ci, (t0, n) in enumerate(tok_chunks()):
            s = 1 if t0 < TC else 0
            xin = xs[ci % 2]
            hT = hs[ci % 2]
            kb.dma("sp", xin[:, :, 0:n], XTv[:, :, t0:t0 + n], R=[self.XT], W=[xin])
            self.normmod(ph, xin, n, lambda k: self.G1[:, l, k, s:s + 1], lambda k: self.MOD[:, l, k, s:s + 1], s, hT, tmp)
            for oc in range(nch):
                c0, r0 = colrow(oc)
                pb = kb.bank()
                for k in range(8):
                    kb.mm(pb[:, 0:n], W[:, k, c0:c0 + 128], hT[:, k, 0:n], k == 0, k == 7, R=[W, hT], W=[pb])
                z = zs[zi % 4]
                zi += 1
                kb.cp(z[:, 0:n], pb[:, 0:n], R=[pb], W=[z], eng=("act" if zi % 2 else "dve"))
                kb.dma("sp", self.ZT[r0:r0 + 128, t0:t0 + n], z[:, 0:n], R=[z], W=[self.ZT])
            if vcol is not None:
                for tl in range(n // 128):
                    pb = kb.bank()
                    for k in range(8):
                        kb.mm(pb[:, 0:128], hT[:, k, tl * 128:(tl + 1) * 128], W[:, k, vcol:vcol + 128], k == 0, k == 7, R=[W, hT], W=[pb])
                    v = vs[vi % 2]
                    vi += 1
                    kb.cp(v[:], pb[:, 0:128], R=[pb], W=[v], eng="dve")
                    kb.dma("sp", self.VT[t0 + tl * 128:t0 + (tl + 1) * 128, :], v[:], R=[v], W=[self.VT])

    def lru(self, l):
        kb = self.kb
        I = self.inp
        j = l // 2
        with kb.phase() as ph:
            cw = ph.sb("cw", [128, 4, 4], F32)
            for tp in range(4):
                kb.dma("sp", cw[:, tp, :], I["lru_conv_w"][j, tp].rearrange("(c p) -> p c", p=128), W=[cw])
            cb = ph.sb("cb", [128, 4], F32)
            kb.dma("sp", cb[:], I["lru_conv_b"][j].rearrange("(c p) -> p c", p=128), W=[cb])
            ba = ph.sb("ba", [128, 2, 4], F32)
            for d_ in range(2):
                kb.dma("sp", ba[:, d_, :], I["lru_ba"][j, d_].rearrange("(c p) -> p c", p=128), W=[ba])
            bx = ph.sb("bx", [128, 2, 4], F32)
            for d_ in range(2):
                kb.dma("sp", bx[:, d_, :], I["lru_bx"][j, d_].rearrange("(c p) -> p c", p=128), W=[bx])
            lam = ph.sb("lam", [128, 2, 4], F32)
            for d_ in range(2):
                kb.dma("sp", lam[:, d_, :], I["lru_lam"][j, d_].rearrange("(c p) -> p c", p=128), W=[lam])
            cl = ph.sb("cl", [128, 2, 4], F32)
            kb.act(cl[:], lam[:], AF.Exp, R=[lam], W=[cl], scale=-1.0)
            kb.act(cl[:], cl[:], AF.Ln, R=[cl], W=[cl], bias=self.oneb[:, 0:1])
            kb.ts(cl[:], cl[:], -8.0, None, ALU.mult, None, R=[cl], W=[cl])
            uraw = ph.sb("uraw", [128, TA], F32)
            u = ph.sb("u", [128, TA], F32)
            ub = ph.sb("ub", [128, TA], BF16)
            H = ph.sb("H", [128, TA], F32)
            gg = ph.sb("gg", [128, TA], F32)
            ob = ph.sb("ob", [128, TA], BF16)
            wg = [[ph.sb("wg%d%d" % (d, a), [128, 128], BF16) for a in range(2)] for d in range(2)]
            tmpn = ["r", "i", "a", "m", "b", "hb"]
            tm = {nm: [ph.sb("l%s%d" % (nm, i), [128, 512], F32) for i in range(2)] for nm in tmpn}
            segs = [(0, TC), (TC, T)]
            for c in range(4):
                kb.dma("sp", uraw[:], self.ZT[(4 + c) * 128:(5 + c) * 128, :], R=[self.ZT], W=[uraw])
                kb.dma("sp", gg[:], self.ZT[c * 128:(c + 1) * 128, :], R=[self.ZT], W=[gg])
                for d in range(2):
                    for a, nmw in enumerate(("lru_wa", "lru_wx")):
                        kb.memset(wg[d][a], 0.0)
                        for b2 in range(2):
                            kb.dma("pool", wg[d][a][b2 * 64:(b2 + 1) * 64, b2 * 64:(b2 + 1) * 64], I[nmw][j, d, 2 * c + b2], W=[wg[d][a]])
                for (s0, ln) in segs:
                    kb.ts(u[:, s0:s0 + ln], uraw[:, s0:s0 + ln], cw[:, 2, c:c + 1], cb[:, c:c + 1], ALU.mult, ALU.add, R=[uraw, cw, cb], W=[u])
                    kb.stt(u[:, s0 + 2:s0 + ln], uraw[:, s0:s0 + ln - 2], cw[:, 0, c:c + 1], u[:, s0 + 2:s0 + ln], ALU.mult, ALU.add, R=[uraw, cw, u], W=[u])
                    kb.stt(u[:, s0 + 1:s0 + ln], uraw[:, s0:s0 + ln - 1], cw[:, 1, c:c + 1], u[:, s0 + 1:s0 + ln], ALU.mult, ALU.add, R=[uraw, cw, u], W=[u])
                    kb.stt(u[:, s0:s0 + ln - 1], uraw[:, s0 + 1:s0 + ln], cw[:, 3, c:c + 1], u[:, s0:s0 + ln - 1], ALU.mult, ALU.add, R=[uraw, cw, u], W=[u])
                kb.cp(ub[:], u[:], R=[u], W=[ub], eng="act")
                for d in range(2):
                    chunks = tok_chunks()
                    order = chunks if d == 0 else [chunks[0]] + chunks[:0:-1]
                    carry = None
                    for ci, (t0, n) in enumerate(order):
                        b_ = ci % 2
                        r, i_, a, m, b, hb = (tm[nm][b_] for nm in tmpn)
                        pa = kb.bank()
                        kb.mm(pa[:, 0:n], wg[d][0][:], ub[:, t0:t0 + n], True, True, R=[wg[d][0], ub], W=[pa])
                        px = kb.bank()
                        kb.mm(px[:, 0:n], wg[d][1][:], ub[:, t0:t0 + n], True, True, R=[wg[d][1], ub], W=[px])
                        kb.act(r[:, 0:n], pa[:, 0:n], AF.Sigmoid, R=[pa, ba], W=[r], bias=ba[:, d, c:c + 1])
                        kb.act(i_[:, 0:n], px[:, 0:n], AF.Sigmoid, R=[px, bx], W=[i_], bias=bx[:, d, c:c + 1])
                        kb.act(a[:, 0:n], r[:, 0:n], AF.Exp, R=[r, cl], W=[a], scale=cl[:, d, c:c + 1])
                        kb.act(m[:, 0:n], a[:, 0:n], AF.Square, R=[a], W=[m])
                        kb.act(m[:, 0:n], m[:, 0:n], AF.Sqrt, R=[m], W=[m], bias=self.oneb[:, 0:1], scale=-1.0)
                        kb.tt(b[:, 0:n], i_[:, 0:n], u[:, t0:t0 + n], ALU.mult, R=[i_, u], W=[b])
                        kb.tt(b[:, 0:n], b[:, 0:n], m[:, 0:n], ALU.mult, R=[b, m], W=[b])
                        if d == 0:
                            init = 0.0 if carry is None else H[:, t0 - 1:t0]
                            kb.op("dve", lambda e, n=n, t0=t0, a=a, b=b, init=init: e.tensor_tensor_scan(
                                out=H[:, t0:t0 + n], data0=a[:, 0:n], data1=b[:, 0:n], initial=init, op0=ALU.mult, op1=ALU.add),
                                R=[a, b, H], W=[H])
                            carry = True
                        else:
                            init = 0.0 if carry is None else carry[:, 0:1]
                            rd = [a, b] + ([tm["hb"][1 - b_]] if carry is not None else [])
                            kb.op("dve", lambda e, n=n, a=a, b=b, hb=hb, init=init: e.tensor_tensor_scan(
                                out=hb[:, 0:n][:, ::-1], data0=a[:, 0:n][:, ::-1], data1=b[:, 0:n][:, ::-1], initial=init,
                                op0=ALU.mult, op1=ALU.add), R=rd, W=[hb])
                            kb.tt(H[:, t0:t0 + n], H[:, t0:t0 + n], hb[:, 0:n], ALU.add, R=[H, hb], W=[H])
                            carry = hb
                kb.act(gg[:], gg[:], AF.Gelu_apprx_tanh, R=[gg], W=[gg])
                kb.tt(ob[:], H[:], gg[:], ALU.mult, R=[H, gg], W=[ob])
                kb.dma("sp", self.MT[c * 128:(c + 1) * 128, :], ob[:], R=[ob], W=[self.MT])

    def attn(self, l):
        kb = self.kb
        I = self.inp
        j = l // 2
        need_ctx = l < DEPTH - 1
        NT = TA // 128
        with kb.phase() as ph:
            QT = ph.sb("QT", [128, 4, TA], BF16)
            KT = ph.sb("KT", [128, TA], BF16)
            VK = ph.sb("VK", [128, NT, 128], BF16)
            kb.dma("sp", VK[:], self.VT.t.rearrange("(n p) d -> p n d", p=128), R=[self.VT], W=[VK])
            perm = ph.sb("perm", [128, 128], F32)
            kb.dma("sp", perm[:], I["k_perm"], W=[perm])
            mb = []
            for nm in ("k_mprev", "k_mnext"):
                mf = ph.sb(nm + "f", [128, 512], F32)
                kb.dma("sp", mf[:], I[nm], W=[mf])
                m_ = ph.sb(nm + "b", [128, 512], BF16)
                kb.cp(m_[:], mf[:], R=[mf], W=[m_])
                mb.append(m_)
            sk = ph.sb("sk", [64, 8], F32)
            kb.dma("sp", sk[:], I["attn_sink"][j].partition_broadcast(64), W=[sk])
            kb.act(sk[:], sk[:], AF.Exp, R=[sk], W=[sk])
            sinkE = ph.sb("sinkE", [64, 2, 4, 128], F32)
            for g in range(2):
                for hj in range(4):
                    kb.cp(sinkE[:, g, hj, :], sk[:, g * 4 + hj:g * 4 + hj + 1].to_broadcast([64, 128]), R=[sk], W=[sinkE])
            zq = [ph.sb("zq%d" % i, [128, 512], F32) for i in range(3)]
            rc = [ph.sb("rc%d" % i, [128, 512], F32) for i in range(2)]
            rs_ = [ph.sb("rsn%d" % i, [128, 512], F32) for i in range(2)]
            t1 = [ph.sb("t1%d" % i, [128, 512], F32) for i in range(2)]
            t2 = [ph.sb("t2%d" % i, [128, 512], F32) for i in range(2)]
            zi = 0
            for ci, (t0, n) in enumerate(tok_chunks()):
                if t0 >= TC:
                    C_ = rc[ci % 2]; S_ = rs_[ci % 2]
                    kb.dma("sp", C_[:], I["k_ropeC"][:, t0 - TC:t0 - TC + n], W=[C_])
                    kb.dma("sp", S_[:], I["k_ropeS"][:, t0 - TC:t0 - TC + n], W=[S_])
                for fc in range(5):
                    z = zq[zi % 3]
                    zi += 1
                    kb.dma("sp", z[:, 0:n], self.ZT[(8 + fc) * 128:(9 + fc) * 128, t0:t0 + n], R=[self.ZT], W=[z])
                    dst = QT[:, fc, t0:t0 + n] if fc < 4 else KT[:, t0:t0 + n]
                    dreg = QT if fc < 4 else KT
                    if t0 < TC:
                        kb.cp(dst, z[:, 0:n], R=[z], W=[dreg], eng="act")
                    else:
                        pb = kb.bank()
                        kb.mm(pb[:, 0:n], perm[:], z[:, 0:n], True, True, R=[perm, z], W=[pb])
                        a_ = t1[zi % 2]; b_ = t2[zi % 2]
                        kb.tt(a_[:, 0:n], z[:, 0:n], C_[:, 0:n], ALU.mult, R=[z, C_], W=[a_])
                        kb.tt(b_[:, 0:n], pb[:, 0:n], S_[:, 0:n], ALU.mult, R=[pb, S_], W=[b_])
                        kb.tt(dst, a_[:, 0:n], b_[:, 0:n], ALU.add, R=[a_, b_], W=[dreg])
            PT = [[ph.sb("PT%d_%d" % (i, k), [128, 512], BF16) for k in range(5)] for i in range(2)]
            den = [ph.sb("den%d" % i, [64, 512], F32) for i in range(2)]
            ot = [ph.sb("ot%d" % i, [64, 512], BF16) for i in range(2)]
            blocks = []
            if need_ctx:
                blocks += [(0, [(0, None), (128, None)]), (128, [(0, None), (128, None)])]
            for n_ in range(T // 128):
                t0 = TC + 128 * n_
                kts = []
                if n_ > 0:
                    kts.append((t0 - 128, 0))
                kts.append((t0, None))
                if n_ < T // 128 - 1:
                    kts.append((t0 + 128, 1))
                kts += [(0, None), (128, None)]
                blocks.append((t0, kts))
            bi = 0
            for (t0, kts) in blocks:
                for g in range(2):
                    pr = slice(g * 64, (g + 1) * 64)
                    pts = PT[bi % 2]
                    for ki, (tk, mk) in enumerate(kts):
                        pb = kb.bank()
                        kb.mm(pb[:], KT[pr, tk:tk + 128], QT[pr, :, t0:t0 + 128], True, mk is None, R=[KT, QT], W=[pb])
                        if mk is not None:
                            kb.mm(pb[:], self.identb[:], mb[mk][:], False, True, R=[self.identb, mb[mk]], W=[pb])
                        kb.act(pts[ki][:], pb[:], AF.Exp, R=[pb], W=[pts[ki]], scale=0.125)
                    po = kb.bank()
                    for ki, (tk, mk) in enumerate(kts):
                        kb.mm(po[0:64, :], VK[:, tk // 128, g * 64:(g + 1) * 64], pts[ki][:], ki == 0, ki == len(kts) - 1, R=[VK, pts[ki]], W=[po])
                    pd = kb.bank()
                    for ki, (tk, mk) in enumerate(kts):
                        kb.mm(pd[0:64, :], self.onesb[:, 0:64], pts[ki][:], ki == 0, ki == len(kts) - 1, R=[self.onesb, pts[ki]], W=[pd])
                    dn = den[bi % 2]; o_ = ot[bi % 2]
                    kb.tt(dn[:], pd[0:64, :], sinkE[:, g, :, :].rearrange("p j t -> p (j t)"), ALU.add, R=[pd, sinkE], W=[dn])
                    kb.recip(dn[:], dn[:], R=[dn], W=[dn])
                    kb.tt(o_[:], po[0:64, :], dn[:], ALU.mult, R=[po, dn], W=[o_])
                    kb.dma("sp", self.MT[512 + g * 256:512 + (g + 1) * 256, t0:t0 + 128].rearrange("(j d) t -> d j t", d=64),
                           o_[:].rearrange("p (j t) -> p j t", t=128), R=[o_], W=[self.MT])
                    bi += 1

    def p3(self, l, wout, units, moe):
        kb = self.kb
        I = self.inp
        need_ctx = l < DEPTH - 1
        j = l // 2
        with kb.phase() as ph:
            Wo = ph.sb("Wo", [128, 8, D], BF16)
            kb.dma("pool", Wo[:], wout.rearrange("(k p) n -> p k n", p=128), W=[Wo])
            x1 = ph.sb("x1", [128, 8, 1024], F32)
            x1r = [TT(x1.t, Reg()) for _ in range(8)]
            mt = [ph.sb("mt%d" % i, [128, 8, 512], BF16) for i in range(2)]
            h2 = ph.sb("h2", [128, 8, 1024], BF16)
            actb = ph.sb("actb", [128, 11, 1024], BF16)
            W2e = ph.sb("W2e", [128, 11, 1024], BF16)
            NR = 4
            ring = [ph.sb("ring%d" % i, [128, 2048], BF16) for i in range(NR)]
            tmp = {"sq": ph.sb("sq", [128, 8, 512], BF16), "rs": ph.sb("rs", [128, 512], F32), "hf": ph.sb("hf", [128, 8, 512], F32)}
            sa = [ph.sb("sa%d" % i, [128, 512], F32) for i in range(2)]
            if moe is not None:
                WE = ph.sb("WE", [128, 8, 1024], BF16)
                rt = ph.sb("rt", [128, 8, 8], F32)
                kb.dma("sp", rt[:], I["moe_router"][j].rearrange("(k p) e -> p k e", p=128), W=[rt])
                sel = ph.sb("sel", [8, 8 * 128], F32)
                kb.dma("sp", sel[:], I["k_sel"], W=[sel])
                selb = ph.sb("selb", [8, 8 * 128], BF16)
                kb.cp(selb[:], sel[:], R=[sel], W=[selb])
                lg = ph.sb("lg", [128, 8], F32)
                m8 = ph.sb("m8", [128, 8], F32)
                gt = ph.sb("gt", [128, 4], F32)
                wtk = ph.sb("wtk", [128, 8], F32)
                wtk2 = ph.sb("wtk2", [128, 8], F32)
                wT = ph.sb("wT", [8, 512], BF16)
            XTv = self.XT.t.rearrange("(k p) t -> p k t", p=128)
            MTv = self.MT.t.rearrange("(k p) t -> p k t", p=128)
            supers = [(0, TC)] if need_ctx else []
            supers += [(TC + 1024 * i, 1024) for i in range(T // 1024)]
            ri = 0
            mi = 0
            si = 0
            for (T0, SN) in supers:
                s = 1 if T0 < TC else 0
                subs = [(o, min(512, SN - o)) for o in range(0, SN, 512)]
                kb.dma("sp", x1[:, :, 0:SN], XTv[:, :, T0:T0 + SN], R=[self.XT], W=x1r)
                for (o, n) in subs:
                    m_ = mt[mi % 2]
                    mi += 1
                    kb.dma("sp", m_[:, :, 0:n], MTv[:, :, T0 + o:T0 + o + n], R=[self.MT], W=[m_])
                    for oc in range(8):
                        pb = kb.bank()
                        for k in range(8):
                            kb.mm(pb[:, 0:n], Wo[:, k, oc * 128:(oc + 1) * 128], m_[:, k, 0:n], k == 0, k == 7, R=[Wo, m_], W=[pb])
                        kb.stt(x1[:, oc, o:o + n], pb[:, 0:n], self.MOD[:, l, 16 + oc, s:s + 1], x1[:, oc, o:o + n], ALU.mult, ALU.add,
                               R=[pb, self.MOD, x1r[oc]], W=[x1r[oc]])
                    xv = TT(x1.t[:, :, o:o + n], None)
                    xv.regs = x1r
                    hv = TT(h2.t[:, :, o:o + n], h2.g)
                    self.normmod(ph, xv, n, lambda k: self.G4[:, l, k, s:s + 1], lambda k: self.MOD[:, l, 24 + k, s:s + 1], s, hv, tmp, fp32out=(moe is not None))
                    if moe is not None:
                        hf = tmp["hf"]
                        for tl in range(n // 128):
                            pb = kb.bank()
                            for k in range(8):
                                kb.mm(pb[:, 0:8], hf[:, k, tl * 128:(tl + 1) * 128], rt[:, k, :], k == 0, k == 7, R=[hf, rt], W=[pb])
                            kb.cp(lg[:], pb[:, 0:8], R=[pb], W=[lg])
                            kb.op("dve", lambda e: e.max(out=m8[:], in_=lg[:]), R=[lg], W=[m8])
                            kb.tt(gt[:, 0:1], m8[:, 1:2], m8[:, 0:1], ALU.subtract, R=[m8], W=[gt])
                            kb.act(gt[:, 1:2], gt[:, 0:1], AF.Exp, R=[gt], W=[gt])
                            kb.ts(gt[:, 2:3], gt[:, 1:2], 1.0, None, ALU.add, None, R=[gt], W=[gt])
                            kb.recip(gt[:, 2:3], gt[:, 2:3], R=[gt], W=[gt])
                            kb.tt(gt[:, 3:4], gt[:, 1:2], gt[:, 2:3], ALU.mult, R=[gt], W=[gt])
                            kb.ts(wtk[:], lg[:], m8[:, 0:1], gt[:, 2:3], ALU.is_equal, ALU.mult, R=[lg, m8, gt], W=[wtk])
                            kb.ts(wtk2[:], lg[:], m8[:, 1:2], gt[:, 3:4], ALU.is_equal, ALU.mult, R=[lg, m8, gt], W=[wtk2])
                            kb.tt(wtk[:], wtk[:], wtk2[:], ALU.add, R=[wtk, wtk2], W=[wtk])
                            pt_ = kb.bank()
                            kb.op("pe", lambda e, pt_=pt_: e.transpose(pt_[0:8, 0:128], wtk[:], self.ident[:]), R=[wtk, self.ident], W=[pt_])
                            kb.cp(wT[:, tl * 128:(tl + 1) * 128], pt_[0:8, 0:128], R=[pt_], W=[wT])
                        for e_ in range(NEXP):
                            pb = kb.bank()
                            kb.mm(pb[:, 0:n], selb[:, e_ * 128:(e_ + 1) * 128], wT[:, 0:n], True, True, R=[selb, wT], W=[pb])
                            kb.cp(WE[:, e_, o:o + n], pb[:, 0:n], R=[pb], W=[WE], eng="act")
                for ui, u in enumerate(units):
                    for fc in range(11):
                        rg = ring[ri % NR]
                        ri += 1
                        kb.dma("sp", rg[:], self.U[u, :, fc, 0:2048], R=[self.U], W=[rg])
                        for (o, n) in subs:
                            pa = kb.bank()
                            for k in range(8):
                                kb.mm(pa[:, 0:n], rg[:, k * 128:(k + 1) * 128], h2[:, k, o:o + n], k == 0, k == 7, R=[rg, h2], W=[pa])
                            pb = kb.bank()
                            for k in range(8):
                                kb.mm(pb[:, 0:n], rg[:, 1024 + k * 128:1024 + (k + 1) * 128], h2[:, k, o:o + n], k == 0, k == 7, R=[rg, h2], W=[pb])
                            s_ = sa[si % 2]
                            si += 1
                            kb.act(s_[:, 0:n], pa[:, 0:n], AF.Silu, R=[pa], W=[s_])
                            kb.tt(actb[:, fc, o:o + n], s_[:, 0:n], pb[:, 0:n], ALU.mult, R=[s_, pb], W=[actb])
                            if moe is not None:
                                kb.tt(actb[:, fc, o:o + n], actb[:, fc, o:o + n], WE[:, ui, o:o + n], ALU.mult, R=[actb, WE], W=[actb], eng="pool")
                    kb.dma("sp", W2e[:], self.U[u, :, :, 2048:3072], R=[self.U], W=[W2e])
                    for oc in range(8):
                        for (o, n) in subs:
                            pb = kb.bank()
                            for fc in range(11):
                                kb.mm(pb[:, 0:n], W2e[:, fc, oc * 128:(oc + 1) * 128], actb[:, fc, o:o + n], fc == 0, fc == 10, R=[W2e, actb], W=[pb])
                            kb.stt(x1[:, oc, o:o + n], pb[:, 0:n], self.MOD[:, l, 40 + oc, s:s + 1], x1[:, oc, o:o + n], ALU.mult, ALU.add,
                                   R=[pb, self.MOD, x1r[oc]], W=[x1r[oc]])
                        if ui == len(units) - 1:
                            kb.dma("sp", self.XT[oc * 128:(oc + 1) * 128, T0:T0 + SN], x1[:, oc, 0:SN], R=[x1r[oc]], W=[self.XT])

    def layer_cd(self, l):
        kb = self.kb
        I = self.inp
        j = l // 2
        with kb.phase() as ph:
            W = ph.sb("Wcd", [128, 8, 3072], BF16)
            wv = I["w_in_cd"][j].rearrange("(k p) n -> p k n", p=128)
            for h in range(2):
                kb.dma("pool", W[:, :, h * 1536:(h + 1) * 1536], wv[:, :, h * 1536:(h + 1) * 1536], W=[W])
            self.p1_generic(ph, l, W, 24, lambda oc: (oc * 128, oc * 128))
        if "s5" not in self.skip:
            self.s5(l)
            self.s5_glu(l)
        if "hg" not in self.skip:
            self.hgrn(l)
        if "moe" not in self.skip:
            self.p3(l, I["w_out_cd"][j], [4 + j * 8 + e for e in range(NEXP)], moe=True)

    def s5(self, l):
        kb = self.kb
        I = self.inp
        j = l // 2
        NJ = TA // 16
        HJ = NJ // 2
        MAGIC = 12582912.0
        TWO_PI = 2.0 * math.pi
        with kb.phase() as ph:
            smr = Reg()
            SM = TT(None, smr)

            def T_(name, shape, dt=F32):
                t = ph.sb(name, shape, dt)
                t.g = smr
                return t

            def tt_(o, a, b, op):
                kb.tt(o, a, b, op, R=[SM], W=[SM])

            def ts_(o, a, s1, s2, op0, op1=None):
                kb.ts(o, a, s1, s2, op0, op1, R=[SM], W=[SM])

            def act_(o, a, f, **kw):
                kb.act(o, a, f, R=[SM], W=[SM], **kw)

            def frac_(o, x, tmp):
                ts_(tmp, x, MAGIC, None, ALU.add)
                ts_(tmp, tmp, -MAGIC, None, ALU.add)
                tt_(o, x, tmp, ALU.subtract)

            PBpad = ph.sb("PBpad", [128, 4, 16, 2, 128], BF16)
            CPpad = ph.sb("CPpad", [128, 4, 17, 2, 128], BF16)
            kb.memset(PBpad, 0.0)
            kb.memset(CPpad, 0.0)
            J1 = ph.sb("J1", [128, NJ], F32)
            kb.dma("sp", J1[:], I["k_j1"], W=[J1])
            Dv = T_("Dv", [128, 4])
            kb.dma("sp", Dv[:], I["s5_d"][j].rearrange("(q p) -> p q", p=128), W=[SM])
            Cnat = ph.sb("Cnat", [16, 32, 64], F32)
            P_ = []
            for d in range(2):
                p = {}
                are = T_("are%d" % d, [128, 16]); aim = T_("aim%d" % d, [128, 16]); ls = T_("ls%d" % d, [128, 16])
                Br = T_("Br%d" % d, [128, 16, 16]); Bi = T_("Bi%d" % d, [128, 16, 16])
                Cr = T_("Cr%d" % d, [128, 16, 16]); Ci = T_("Ci%d" % d, [128, 16, 16])
                for a in range(2):
                    pr = slice(a * 64, (a + 1) * 64)
                    kb.dma("sp", are[pr, :], I["s5_a_re"][j, d].rearrange("(P a) n -> a n P", a=2)[a], W=[SM])
                    kb.dma("sp", aim[pr, :], I["s5_a_im"][j, d].rearrange("(P a) n -> a n P", a=2)[a], W=[SM])
                    kb.dma("sp", ls[pr, :], I["s5_log_step"][j, d].rearrange("(P a) -> a P", a=2)[a].partition_broadcast(64), W=[SM])
                    kb.dma("sp", Br[pr, :, :], I["s5_b_re"][j, d].rearrange("(P a) n c -> a n P c", a=2)[a], W=[SM])
                    kb.dma("sp", Bi[pr, :, :], I["s5_b_im"][j, d].rearrange("(P a) n c -> a n P c", a=2)[a], W=[SM])
                for (nm, dst) in (("s5_c_re", Cr), ("s5_c_im", Ci)):
                    kb.dma("sp", Cnat[:], I[nm][j, d].rearrange("g c n -> c g n"), W=[Cnat])
                    pb = kb.bank()
                    for P in range(16):
                        kb.op("pe", lambda e, pb=pb, P=P: e.transpose(pb[:, P * 16:(P + 1) * 16],
                              Cnat[:, 2 * P:2 * P + 2, :].rearrange("c a n -> c (a n)"), self.ident[0:16, 0:16]),
                              R=[Cnat, self.ident], W=[pb])
                    kb.cp(dst[:].rearrange("p a c -> p (a c)"), pb[:, 0:256], R=[pb], W=[SM])
                lr = T_("lr%d" % d, [128, 16]); dt = T_("dt%d" % d, [128, 16]); tq = T_("tq%d" % d, [128, 16])
                mag = T_("mag%d" % d, [128, 16]); mg16 = T_("mg16%d" % d, [128, 16]); trn = T_("trn%d" % d, [128, 16])
                tmp = T_("tmp%d" % d, [128, 16]); fx = T_("fx%d" % d, [128, 16]); sv = T_("sv%d" % d, [128, 16]); cv = T_("cv%d" % d, [128, 16])
                lbr = T_("lbr%d" % d, [128, 16]); lbi = T_("lbi%d" % d, [128, 16]); zr = T_("zr%d" % d, [128, 16])
                den = T_("den%d" % d, [128, 16]); fr = T_("fr%d" % d, [128, 16]); fi = T_("fi%d" % d, [128, 16]); t16 = T_("t16%d" % d, [128, 16])
                u1 = T_("u1%d" % d, [128, 16]); u2 = T_("u2%d" % d, [128, 16])
                ts_(lr[:], are[:], -1e-4, None, ALU.min)
                act_(dt[:], ls[:], AF.Exp)
                tt_(tq[:], lr[:], dt[:], ALU.mult)
                act_(mag[:], tq[:], AF.Exp)
                act_(mg16[:], tq[:], AF.Exp, scale=16.0)
                tt_(trn[:], aim[:], dt[:], ALU.mult)
                ts_(trn[:], trn[:], 1.0 / TWO_PI, None, ALU.mult)
                frac_(fx[:], trn[:], tmp[:])
                act_(sv[:], fx[:], AF.Sin, scale=TWO_PI)
                ts_(u1[:], trn[:], 0.25, None, ALU.add)
                frac_(fx[:], u1[:], tmp[:])
                act_(cv[:], fx[:], AF.Sin, scale=TWO_PI)
                tt_(lbr[:], mag[:], cv[:], ALU.mult)
                tt_(lbi[:], mag[:], sv[:], ALU.mult)
                ts_(zr[:], lbr[:], -1.0, None, ALU.add)
                tt_(den[:], lr[:], lr[:], ALU.mult)
                tt_(u1[:], aim[:], aim[:], ALU.mult)
                tt_(den[:], den[:], u1[:], ALU.add)
                kb.recip(den[:], den[:], R=[SM], W=[SM])
                tt_(u1[:], zr[:], lr[:], ALU.mult)
                tt_(u2[:], lbi[:], aim[:], ALU.mult)
                tt_(u1[:], u1[:], u2[:], ALU.add)
                tt_(fr[:], u1[:], den[:], ALU.mult)
                tt_(u1[:], lbi[:], lr[:], ALU.mult)
                tt_(u2[:], zr[:], aim[:], ALU.mult)
                tt_(u1[:], u1[:], u2[:], ALU.subtract)
                tt_(fi[:], u1[:], den[:], ALU.mult)
                ts_(u1[:], trn[:], 16.0, None, ALU.mult)
                frac_(t16[:], u1[:], tmp[:])
                bbr = T_("bbr%d" % d, [128, 16, 16]); bbi = T_("bbi%d" % d, [128, 16, 16])
                w1 = T_("w1%d" % d, [128, 16, 16]); w2 = T_("w2%d" % d, [128, 16, 16])
                frb = fr[:].unsqueeze(2).to_broadcast([128, 16, 16])
                fib = fi[:].unsqueeze(2).to_broadcast([128, 16, 16])
                tt_(w1[:], Br[:], frb, ALU.mult)
                tt_(w2[:], Bi[:], fib, ALU.mult)
                tt_(bbr[:], w1[:], w2[:], ALU.subtract)
                tt_(w1[:], Bi[:], frb, ALU.mult)
                tt_(w2[:], Br[:], fib, ALU.mult)
                tt_(bbi[:], w1[:], w2[:], ALU.add)
                pwr = T_("pwr%d" % d, [128, 17, 16]); pwi = T_("pwi%d" % d, [128, 17, 16])
                kb.memset(TT(pwr.t[:, 0, :], smr), 1.0, eng="dve") if False else kb.op("dve", lambda e: e.memset(pwr[:, 0, :], 1.0), W=[SM])
                kb.op("dve", lambda e: e.memset(pwi[:, 0, :], 0.0), W=[SM])
                for tau in range(16):
                    tt_(u1[:], pwr[:, tau, :], lbr[:], ALU.mult)
                    tt_(u2[:], pwi[:, tau, :], lbi[:], ALU.mult)
                    tt_(pwr[:, tau + 1, :], u1[:], u2[:], ALU.subtract)
                    tt_(u1[:], pwr[:, tau, :], lbi[:], ALU.mult)
                    tt_(u2[:], pwi[:, tau, :], lbr[:], ALU.mult)
                    tt_(pwi[:, tau + 1, :], u1[:], u2[:], ALU.add)
                p.update(bbr=bbr, bbi=bbi, Cr=Cr, Ci=Ci, pwr=pwr, pwi=pwi, mg16=mg16, t16=t16)
                P_.append(p)
            Y = ph.sb("Y", [128, TA], F32)
            up = [ph.sb("up%d" % d, [128, 16, NJ], BF16) for d in range(2)]
            Kt = ph.sb("Kt", [128, 16, 128], BF16)
            WGT = ph.sb("WGT", [128, 16, 2, 128], BF16)
            Hr = ph.sb("Hr", [128, 4, NJ], BF16)
            Hi = ph.sb("Hi", [128, 4, NJ], BF16)
            Ec = ph.sb("Ec", [128, NJ], F32)
            Es = ph.sb("Es", [128, NJ], F32)
            tA, tB, tC, tD, tE, tF = [ph.sb("s5t%d" % i, [128, NJ], F32) for i in range(6)]
            x1 = T_("x1t", [128, 17, 16]); x2 = T_("x2t", [128, 17, 16])
            Yv = Y.t[:].rearrange("p (j t) -> p j t", t=16)
            Ycv = Y.t[:, 0:TC].rearrange("p (j t) -> p j t", t=16)
            Yxv = Y.t[:, TC:TA].rearrange("p (j t) -> p j t", t=16)
            for q in range(4):
                kb.dma("sp", Y[:], self.ZT[q * 128:(q + 1) * 128, :], R=[self.ZT], W=[Y])
                upf0 = up[0].t[:].rearrange("p s j -> p (s j)")
                upv0 = up[0].t[:].rearrange("p s j -> p j s")
                upv1 = up[1].t[:].rearrange("p s j -> p j s")
                kb.cp(upv0, Yv, R=[Y], W=[up[0]], eng="act")
                kb.cp(upv1[:, 0:16, :], Ycv[:, ::-1, ::-1], R=[Y], W=[up[1]])
                kb.cp(upv1[:, 16:NJ, :], Yxv[:, ::-1, ::-1], R=[Y], W=[up[1]])
                kb.ts(Y[:], Y[:], Dv[:, q:q + 1], None, ALU.mult, None, R=[Y, SM], W=[Y])
                for d in range(2):
                    p = P_[d]
                    for pi in range(4):
                        P = q * 4 + pi
                        oA = 32 * pi
                        for (src_r, src_i, pad, nt, t0_, negim) in ((p["bbr"], p["bbi"], PBpad, 16, 0, False), (p["Cr"], p["Ci"], CPpad, 17, 0, True)):
                            pr_b = p["pwr"][:, 0:nt, P].unsqueeze(2).to_broadcast([128, nt, 16])
                            pi_b = p["pwi"][:, 0:nt, P].unsqueeze(2).to_broadcast([128, nt, 16])
                            sr_b = src_r[:, P, :].unsqueeze(1).to_broadcast([128, nt, 16])
                            si_b = src_i[:, P, :].unsqueeze(1).to_broadcast([128, nt, 16])
                            tt_(x1[:, 0:nt, :], pr_b, sr_b, ALU.mult)
                            tt_(x2[:, 0:nt, :], pi_b, si_b, ALU.mult)
                            for a in range(2):
                                prt = slice(a * 64, (a + 1) * 64)
                                kb.tt(pad[prt, pi, :, 0, oA + 16 * a:oA + 16 * a + 16], x1[prt, 0:nt, :], x2[prt, 0:nt, :], ALU.subtract, R=[SM], W=[pad])
                            tt_(x1[:, 0:nt, :], pr_b, si_b, ALU.mult)
                            tt_(x2[:, 0:nt, :], pi_b, sr_b, ALU.mult)
                            for a in range(2):
                                prt = slice(a * 64, (a + 1) * 64)
                                if negim:
                                    kb.stt(pad[prt, pi, :, 1, oA + 16 * a:oA + 16 * a + 16], x1[prt, 0:nt, :], -1.0, x2[prt, 0:nt, :], ALU.mult, ALU.subtract, R=[SM], W=[pad])
                                else:
                                    kb.tt(pad[prt, pi, :, 1, oA + 16 * a:oA + 16 * a + 16], x1[prt, 0:nt, :], x2[prt, 0:nt, :], ALU.add, R=[SM], W=[pad])
                    for tau in range(16):
                        pb = kb.bank()
                        for pi in range(4):
                            kb.mm(pb[:, 0:128], PBpad[:, pi, tau, 0, :], CPpad[:, pi, 0, 0, :], pi == 0, False, R=[PBpad, CPpad], W=[pb])
                            kb.mm(pb[:, 0:128], PBpad[:, pi, tau, 1, :], CPpad[:, pi, 0, 1, :], False, pi == 3, R=[PBpad, CPpad], W=[pb])
                        kb.cp(Kt[:, tau, :], pb[:, 0:128], R=[pb], W=[Kt], eng=("act" if tau % 2 else "dve"))
                    for pi in range(4):
                        P = q * 4 + pi
                        for tb in range(4):
                            pb = kb.bank()
                            pbv = pb.t[:].bitcast(BF16)
                            for k8 in range(8):
                                tau = tb * 4 + k8 // 2
                                ri = k8 % 2
                                kb.op("pe", lambda e, pbv=pbv, k8=k8, tau=tau, ri=ri, pi=pi: e.transpose(pbv[:, k8 * 128:(k8 + 1) * 128], PBpad[:, pi, tau, ri, :], self.identb[:]),
                                      R=[PBpad, self.identb], W=[pb])
                            kb.cp(WGT[:, tb * 4:(tb + 1) * 4, :, :].rearrange("p a b c -> p (a b c)"), pbv[:, 0:1024], R=[pb], W=[WGT], eng=("act" if tb % 2 else "dve"))
                        kb.ts(tA[:], J1[:], p["t16"][:, P:P + 1], None, ALU.mult, None, R=[J1, SM], W=[tA])
                        kb.ts(tB[:], tA[:], MAGIC, None, ALU.add, None, R=[tA], W=[tB])
                        kb.ts(tB[:], tB[:], -MAGIC, None, ALU.add, None, R=[tB], W=[tB])
                        kb.tt(tB[:], tA[:], tB[:], ALU.subtract, R=[tA, tB], W=[tB])
                        kb.act(Es[:], tB[:], AF.Sin, R=[tB], W=[Es], scale=TWO_PI)
                        kb.ts(tA[:], tA[:], 0.25, None, ALU.add, None, R=[tA], W=[tA])
                        kb.ts(tB[:], tA[:], MAGIC, None, ALU.add, None, R=[tA], W=[tB])
                        kb.ts(tB[:], tB[:], -MAGIC, None, ALU.add, None, R=[tB], W=[tB])
                        kb.tt(tB[:], tA[:], tB[:], ALU.subtract, R=[tA, tB], W=[tB])
                        kb.act(Ec[:], tB[:], AF.Sin, R=[tB], W=[Ec], scale=TWO_PI)
                        G = [[None, None], [None, None]]
                        for ri in range(2):
                            for hh in range(2):
                                pb = kb.bank()
                                for s in range(16):
                                    kb.mm(pb[:, 0:HJ], WGT[:, 15 - s, ri, :], up[d][:, s, hh * HJ:(hh + 1) * HJ], s == 0, s == 15, R=[WGT, up[d]], W=[pb])
                                G[ri][hh] = pb
                        for hh in range(2):
                            cs = slice(hh * HJ, (hh + 1) * HJ)
                            Gr = G[0][hh]; Gi = G[1][hh]
                            kb.tt(tA[:, cs], Gr[:, 0:HJ], Ec[:, cs], ALU.mult, R=[Gr, Ec], W=[tA])
                            kb.tt(tB[:, cs], Gi[:, 0:HJ], Es[:, cs], ALU.mult, R=[Gi, Es], W=[tB])
                            kb.tt(tC[:, cs], tA[:, cs], tB[:, cs], ALU.add, R=[tA, tB], W=[tC])
                            kb.tt(tA[:, cs], Gi[:, 0:HJ], Ec[:, cs], ALU.mult, R=[Gi, Ec], W=[tA])
                            kb.tt(tB[:, cs], Gr[:, 0:HJ], Es[:, cs], ALU.mult, R=[Gr, Es], W=[tB])
                            kb.tt(tD[:, cs], tA[:, cs], tB[:, cs], ALU.subtract, R=[tA, tB], W=[tD])
                        mgb = p["mg16"][:, P:P + 1].to_broadcast([128, NJ])
                        kb.op("dve", lambda e, mgb=mgb: e.tensor_tensor_scan(out=tE[:], data0=mgb, data1=tC[:], initial=0.0, op0=ALU.mult, op1=ALU.add), R=[tC, SM], W=[tE])
                        kb.op("dve", lambda e, mgb=mgb: e.tensor_tensor_scan(out=tF[:], data0=mgb, data1=tD[:], initial=0.0, op0=ALU.mult, op1=ALU.add), R=[tD, SM], W=[tF])
                        kb.tt(tA[:], tE[:], Ec[:], ALU.mult, R=[tE, Ec], W=[tA])
                        kb.tt(tB[:], tF[:], Es[:], ALU.mult, R=[tF, Es], W=[tB])
                        kb.tt(Hr[:, pi, :], tA[:], tB[:], ALU.subtract, R=[tA, tB], W=[Hr])
                        kb.tt(tA[:], tF[:], Ec[:], ALU.mult, R=[tF, Ec], W=[tA])
                        kb.tt(tB[:], tE[:], Es[:], ALU.mult, R=[tE, Es], W=[tB])
                        kb.tt(Hi[:, pi, :], tA[:], tB[:], ALU.add, R=[tA, tB], W=[Hi])
                    for t in range(16):
                        for hh in range(2):
                            c0 = hh * HJ
                            pb = kb.bank()
                            for s in range(t + 1):
                                kb.mm(pb[:, 0:HJ], Kt[:, t - s, :], up[d][:, s, c0:c0 + HJ], s == 0, False, R=[Kt, up[d]], W=[pb])
                            lo = 1 if hh == 0 else 0
                            for pi in range(4):
                                kb.mm(pb[:, lo:HJ], CPpad[:, pi, t + 1, 0, :], Hr[:, pi, c0 + lo - 1:c0 + HJ - 1], False, False, R=[CPpad, Hr], W=[pb])
                                kb.mm(pb[:, lo:HJ], CPpad[:, pi, t + 1, 1, :], Hi[:, pi, c0 + lo - 1:c0 + HJ - 1], False, pi == 3, R=[CPpad, Hi], W=[pb])
                            if d == 0:
                                kb.tt(Yv[:, c0:c0 + HJ, t], pb[:, 0:HJ], Yv[:, c0:c0 + HJ, t], ALU.add, R=[pb, Y], W=[Y])
                            else:
                                tm = 15 - t
                                if hh == 0:
                                    kb.tt(Ycv[:, :, tm][:, ::-1], pb[:, 0:16], Ycv[:, :, tm][:, ::-1], ALU.add, R=[pb, Y], W=[Y])
                                    kb.tt(Yxv[:, 264:512, tm][:, ::-1], pb[:, 16:HJ], Yxv[:, 264:512, tm][:, ::-1], ALU.add, R=[pb, Y], W=[Y])
                                else:
                                    kb.tt(Yxv[:, 0:264, tm][:, ::-1], pb[:, 0:HJ], Yxv[:, 0:264, tm][:, ::-1], ALU.add, R=[pb, Y], W=[Y])
                kb.act(upf0, Y[:], AF.Gelu_apprx_tanh, R=[Y], W=[up[0]])
                kb.dma("sp", self.YG[q * 128:(q + 1) * 128, :], upf0, R=[up[0]], W=[self.YG])

    def s5_glu(self, l):
        kb = self.kb
        I = self.inp
        j = l // 2
        with kb.phase() as ph:
            Wg = ph.sb("Wg", [128, 4, 512], BF16)
            kb.dma("pool", Wg[:], I["s5_w_glu"][j].rearrange("(k p) n -> p k n", p=128), W=[Wg])
            bg = ph.sb("bg", [128, 4], F32)
            kb.dma("sp", bg[:], I["s5_b_glu"][j].rearrange("(q p) -> p q", p=128), W=[bg])
            yg = [ph.sb("yg%d" % i, [128, 4, 512], BF16) for i in range(2)]
            sg = [ph.sb("sg%d" % i, [128, 512], F32) for i in range(2)]
            ob = [ph.sb("gob%d" % i, [128, 512], BF16) for i in range(2)]
            YGv = self.YG.t.rearrange("(k p) t -> p k t", p=128)
            it = 0
            for ci, (t0, n) in enumerate(tok_chunks()):
                y_ = yg[ci % 2]
                kb.dma("sp", y_[:, :, 0:n], YGv[:, :, t0:t0 + n], R=[self.YG], W=[y_])
                for oc in range(4):
                    pb = kb.bank()
                    for k in range(4):
                        kb.mm(pb[:, 0:n], Wg[:, k, oc * 128:(oc + 1) * 128], y_[:, k, 0:n], k == 0, k == 3, R=[Wg, y_], W=[pb])
                    s_ = sg[it % 2]; o_ = ob[it % 2]
                    it += 1
                    kb.act(s_[:, 0:n], pb[:, 0:n], AF.Sigmoid, R=[pb, bg], W=[s_], bias=bg[:, oc:oc + 1])
                    kb.tt(o_[:, 0:n], y_[:, oc, 0:n], s_[:, 0:n], ALU.mult, R=[y_, s_], W=[o_])
                    kb.dma("sp", self.MT[oc * 128:(oc + 1) * 128, t0:t0 + n], o_[:, 0:n], R=[o_], W=[self.MT])

    def hgrn(self, l):
        kb = self.kb
        I = self.inp
        j = l // 2
        NBX = 1024
        blocks_mem = [(0, TC)] + [(TC + NBX * i, NBX) for i in range(T // NBX)]
        orders = [blocks_mem, [blocks_mem[0]] + blocks_mem[:0:-1]]
        with kb.phase() as ph:
            smr = Reg()
            SM = TT(None, smr)

            def T_(name, shape, dt=F32):
                t = ph.sb(name, shape, dt)
                t.g = smr
                return t
            raw = T_("raw", [128, 2, 4])
            for ly in range(2):
                kb.dma("sp", raw[:, ly, :], I["hg_lb_raw"][ly].rearrange("(h p) -> p h", p=128), W=[SM])
            lb = T_("lb", [128, 4]); oml = T_("oml", [128, 4])
            if j == 0:
                kb.op("dve", lambda e: e.memset(lb[:], 0.0), W=[SM])
                kb.op("dve", lambda e: e.memset(oml[:], 1.0), W=[SM])
            else:
                mx = T_("mx", [128, 4]); e0 = T_("e0", [128, 4]); e1 = T_("e1", [128, 4])
                kb.tt(mx[:], raw[:, 0, :], raw[:, 1, :], ALU.max, R=[SM], W=[SM])
                kb.tt(e0[:], raw[:, 0, :], mx[:], ALU.subtract, R=[SM], W=[SM])
                kb.tt(e1[:], raw[:, 1, :], mx[:], ALU.subtract, R=[SM], W=[SM])
                kb.act(e0[:], e0[:], AF.Exp, R=[SM], W=[SM])
                kb.act(e1[:], e1[:], AF.Exp, R=[SM], W=[SM])
                kb.tt(e0[:], e0[:], e1[:], ALU.add, R=[SM], W=[SM])
                kb.recip(e0[:], e0[:], R=[SM], W=[SM])
                kb.tt(lb[:], e1[:], e0[:], ALU.mult, R=[SM], W=[SM])
                kb.ts(oml[:], lb[:], -1.0, 1.0, ALU.mult, ALU.add, R=[SM], W=[SM])
            hgn = T_("hgn", [128, 1])
            kb.dma("sp", hgn[:], I["hg_norm"][j].rearrange("(p o) -> p o", o=1), W=[SM])
            M01 = ph.sb("M01", [128, NBX], F32)
            kb.dma("sp", M01[:], I["k_m01"], W=[M01])
            mk = ph.sb("mk128", [128, 128], F32)
            kb.dma("sp", mk[:], I["k_mask128"], W=[mk])
            C_ = []
            for d in range(4):
                c = {}
                c["Ost"] = ph.sb("Ost%d" % d, [128, NBX], F32)
                for nm in ("A", "B", "C", "Dd", "Ee", "Q", "V32"):
                    c[nm] = ph.sb("h%s%d" % (nm, d), [128, NBX], F32)
                for nm in ("q1b", "qmb", "kmb"):
                    c[nm] = ph.sb("h%s%d" % (nm, d), [128, NBX], BF16)
                c["khT"] = ph.sb("khT%d" % d, [128, NBX // 128, 128], BF16)
                c["vT"] = ph.sb("vT%d" % d, [128, NBX // 128, 128], BF16)
                c["S32"] = ph.sb("S32%d" % d, [128, 128], F32)
                c["As"] = ph.sb("As%d" % d, [128, 128], F32)
                c["Sb"] = ph.sb("Sb%d" % d, [128, 128], BF16)
                c["eb"] = ph.sb("eb%d" % d, [128, NBX // 64], F32)
                c["AT"] = [ph.sb("AT%d%d" % (d, i), [128, 128], BF16) for i in range(2)]
                C_.append(c)
            os_ = ph.sb("os_", [128, 512], F32); sqh = ph.sb("sqh", [128, 512], BF16); rsh = ph.sb("rsh", [128, 512], F32)
            gq = ph.sb("gq", [128, 512], F32); ohb = ph.sb("ohb", [128, 512], BF16)
            oa = ph.sb("oa", [128, 512], F32); obb = ph.sb("obb", [128, 512], F32)
            hbk = [0]

            def lbank():
                p = kb.ps[4 + hbk[0] % 4]
                hbk[0] += 1
                return p

            def prepass(ci, hd, m0, NB):
                c = C_[ci]
                d = ci % 2
                rv = (lambda ap: ap[:, ::-1]) if d else (lambda ap: ap)
                nch = NB // 64
                A, B, C, Dd, Ee, Q = (c[nm] for nm in ("A", "B", "C", "Dd", "Ee", "Q"))
                zrow = (1024 if d == 0 else 1536) + hd * 128
                kb.dma("sp", A[:, 0:NB], self.ZT[zrow:zrow + 128, m0:m0 + NB], R=[self.ZT], W=[A])
                kb.dma("sp", Q[:, 0:NB], self.ZT[512 + hd * 128:512 + (hd + 1) * 128, m0:m0 + NB], R=[self.ZT], W=[Q])
                kb.dma("sp", Ee[:, 0:NB], self.ZT[2048 + hd * 128:2048 + (hd + 1) * 128, m0:m0 + NB], R=[self.ZT], W=[Ee])
                kb.cp(c["V32"][:, 0:NB], rv(Ee[:, 0:NB]), R=[Ee], W=[c["V32"]])
                if d:
                    kb.cp(Dd[:, 0:NB], rv(A[:, 0:NB]), R=[A], W=[Dd])
                    kb.act(B[:, 0:NB], Dd[:, 0:NB], AF.Sigmoid, R=[Dd], W=[B])
                else:
                    kb.act(B[:, 0:NB], A[:, 0:NB], AF.Sigmoid, R=[A], W=[B])
                kb.ts(B[:, 0:NB], B[:, 0:NB], oml[:, hd:hd + 1], lb[:, hd:hd + 1], ALU.mult, ALU.add, R=[B, SM], W=[B])
                kb.act(C[:, 0:NB], B[:, 0:NB], AF.Ln, R=[B], W=[C])
                kb.ts(B[:, 0:NB], B[:, 0:NB], -1.0, 1.0, ALU.mult, ALU.add, R=[B], W=[B])
                kb.op("dve", lambda e: e.tensor_tensor_scan(out=A[:, 0:NB], data0=M01[:, 0:NB], data1=C[:, 0:NB], initial=0.0, op0=ALU.mult, op1=ALU.add),
                      R=[M01, C], W=[A])
                Av = A.t[:, 0:NB].rearrange("p (n c) -> p n c", c=64)
                Dv3 = Dd.t[:, 0:NB].rearrange("p (n c) -> p n c", c=64)
                kb.act(c["eb"][:, 0:nch], Av[:, :, 63], AF.Exp, R=[A], W=[c["eb"]])
                if d:
                    kb.cp(C[:, 0:NB], rv(Q[:, 0:NB]), R=[Q], W=[C])
                    kb.act(C[:, 0:NB], C[:, 0:NB], AF.Silu, R=[C], W=[C])
                else:
                    kb.act(C[:, 0:NB], Q[:, 0:NB], AF.Silu, R=[Q], W=[C])
                kb.act(Ee[:, 0:NB], A[:, 0:NB], AF.Exp, R=[A], W=[Ee])
                kb.tt(c["q1b"][:, 0:NB], C[:, 0:NB], Ee[:, 0:NB], ALU.mult, R=[C, Ee], W=[c["q1b"]])
                kb.tt(Dv3, Av, Av[:, :, 31:32].to_broadcast([128, nch, 64]), ALU.subtract, R=[A], W=[Dd])
                kb.ts(Dd[:, 0:NB], Dd[:, 0:NB], -80.0, 80.0, ALU.max, ALU.min, R=[Dd], W=[Dd])
                kb.act(Ee[:, 0:NB], Dd[:, 0:NB], AF.Exp, R=[Dd], W=[Ee])
                kb.tt(c["qmb"][:, 0:NB], C[:, 0:NB], Ee[:, 0:NB], ALU.mult, R=[C, Ee], W=[c["qmb"]])
                kb.act(Ee[:, 0:NB], Dd[:, 0:NB], AF.Exp, R=[Dd], W=[Ee], scale=-1.0)
                kb.tt(c["kmb"][:, 0:NB], B[:, 0:NB], Ee[:, 0:NB], ALU.mult, R=[B, Ee], W=[c["kmb"]])
                kb.tt(Dv3, Av[:, :, 63:64].to_broadcast([128, nch, 64]), Av, ALU.subtract, R=[A], W=[Dd])
                kb.act(Ee[:, 0:NB], Dd[:, 0:NB], AF.Exp, R=[Dd], W=[Ee])
                kb.tt(Dd[:, 0:NB], B[:, 0:NB], Ee[:, 0:NB], ALU.mult, R=[B, Ee, Dd], W=[Dd])
                for dc in range(NB // 128):
                    if "hgT" in self.skip:
                        break
                    pb = lbank()
                    pb2 = lbank()
                    kb.op("pe", lambda e, pb=pb, dc=dc: e.transpose(pb[:, 0:128], Dd[:, dc * 128:(dc + 1) * 128], self.ident[:]), R=[Dd, self.ident], W=[pb])
                    kb.op("pe", lambda e, pb2=pb2, dc=dc: e.transpose(pb2[:, 0:128], c["V32"][:, dc * 128:(dc + 1) * 128], self.ident[:]), R=[c["V32"], self.ident], W=[pb2])
                    kb.cp(c["khT"][:, dc, :], pb[:, 0:128], R=[pb], W=[c["khT"]], eng="act")
                    kb.cp(c["vT"][:, dc, :], pb2[:, 0:128], R=[pb2], W=[c["vT"]], eng="dve")

            st = {}

            def stepA(ci, dc):
                c = C_[ci]
                d = ci
                cols = slice(dc * 128, (dc + 1) * 128)
                pA = lbank()
                kb.mm(pA[:, 0:128], c["kmb"][:, cols], c["qmb"][:, cols], True, True, R=[c["kmb"], c["qmb"]], W=[pA])
                AT = c["AT"][dc % 2]
                As = c["As"]
                kb.ts(As[:], pA[:, 0:128], -1e30, 1e30, ALU.max, ALU.min, R=[pA], W=[As])
                kb.tt(AT[:], As[:], mk[:], ALU.mult, R=[As, mk], W=[AT])
                pO = kb.ps[ci]
                kb.mm(pO[:, 0:128], c["vT"][:, dc, :], AT[:], True, False, R=[c["vT"], AT], W=[pO])
                st[ci] = pO
                half(ci, dc, 0, pO)

            def half(ci, dc, hf, pO):
                c = C_[ci]
                ch = 2 * dc + hf
                c64 = slice(dc * 128 + hf * 64, dc * 128 + hf * 64 + 64)
                prt = slice(hf * 64, hf * 64 + 64)
                kb.mm(pO[:, hf * 64:(hf + 1) * 64], c["Sb"][:], c["q1b"][:, c64], False, hf == 1, R=[c["Sb"], c["q1b"]], W=[pO])
                pU = lbank()
                kb.mm(pU[:, 0:128], c["khT"][prt, dc, :], c["vT"][prt, dc, :], True, True, R=[c["khT"], c["vT"]], W=[pU])
                kb.stt(c["Sb"][:], c["S32"][:], c["eb"][:, ch:ch + 1], pU[:, 0:128], ALU.mult, ALU.add, R=[c["S32"], c["eb"], pU], W=[c["Sb"]])
                kb.stt(c["S32"][:], c["S32"][:], c["eb"][:, ch:ch + 1], pU[:, 0:128], ALU.mult, ALU.add, R=[c["S32"], c["eb"], pU], W=[c["S32"]])

            def stepB(ci, dc, NB):
                pO = st[ci]
                c = C_[ci]
                half(ci, dc, 1, pO)
                if ci % 2 == 0:
                    kb.cp(c["Ost"][:, dc * 128:(dc + 1) * 128], pO[:, 0:128], R=[pO], W=[c["Ost"]], eng="act")
                else:
                    a = NB - 128 * (dc + 1)
                    kb.cp(c["Ost"][:, a:a + 128][:, ::-1], pO[:, 0:128], R=[pO], W=[c["Ost"]], eng="dve")

            for hp in range(2):
                for ci in range(4):
                    kb.memset(C_[ci]["S32"], 0.0)
                    kb.memset(C_[ci]["Sb"], 0.0)
                for bi in range(len(blocks_mem)):
                    NB = orders[0][bi][1]
                    for ci in range(4):
                        prepass(ci, 2 * hp + ci // 2, orders[ci % 2][bi][0], NB)
                    for dc in range(NB // 128):
                        for ci in range(4):
                            stepA(ci, dc)
                        for ci in range(4):
                            stepB(ci, dc, NB)
                    for ci in range(4):
                        hd = 2 * hp + ci // 2
                        m0 = orders[ci % 2][bi][0]
                        kb.dma("sp", self.OD[ci % 2, hd * 128:(hd + 1) * 128, m0:m0 + NB], C_[ci]["Ost"][:, 0:NB], R=[C_[ci]["Ost"]], W=[self.ODr])
            kb.barrier()
            for hd in range(4):
                for (t0, n) in tok_chunks():
                    kb.dma("sp", oa[:, 0:n], self.OD[0, hd * 128:(hd + 1) * 128, t0:t0 + n], R=[self.ODr], W=[oa])
                    kb.dma("sp", obb[:, 0:n], self.OD[1, hd * 128:(hd + 1) * 128, t0:t0 + n], R=[self.ODr], W=[obb])
                    kb.tt(os_[:, 0:n], oa[:, 0:n], obb[:, 0:n], ALU.add, R=[oa, obb], W=[os_])
                    kb.act(sqh[:, 0:n], os_[:, 0:n], AF.Square, R=[os_], W=[sqh])
                    pb = kb.bank()
                    kb.mm(pb[:, 0:n], self.onesb[:], sqh[:, 0:n], True, True, R=[self.onesb, sqh], W=[pb])
                    kb.act(rsh[:, 0:n], pb[:, 0:n], AF.Sqrt, R=[pb], W=[rsh], bias=self.epsb[:, 0:1], scale=1.0 / 128.0)
                    kb.recip(rsh[:, 0:n], rsh[:, 0:n], R=[rsh], W=[rsh])
                    kb.dma("sp", gq[:, 0:n], self.ZT[2560 + hd * 128:2560 + (hd + 1) * 128, t0:t0 + n], R=[self.ZT], W=[gq])
                    kb.act(gq[:, 0:n], gq[:, 0:n], AF.Silu, R=[gq], W=[gq])
                    kb.stt(os_[:, 0:n], os_[:, 0:n], hgn[:, 0:1], rsh[:, 0:n], ALU.mult, ALU.mult, R=[os_, SM, rsh], W=[os_])
                    kb.tt(ohb[:, 0:n], os_[:, 0:n], gq[:, 0:n], ALU.mult, R=[os_, gq], W=[ohb])
                    kb.dma("sp", self.MT[512 + hd * 128:512 + (hd + 1) * 128, t0:t0 + n], ohb[:, 0:n], R=[ohb], W=[self.MT])

    def dump_xt(self):
        kb = self.kb
        with kb.phase() as ph:
            for k in range(8):
                if "nodbgm" in self.skip:
                    break
                kb.dma("sp", self.dbgm[k * 128:(k + 1) * 128, :], self.MT[k * 128:(k + 1) * 128, :], R=[self.MT])
            st = [ph.sb("dm%d" % i, [128, 8, 512], F32) for i in range(2)]
            XTv = self.XT.t.rearrange("(k p) t -> p k t", p=128)
            Dv = self.dbg.rearrange("(k p) t -> p k t", p=128)
            for ci, (t0, n) in enumerate(tok_chunks()):
                s = st[ci % 2]
                kb.dma("sp", s[:, :, 0:n], XTv[:, :, t0:t0 + n], R=[self.XT], W=[s])
                kb.dma("sp", Dv[:, :, t0:t0 + n], s[:, :, 0:n], R=[s])

    def final(self):
        kb = self.kb
        with kb.phase() as ph:
            xs = [ph.sb("fx%d" % i, [128, 8, 512], F32) for i in range(2)]
            sq = ph.sb("fsq", [128, 8, 512], BF16)
            rs = ph.sb("frs", [128, 512], F32)
            yf = [ph.sb("fy%d" % i, [128, 8, 512], F32) for i in range(2)]
            yo = [ph.sb("fo%d" % i, [128, D], F32) for i in range(3)]
            XTv = self.XT.t.rearrange("(k p) t -> p k t", p=128)
            oi = 0
            for ci, (t0, n) in enumerate(tok_chunks()[1:]):
                xin = xs[ci % 2]
                y_ = yf[ci % 2]
                kb.dma("sp", xin[:], XTv[:, :, t0:t0 + n], R=[self.XT], W=[xin])
                kb.act(sq[:], xin[:], AF.Square, R=[xin], W=[sq])
                pb = kb.bank()
                for k in range(8):
                    kb.mm(pb[:], self.onesb[:], sq[:, k, :], k == 0, k == 7, R=[sq, self.onesb], W=[pb])
                kb.act(rs[:], pb[:], AF.Sqrt, R=[pb], W=[rs], bias=self.epsb[:, 0:1], scale=1.0 / D)
                kb.recip(rs[:], rs[:], R=[rs], W=[rs])
                for k in range(8):
                    kb.stt(y_[:, k, :], xin[:, k, :], self.fn[:, k:k + 1], rs[:], ALU.mult, ALU.mult, R=[xin, rs, self.fn], W=[y_])
                for tl in range(4):
                    o_ = yo[oi % 3]
                    oi += 1
                    for h in range(2):
                        pb = kb.bank()
                        for q in range(4):
                            k = h * 4 + q
                            kb.op("pe", lambda e, pb=pb, q=q, k=k, y_=y_, tl=tl: e.transpose(pb[:, q * 128:(q + 1) * 128], y_[:, k, tl * 128:(tl + 1) * 128], self.ident[:]),
                                  R=[y_, self.ident], W=[pb])
                        kb.cp(o_[:, h * 512:(h + 1) * 512], pb[:], R=[pb], W=[o_], eng=("act" if h else "dve"))
                    r0 = t0 - TC + tl * 128
                    kb.dma("sp", self.y[r0:r0 + 128, :], o_[:], R=[o_])


def host_consts():
    c = {}
    c["k_ident"] = np.eye(128, dtype=np.float32)
    pm = np.zeros((128, 128), np.float32)
    for m in range(128):
        pm[m ^ 16, m] = 1.0
    c["k_perm"] = pm
    rows = T // 64
    t = np.arange(T)
    row = (t // 64).astype(np.float32)
    col = (t % 64).astype(np.float32)
    inv = (10000.0 ** (-np.arange(16, dtype=np.float32) / 16.0)).astype(np.float32)
    C = np.zeros((128, T), np.float32)
    S = np.zeros((128, T), np.float32)
    for m in range(128):
        i = m % 16
        axis = (m % 64) // 32
        half = (m % 32) // 16
        pos = row if axis == 0 else col
        ang = (pos * inv[i]).astype(np.float32)
        C[m] = np.cos(ang)
        S[m] = np.sin(ang) * (-1.0 if half == 0 else 1.0)
    c["k_ropeC"] = C
    c["k_ropeS"] = S
    kl = np.arange(128)[:, None]
    ql = np.arange(128)[None, :]
    mp = np.where(kl >= ql, 0.0, -30000.0).astype(np.float32)
    mn = np.where(kl <= ql, 0.0, -30000.0).astype(np.float32)
    c["k_mprev"] = np.tile(mp, (1, 4))
    c["k_mnext"] = np.tile(mn, (1, 4))
    sel = np.zeros((8, 8, 128), np.float32)
    for e in range(8):
        sel[e, e, :] = 1.0
    c["k_sel"] = sel.reshape(8, 8 * 128)
    c["k_j1"] = np.tile(np.arange(1, TA // 16 + 1, dtype=np.float32)[None, :], (128, 1))
    m01 = np.ones((128, 1024), np.float32)
    m01[:, ::64] = 0.0
    c["k_m01"] = m01
    s_ = np.arange(128)[:, None]
    t_ = np.arange(128)[None, :]
    c["k_mask128"] = (((s_ // 64) == (t_ // 64)) & (s_ <= t_)).astype(np.float32)
    return c


_CACHE = {}


def run(inputs, depth_run=DEPTH, debug=False, layers=None, build_only=False, skip=(), ncores=8):
    nc = bass.Bass("TRN2", target_bir_lowering=False)
    prog = Prog(nc, depth_run=depth_run, debug=debug, layers=layers, skip=skip)
    prog.build()
    if build_only:
        return prog
    consts = host_consts()
    shared = {k: np.ascontiguousarray(v) for k, v in inputs.items() if k not in ("x", "c", "ctx")}
    shared.update(consts)
    in_maps = []
    for core in range(ncores):
        b = core % 4
        m = dict(shared)
        m["x"] = np.ascontiguousarray(inputs["x"][b])
        m["c"] = np.ascontiguousarray(inputs["c"][b])
        m["ctx"] = np.ascontiguousarray(inputs["ctx"][b])
        in_maps.append(m)
    res = run_bass_kernel_spmd(nc, in_maps, core_ids=list(range(ncores)))
    return res


def kernel(**inputs):
    inputs = {k: np.asarray(v) for k, v in inputs.items()}
    res = run(inputs)
    out = np.stack([np.asarray(res.results[b]["y"]) for b in range(4)], axis=0)
    return out.astype(np.float32)
```

```python
import contextlib
import math
import numpy as np
import concourse.bass as bass
import concourse.mybir as mybir
from concourse.bass_utils import run_bass_kernel_spmd

F32 = mybir.dt.float32
BF16 = mybir.dt.bfloat16
AF = mybir.ActivationFunctionType
ALU = mybir.AluOpType

D = 1024
T = 8192
TC = 256
TA = T + TC
DEPTH = 4
EPS = 1e-6
NDS = 24
SAME_SYNC = True
FFD = 2816
EXD = 1408
NEXP = 8


class Reg:
    __slots__ = ("w", "r")

    def __init__(self):
        self.w = None
        self.r = {}


class TT:
    def __init__(self, t, reg=None):
        self.t = t
        self.g = reg if reg is not None else Reg()

    def __getitem__(self, k):
        return self.t[k]


class KB:
    def __init__(self, nc):
        self.nc = nc
        self.E = {"pe": nc.tensor, "act": nc.scalar, "dve": nc.vector, "pool": nc.gpsimd, "sp": nc.sync}
        self.es = contextlib.ExitStack()
        self.sem = {e: self.es.enter_context(nc.semaphore("s_" + e)) for e in self.E}
        self.cnt = {e: 0 for e in self.E}
        self.known = {e: {} for e in self.E}
        self.dsem = [self.es.enter_context(nc.semaphore("d%d" % i)) for i in range(NDS)]
        self.dcnt = [0] * NDS
        self.dnext = 0
        self.ps = [TT(self.es.enter_context(nc.psum_tensor("ps%d" % i, [128, 512], F32))) for i in range(8)]
        self.psn = 0
        self.uid = 0
        self.ninst = 0

    def _need(self, eng, tok):
        if tok is None:
            return
        kind, key, val = tok
        if kind == "e" and key == eng and (eng == "pe" or not SAME_SYNC):
            return
        kk = (kind, key)
        if self.known[eng].get(kk, 0) >= val:
            return
        sem = self.sem[key] if kind == "e" else self.dsem[key]
        self.E[eng].wait_ge(sem, val)
        self.known[eng][kk] = val

    def _deps(self, eng, R, W):
        for r in R:
            self._need(eng, r.g.w)
        for w in W:
            self._need(eng, w.g.w)
            for t in list(w.g.r.values()):
                self._need(eng, t)

    def _commit(self, tok, R, W):
        for r in R:
            r.g.r[(tok[0], tok[1])] = tok
        for w in W:
            w.g.w = tok
            w.g.r = {}

    def op(self, eng, fn, R=(), W=()):
        self._deps(eng, R, W)
        inst = fn(self.E[eng])
        self.cnt[eng] += 1
        inst.then_inc(self.sem[eng], 1)
        tok = ("e", eng, self.cnt[eng])
        self._commit(tok, R, W)
        self.ninst += 1
        return tok

    def dma(self, q, out, in_, R=(), W=(), **kw):
        i = self.dnext
        self.dnext = (self.dnext + 1) % NDS
        if self.dcnt[i] > 0:
            self._need(q, ("d", i, 16 * self.dcnt[i]))
        self._deps(q, R, W)
        inst = self.E[q].dma_start(out=out, in_=in_, **kw)
        inst.then_inc(self.dsem[i], 16)
        self.dcnt[i] += 1
        tok = ("d", i, 16 * self.dcnt[i])
        self._commit(tok, R, W)
        self.ninst += 1
        return tok

    def barrier(self):
        for e in self.E:
            for o in self.E:
                if o != e and self.cnt[o] > 0:
                    self._need(e, ("e", o, self.cnt[o]))
            for i in range(NDS):
                if self.dcnt[i] > 0:
                    self._need(e, ("d", i, 16 * self.dcnt[i]))

    @contextlib.contextmanager
    def phase(self):
        ph = Phase(self)
        with ph.es:
            yield ph
            self.barrier()

    def bank(self):
        p = self.ps[self.psn]
        self.psn = (self.psn + 1) % 8
        return p

    def dram(self, name, shape, dt):
        return TT(self.nc.dram_tensor(name, shape, dt, kind="Internal").ap())

    def mm(self, out, lhsT, rhs, start, stop, R, W):
        return self.op("pe", lambda e: e.matmul(out, lhsT=lhsT, rhs=rhs, start=start, stop=stop), R=R, W=W)

    def act(self, out, in_, func, R, W, bias=None, scale=None):
        kw = {}
        if bias is not None:
            kw["bias"] = bias
        if scale is not None:
            kw["scale"] = scale
        return self.op("act", lambda e: e.activation(out=out, in_=in_, func=func, **kw), R=R, W=W)

    def ts(self, out, in0, s1, s2, op0, op1, R, W, eng="dve"):
        if op1 is None:
            return self.op(eng, lambda e: e.tensor_scalar(out=out, in0=in0, scalar1=s1, scalar2=None, op0=op0), R=R, W=W)
        return self.op(eng, lambda e: e.tensor_scalar(out=out, in0=in0, scalar1=s1, scalar2=s2, op0=op0, op1=op1), R=R, W=W)

    def stt(self, out, in0, scalar, in1, op0, op1, R, W):
        return self.op("dve", lambda e: e.scalar_tensor_tensor(out=out, in0=in0, scalar=scalar, in1=in1, op0=op0, op1=op1), R=R, W=W)

    def tt(self, out, in0, in1, op, R, W, eng="dve"):
        return self.op(eng, lambda e: e.tensor_tensor(out=out, in0=in0, in1=in1, op=op), R=R, W=W)

    def cp(self, out, in_, R, W, eng="dve"):
        if eng == "act":
            return self.op("act", lambda e: e.copy(out=out, in_=in_), R=R, W=W)
        return self.op(eng, lambda e: e.tensor_copy(out=out, in_=in_), R=R, W=W)

    def recip(self, out, in_, R, W):
        return self.op("dve", lambda e: e.reciprocal(out=out, in_=in_), R=R, W=W)

    def memset(self, t, val, eng="pool"):
        return self.op(eng, lambda e: e.memset(t.t[:], val), W=[t])


class Phase:
    def __init__(self, kb):
        self.kb = kb
        self.es = contextlib.ExitStack()

    def sb(self, name, shape, dt):
        self.kb.uid += 1
        return TT(self.es.enter_context(self.kb.nc.sbuf_tensor("%s_%d" % (name, self.kb.uid), list(shape), dt)))


def tok_chunks():
    return [(0, TC)] + [(TC + 512 * i, 512) for i in range(T // 512)]


class Prog:
    def __init__(self, nc, depth_run=DEPTH, debug=False, layers=None, skip=()):
        self.skip = set(skip)
        self.nc = nc
        self.kb = KB(nc)
        self.depth_run = depth_run
        self.layers = list(range(depth_run)) if layers is None else layers
        self.debug = debug
        self.inp = {}

    def din(self, name, shape, dt=F32):
        a = self.nc.dram_tensor(name, list(shape), dt, kind="ExternalInput").ap()
        self.inp[name] = a
        return a

    def declare(self):
        d = self.din
        d("x", [T, D]); d("c", [D]); d("ctx", [TC, D]); d("c_ctx", [D])
        d("w_mod", [DEPTH, D, 6 * D]); d("b_mod", [DEPTH, 6 * D])
        d("norm_mix", [DEPTH, D]); d("norm_ffn", [DEPTH, D]); d("final_norm", [D])
        d("w_in_ab", [2, D, 1792]); d("lru_conv_w", [2, 4, 512]); d("lru_conv_b", [2, 512])
        d("lru_wa", [2, 2, 8, 64, 64]); d("lru_ba", [2, 2, 512]); d("lru_wx", [2, 2, 8, 64, 64]); d("lru_bx", [2, 2, 512])
        d("lru_lam", [2, 2, 512]); d("attn_sink", [2, 8]); d("w_out_ab", [2, D, D])
        d("ffn_w1", [2, D, FFD]); d("ffn_w3", [2, D, FFD]); d("ffn_w2", [2, FFD, D])
        d("w_in_cd", [2, D, 3072])
        d("s5_a_re", [2, 2, 32, 64]); d("s5_a_im", [2, 2, 32, 64]); d("s5_log_step", [2, 2, 32])
        d("s5_b_re", [2, 2, 32, 64, 16]); d("s5_b_im", [2, 2, 32, 64, 16])
        d("s5_c_re", [2, 2, 32, 16, 64]); d("s5_c_im", [2, 2, 32, 16, 64])
        d("s5_d", [2, 512]); d("s5_w_glu", [2, 512, 512]); d("s5_b_glu", [2, 512])
        d("hg_lb_raw", [2, 512]); d("hg_norm", [2, 128]); d("w_out_cd", [2, D, D])
        d("moe_router", [2, D, 8]); d("moe_w1", [2, 8, D, EXD]); d("moe_w3", [2, 8, D, EXD]); d("moe_w2", [2, 8, EXD, D])
        d("k_ident", [128, 128]); d("k_perm", [128, 128]); d("k_ropeC", [128, T]); d("k_ropeS", [128, T])
        d("k_mprev", [128, 512]); d("k_mnext", [128, 512]); d("k_sel", [8, 8 * 128])
        d("k_j1", [128, TA // 16]); d("k_m01", [128, 1024]); d("k_mask128", [128, 128])
        self.y = self.nc.dram_tensor("y", [T, D], F32, kind="ExternalOutput").ap()
        if self.debug:
            self.dbg = self.nc.dram_tensor("dbg", [D, TA], F32, kind="ExternalOutput").ap()
            self.dbgm = self.nc.dram_tensor("dbgm", [D, TA], BF16, kind="ExternalOutput").ap()
        kb = self.kb
        self.XT = kb.dram("XT", [D, TA], F32)
        self.ZT = kb.dram("ZT", [3072, TA], F32)
        self.MT = kb.dram("MT", [D, TA], BF16)
        self.VT = kb.dram("VT", [TA, 128], BF16)
        self.U = kb.dram("U", [20, 128, 11, 3072], BF16)
        self.YG = kb.dram("YG", [512, TA], BF16)
        self.OD = self.nc.dram_tensor("OD", [2, 512, TA], F32, kind="Internal").ap()
        self.ODr = TT(None)

    def build(self):
        nc = self.nc
        kb = self.kb
        self.declare()
        with contextlib.ExitStack() as gs:
            gs.enter_context(nc.allow_non_contiguous_dma(reason="small strided parameter loads"))
            gs.enter_context(nc.allow_low_precision(reason="bf16 matmul operands, fp32 accumulation"))
            self.G = Phase(kb)
            gs.enter_context(self.G.es)
            self.setup_globals()
            self.phase0_mod()
            self.convert_weights()
            self.ingest()
            for l in self.layers:
                if l % 2 == 0:
                    self.layer_ab(l)
                else:
                    self.layer_cd(l)
            if self.debug:
                self.dump_xt()
            self.final()
            kb.barrier()
        kb.es.close()
        return nc

    def setup_globals(self):
        kb = self.kb
        G = self.G
        I = self.inp
        self.ident = G.sb("ident", [128, 128], F32)
        kb.dma("sp", self.ident[:], I["k_ident"], W=[self.ident])
        self.identb = G.sb("identb", [128, 128], BF16)
        kb.cp(self.identb[:], self.ident[:], R=[self.ident], W=[self.identb])
        self.onesb = G.sb("onesb", [128, 128], BF16)
        kb.memset(self.onesb, 1.0)
        self.MOD = G.sb("MOD", [128, DEPTH, 48, 2], F32)
        self.G1 = G.sb("G1", [128, DEPTH, 8, 2], F32)
        self.G4 = G.sb("G4", [128, DEPTH, 8, 2], F32)
        self.oneb = G.sb("oneb", [128, 1], F32)
        kb.memset(self.oneb, 1.0)
        self.epsb = G.sb("epsb", [128, 1], F32)
        kb.memset(self.epsb, EPS)
        self.fn = G.sb("fn", [128, 8], F32)
        kb.dma("sp", self.fn[:], I["final_norm"].rearrange("(k p) -> p k", p=128), W=[self.fn])

    def phase0_mod(self):
        kb = self.kb
        I = self.inp
        with kb.phase() as ph:
            cs = ph.sb("cs", [128, 8, 2], F32)
            kb.dma("sp", cs[:, :, 0], I["c"].rearrange("(k p) -> p k", p=128), W=[cs])
            kb.dma("sp", cs[:, :, 1], I["c_ctx"].rearrange("(k p) -> p k", p=128), W=[cs])
            sc = ph.sb("sc", [128, 8, 2], F32)
            kb.act(sc[:], cs[:], AF.Silu, R=[cs], W=[sc])
            bm = ph.sb("bm", [128, DEPTH, 48], F32)
            for l_ in range(DEPTH):
                kb.dma("sp", bm[:, l_, :], I["b_mod"][l_].rearrange("(j p) -> p j", p=128), W=[bm])
            nm = ph.sb("nm", [128, DEPTH, 8], F32)
            for l_ in range(DEPTH):
                kb.dma("sp", nm[:, l_, :], I["norm_mix"][l_].rearrange("(k p) -> p k", p=128), W=[nm])
            nf = ph.sb("nf", [128, DEPTH, 8], F32)
            for l_ in range(DEPTH):
                kb.dma("sp", nf[:, l_, :], I["norm_ffn"][l_].rearrange("(k p) -> p k", p=128), W=[nf])
            wm = [ph.sb("wm%d" % i, [128, 8, 1024], F32) for i in range(2)]
            it = 0
            for l in range(DEPTH):
                for m in range(6):
                    w = wm[it % 2]
                    it += 1
                    kb.dma("sp", w[:], I["w_mod"][l].rearrange("(k p) n -> p k n", p=128)[:, :, m * 1024:(m + 1) * 1024], W=[w])
                    for dc in range(8):
                        pb = kb.bank()
                        for k in range(8):
                            kb.mm(pb[:, 0:2], w[:, k, dc * 128:(dc + 1) * 128], sc[:, k, :], k == 0, k == 7, R=[w, sc], W=[pb])
                        j = m * 8 + dc
                        kb.ts(self.MOD[:, l, j, :], pb[:, 0:2], bm[:, l, j:j + 1], None, ALU.add, None, R=[pb, bm], W=[self.MOD])
            for l in range(DEPTH):
                for s in range(2):
                    kb.stt(self.G1[:, l, :, s], self.MOD[:, l, 8:16, s], 1.0, nm[:, l, :], ALU.add, ALU.mult, R=[self.MOD, nm], W=[self.G1])
                    kb.stt(self.G4[:, l, :, s], self.MOD[:, l, 32:40, s], 1.0, nf[:, l, :], ALU.add, ALU.mult, R=[self.MOD, nf], W=[self.G4])

    def convert_weights(self):
        kb = self.kb
        I = self.inp
        need = []
        for l in self.layers:
            j = l // 2
            if l % 2 == 0:
                for pe in range(2):
                    need.append((j * 2 + pe, I["ffn_w1"][j][:, pe * EXD:(pe + 1) * EXD], I["ffn_w3"][j][:, pe * EXD:(pe + 1) * EXD],
                                 I["ffn_w2"][j][pe * EXD:(pe + 1) * EXD, :]))
            else:
                for e in range(NEXP):
                    need.append((4 + j * 8 + e, I["moe_w1"][j, e], I["moe_w3"][j, e], I["moe_w2"][j, e]))
        with kb.phase() as ph:
            sf = [ph.sb("cvf%d" % i, [128, 8, EXD], F32) for i in range(3)]
            sbb = [ph.sb("cvb%d" % i, [128, 11, 1024], BF16) for i in range(2)]
            engs = ["pool", "dve", "act"]
            jobs = []
            for (u, w1, w3, w2) in need:
                jobs.append(("a", u, 0, w1))
                jobs.append(("a", u, 1, w3))
                jobs.append(("b", u, 2, w2))

            def fview(f):
                return f.t[:].rearrange("p k n -> p (k n)")[:, 0:11 * 1024].rearrange("p (f n) -> p f n", n=1024)

            def load(i):
                kind, u, mi, w = jobs[i]
                f = sf[i % 3]
                if kind == "a":
                    kb.dma("sp", f[:], w.rearrange("(k p) n -> p k n", p=128), W=[f])
                else:
                    kb.dma("sp", fview(f), w.rearrange("(f p) n -> p f n", p=128), W=[f])

            def cast_store(i):
                kind, u, mi, w = jobs[i]
                f = sf[i % 3]; b = sbb[i % 2]
                eng = engs[i % 3]
                if kind == "a":
                    for k in range(8):
                        kb.cp(b[:, :, k * 128:(k + 1) * 128], f[:, k, :].rearrange("p (f c) -> p f c", c=128), R=[f], W=[b], eng=eng)
                else:
                    kb.cp(b[:], fview(f), R=[f], W=[b], eng=eng)
                kb.dma("sp", self.U[u, :, :, mi * 1024:(mi + 1) * 1024], b[:], R=[b], W=[self.U])

            if jobs:
                load(0)
            for i in range(len(jobs)):
                if i + 1 < len(jobs):
                    load(i + 1)
                cast_store(i)

    def ingest(self):
        kb = self.kb
        I = self.inp
        with kb.phase() as ph:
            tin = [ph.sb("tin%d" % i, [128, D], F32) for i in range(3)]
            st = [ph.sb("tst%d" % i, [128, 8, 512], F32) for i in range(2)]
            ci = 0
            ti = 0
            for (t0, n) in tok_chunks():
                s = st[ci % 2]
                ci += 1
                for tl in range(n // 128):
                    a = tin[ti % 3]
                    ti += 1
                    tt0 = t0 + tl * 128
                    src = I["ctx"][tt0:tt0 + 128, :] if tt0 < TC else I["x"][tt0 - TC:tt0 - TC + 128, :]
                    kb.dma("sp", a[:], src, W=[a])
                    for h in range(2):
                        pb = kb.bank()
                        for q in range(4):
                            k = h * 4 + q
                            kb.op("pe", lambda e, pb=pb, q=q, k=k, a=a: e.transpose(pb[:, q * 128:(q + 1) * 128], a[:, k * 128:(k + 1) * 128], self.ident[:]),
                                  R=[a, self.ident], W=[pb])
                        kb.cp(s[:, h * 4:(h + 1) * 4, tl * 128:(tl + 1) * 128], pb[:].rearrange("p (q t) -> p q t", t=128), R=[pb], W=[s],
                              eng=("act" if h else "dve"))
                kb.dma("sp", self.XT.t.rearrange("(k p) t -> p k t", p=128)[:, :, t0:t0 + n], s[:, :, 0:n], R=[s], W=[self.XT])

    def normmod(self, ph, xin, n, Gt, M0, sidx, hT, tmp, fp32out=False):
        kb = self.kb
        sq = tmp["sq"]
        xr = getattr(xin, "regs", None) or [xin]
        kb.act(sq[:, :, 0:n], xin[:, :, 0:n], AF.Square, R=xr, W=[sq])
        pb = kb.bank()
        for k in range(8):
            kb.mm(pb[:, 0:n], self.onesb[:], sq[:, k, 0:n], k == 0, k == 7, R=[sq, self.onesb], W=[pb])
        rs = tmp["rs"]
        kb.act(rs[:, 0:n], pb[:, 0:n], AF.Sqrt, R=[pb], W=[rs], bias=self.epsb[:, 0:1], scale=1.0 / D)
        kb.recip(rs[:, 0:n], rs[:, 0:n], R=[rs], W=[rs])
        hf = tmp["hf"]
        for k in range(8):
            kb.stt(hf[:, k, 0:n], xin[:, k, 0:n], Gt(k), rs[:, 0:n], ALU.mult, ALU.mult, R=[xr[k] if len(xr) == 8 else xr[0], rs, self.G1, self.G4], W=[hf])
            if fp32out:
                kb.act(hf[:, k, 0:n], hf[:, k, 0:n], AF.Identity, R=[hf, self.MOD], W=[hf], bias=M0(k))
                kb.cp(hT[:, k, 0:n], hf[:, k, 0:n], R=[hf], W=[hT], eng="pool")
            else:
                kb.act(hT[:, k, 0:n], hf[:, k, 0:n], AF.Identity, R=[hf, self.MOD], W=[hT], bias=M0(k))

    def layer_ab(self, l):
        kb = self.kb
        I = self.inp
        j = l // 2
        need_ctx = l < DEPTH - 1
        with kb.phase() as ph:
            W = ph.sb("Win", [128, 8, 1792], BF16)
            wv = I["w_in_ab"][j].rearrange("(k p) n -> p k n", p=128)
            kb.dma("pool", W[:, :, 0:1024], wv[:, :, 0:1024], W=[W])
            for jj in range(4):
                for hh in range(2):
                    kb.dma("pool", W[:, :, 1024 + jj * 128 + hh * 64:1024 + jj * 128 + hh * 64 + 64],
                           wv[:, :, 1024 + (hh * 4 + jj) * 64:1024 + (hh * 4 + jj) * 64 + 64], W=[W])
            kb.dma("pool", W[:, :, 1536:1792], wv[:, :, 1536:1792], W=[W])
            self.p1_generic(ph, l, W, 13, lambda oc: (oc * 128, oc * 128), vcol=1664)
        self.lru(l)
        self.attn(l)
        self.p3(l, I["w_out_ab"][j], [(j * 2 + pe) for pe in range(2)], moe=None)

    def p1_generic(self, ph, l, W, nch, colrow, vcol=None):
        kb = self.kb
        xs = [ph.sb("xin%d" % i, [128, 8, 512], F32) for i in range(2)]
        hs = [ph.sb("hT%d" % i, [128, 8, 512], BF16) for i in range(2)]
        tmp = {"sq": ph.sb("sq", [128, 8, 512], BF16), "rs": ph.sb("rs", [128, 512], F32), "hf": ph.sb("hf", [128, 8, 512], F32)}
        zs = [ph.sb("zs%d" % i, [128, 512], F32) for i in range(4)]
        vs = [ph.sb("vs%d" % i, [128, 128], BF16) for i in range(2)]
        XTv = self.XT.t.rearrange("(k p) t -> p k t", p=128)
        zi = 0
        vi = 0
        for ci, (t0, n) in enumerate(tok_chunks()):
            s = 1 if t0 < TC else 0
            xin = xs[ci % 2]
            hT = hs[ci % 2]
            kb.dma("sp", xin[:, :, 0:n], XTv[:, :, t0:t0 + n], R=[self.XT], W=[xin])
            self.normmod(ph, xin, n, lambda k: self.G1[:, l, k, s:s + 1], lambda k: self.MOD[:, l, k, s:s + 1], s, hT, tmp)
            for oc in range(nch):
                c0, r0 = colrow(oc)
                pb = kb.bank()
                for k in range(8):
                    kb.mm(pb[:, 0:n], W[:, k, c0:c0 + 128], hT[:, k, 0:n], k == 0, k == 7, R=[W, hT], W=[pb])
                z = zs[zi % 4]
                zi += 1
                kb.cp(z[:, 0:n], pb[:, 0:n], R=[pb], W=[z], eng=("act" if zi % 2 else "dve"))
                kb.dma("sp", self.ZT[r0:r0 + 128, t0:t0 + n], z[:, 0:n], R=[z], W=[self.ZT])
            if vcol is not None:
                for tl in range(n // 128):
                    pb = kb.bank()
                    for k in range(8):
                        kb.mm(pb[:, 0:128], hT[:, k, tl * 128:(tl + 1) * 128], W[:, k, vcol:vcol + 128], k == 0, k == 7, R=[W, hT], W=[pb])
                    v = vs[vi % 2]
                    vi += 1
                    kb.cp(v[:], pb[:, 0:128], R=[pb], W=[v], eng="dve")
                    kb.dma("sp", self.VT[t0 + tl * 128:t0 + (tl + 1) * 128, :], v[:], R=[v], W=[self.VT])

    def lru(self, l):
        kb = self.kb
        I = self.inp
        j = l // 2
        with kb.phase() as ph:
            cw = ph.sb("cw", [128, 4, 4], F32)
            for tp in range(4):
                kb.dma("sp", cw[:, tp, :], I["lru_conv_w"][j, tp].rearrange("(c p) -> p c", p=128), W=[cw])
            cb = ph.sb("cb", [128, 4], F32)
            kb.dma("sp", cb[:], I["lru_conv_b"][j].rearrange("(c p) -> p c", p=128), W=[cb])
            ba = ph.sb("ba", [128, 2, 4], F32)
            for d_ in range(2):
                kb.dma("sp", ba[:, d_, :], I["lru_ba"][j, d_].rearrange("(c p) -> p c", p=128), W=[ba])
            bx = ph.sb("bx", [128, 2, 4], F32)
            for d_ in range(2):
                kb.dma("sp", bx[:, d_, :], I["lru_bx"][j, d_].rearrange("(c p) -> p c", p=128), W=[bx])
            lam = ph.sb("lam", [128, 2, 4], F32)
            for d_ in range(2):
                kb.dma("sp", lam[:, d_, :], I["lru_lam"][j, d_].rearrange("(c p) -> p c", p=128), W=[lam])
            cl = ph.sb("cl", [128, 2, 4], F32)
            kb.act(cl[:], lam[:], AF.Exp, R=[lam], W=[cl], scale=-1.0)
            kb.act(cl[:], cl[:], AF.Ln, R=[cl], W=[cl], bias=self.oneb[:, 0:1])
            kb.ts(cl[:], cl[:], -8.0, None, ALU.mult, None, R=[cl], W=[cl])
            uraw = ph.sb("uraw", [128, TA], F32)
            u = ph.sb("u", [128, TA], F32)
            ub = ph.sb("ub", [128, TA], BF16)
            H = ph.sb("H", [128, TA], F32)
            gg = ph.sb("gg", [128, TA], F32)
            ob = ph.sb("ob", [128, TA], BF16)
            wg = [[ph.sb("wg%d%d" % (d, a), [128, 128], BF16) for a in range(2)] for d in range(2)]
            tmpn = ["r", "i", "a", "m", "b", "hb"]
            tm = {nm: [ph.sb("l%s%d" % (nm, i), [128, 512], F32) for i in range(2)] for nm in tmpn}
            segs = [(0, TC), (TC, T)]
            for c in range(4):
                kb.dma("sp", uraw[:], self.ZT[(4 + c) * 128:(5 + c) * 128, :], R=[self.ZT], W=[uraw])
                kb.dma("sp", gg[:], self.ZT[c * 128:(c + 1) * 128, :], R=[self.ZT], W=[gg])
                for d in range(2):
                    for a, nmw in enumerate(("lru_wa", "lru_wx")):
                        kb.memset(wg[d][a], 0.0)
                        for b2 in range(2):
                            kb.dma("pool", wg[d][a][b2 * 64:(b2 + 1) * 64, b2 * 64:(b2 + 1) * 64], I[nmw][j, d, 2 * c + b2], W=[wg[d][a]])
                for (s0, ln) in segs:
                    kb.ts(u[:, s0:s0 + ln], uraw[:, s0:s0 + ln], cw[:, 2, c:c + 1], cb[:, c:c + 1], ALU.mult, ALU.add, R=[uraw, cw, cb], W=[u])
                    kb.stt(u[:, s0 + 2:s0 + ln], uraw[:, s0:s0 + ln - 2], cw[:, 0, c:c + 1], u[:, s0 + 2:s0 + ln], ALU.mult, ALU.add, R=[uraw, cw, u], W=[u])
                    kb.stt(u[:, s0 + 1:s0 + ln], uraw[:, s0:s0 + ln - 1], cw[:, 1, c:c + 1], u[:, s0 + 1:s0 + ln], ALU.mult, ALU.add, R=[uraw, cw, u], W=[u])
                    kb.stt(u[:, s0:s0 + ln - 1], uraw[:, s0 + 1:s0 + ln], cw[:, 3, c:c + 1], u[:, s0:s0 + ln - 1], ALU.mult, ALU.add, R=[uraw, cw, u], W=[u])
                kb.cp(ub[:], u[:], R=[u], W=[ub], eng="act")
                for d in range(2):
                    chunks = tok_chunks()
                    order = chunks if d == 0 else [chunks[0]] + chunks[:0:-1]
                    carry = None
                    for ci, (t0, n) in enumerate(order):
                        b_ = ci % 2
                        r, i_, a, m, b, hb = (tm[nm][b_] for nm in tmpn)
                        pa = kb.bank()
                        kb.mm(pa[:, 0:n], wg[d][0][:], ub[:, t0:t0 + n], True, True, R=[wg[d][0], ub], W=[pa])
                        px = kb.bank()
                        kb.mm(px[:, 0:n], wg[d][1][:], ub[:, t0:t0 + n], True, True, R=[wg[d][1], ub], W=[px])
                        kb.act(r[:, 0:n], pa[:, 0:n], AF.Sigmoid, R=[pa, ba], W=[r], bias=ba[:, d, c:c + 1])
                        kb.act(i_[:, 0:n], px[:, 0:n], AF.Sigmoid, R=[px, bx], W=[i_], bias=bx[:, d, c:c + 1])
                        kb.act(a[:, 0:n], r[:, 0:n], AF.Exp, R=[r, cl], W=[a], scale=cl[:, d, c:c + 1])
                        kb.act(m[:, 0:n], a[:, 0:n], AF.Square, R=[a], W=[m])
                        kb.act(m[:, 0:n], m[:, 0:n], AF.Sqrt, R=[m], W=[m], bias=self.oneb[:, 0:1], scale=-1.0)
                        kb.tt(b[:, 0:n], i_[:, 0:n], u[:, t0:t0 + n], ALU.mult, R=[i_, u], W=[b])
                        kb.tt(b[:, 0:n], b[:, 0:n], m[:, 0:n], ALU.mult, R=[b, m], W=[b])
                        if d == 0:
                            init = 0.0 if carry is None else H[:, t0 - 1:t0]
                            kb.op("dve", lambda e, n=n, t0=t0, a=a, b=b, init=init: e.tensor_tensor_scan(
                                out=H[:, t0:t0 + n], data0=a[:, 0:n], data1=b[:, 0:n], initial=init, op0=ALU.mult, op1=ALU.add),
                                R=[a, b, H], W=[H])
                            carry = True
                        else:
                            init = 0.0 if carry is None else carry[:, 0:1]
                            rd = [a, b] + ([tm["hb"][1 - b_]] if carry is not None else [])
                            kb.op("dve", lambda e, n=n, a=a, b=b, hb=hb, init=init: e.tensor_tensor_scan(
                                out=hb[:, 0:n][:, ::-1], data0=a[:, 0:n][:, ::-1], data1=b[:, 0:n][:, ::-1], initial=init,
                                op0=ALU.mult, op1=ALU.add), R=rd, W=[hb])
                            kb.tt(H[:, t0:t0 + n], H[:, t0:t0 + n], hb[:, 0:n], ALU.add, R=[H, hb], W=[H])
                            carry = hb
                kb.act(gg[:], gg[:], AF.Gelu_apprx_tanh, R=[gg], W=[gg])
                kb.tt(ob[:], H[:], gg[:], ALU.mult, R=[H, gg], W=[ob])
                kb.dma("sp", self.MT[c * 128:(c + 1) * 128, :], ob[:], R=[ob], W=[self.MT])

    def attn(self, l):
        kb = self.kb
        I = self.inp
        j = l // 2
        need_ctx = l < DEPTH - 1
        NT = TA // 128
        with kb.phase() as ph:
            QT = ph.sb("QT", [128, 4, TA], BF16)
            KT = ph.sb("KT", [128, TA], BF16)
            VK = ph.sb("VK", [128, NT, 128], BF16)
            kb.dma("sp", VK[:], self.VT.t.rearrange("(n p) d -> p n d", p=128), R=[self.VT], W=[VK])
            perm = ph.sb("perm", [128, 128], F32)
            kb.dma("sp", perm[:], I["k_perm"], W=[perm])
            mb = []
            for nm in ("k_mprev", "k_mnext"):
                mf = ph.sb(nm + "f", [128, 512], F32)
                kb.dma("sp", mf[:], I[nm], W=[mf])
                m_ = ph.sb(nm + "b", [128, 512], BF16)
                kb.cp(m_[:], mf[:], R=[mf], W=[m_])
                mb.append(m_)
            sk = ph.sb("sk", [64, 8], F32)
            kb.dma("sp", sk[:], I["attn_sink"][j].partition_broadcast(64), W=[sk])
            kb.act(sk[:], sk[:], AF.Exp, R=[sk], W=[sk])
            sinkE = ph.sb("sinkE", [64, 2, 4, 128], F32)
            for g in range(2):
                for hj in range(4):
                    kb.cp(sinkE[:, g, hj, :], sk[:, g * 4 + hj:g * 4 + hj + 1].to_broadcast([64, 128]), R=[sk], W=[sinkE])
            zq = [ph.sb("zq%d" % i, [128, 512], F32) for i in range(3)]
            rc = [ph.sb("rc%d" % i, [128, 512], F32) for i in range(2)]
            rs_ = [ph.sb("rsn%d" % i, [128, 512], F32) for i in range(2)]
            t1 = [ph.sb("t1%d" % i, [128, 512], F32) for i in range(2)]
            t2 = [ph.sb("t2%d" % i, [128, 512], F32) for i in range(2)]
            zi = 0
            for ci, (t0, n) in enumerate(tok_chunks()):
                if t0 >= TC:
                    C_ = rc[ci % 2]; S_ = rs_[ci % 2]
                    kb.dma("sp", C_[:], I["k_ropeC"][:, t0 - TC:t0 - TC + n], W=[C_])
                    kb.dma("sp", S_[:], I["k_ropeS"][:, t0 - TC:t0 - TC + n], W=[S_])
                for fc in range(5):
                    z = zq[zi % 3]
                    zi += 1
                    kb.dma("sp", z[:, 0:n], self.ZT[(8 + fc) * 128:(9 + fc) * 128, t0:t0 + n], R=[self.ZT], W=[z])
                    dst = QT[:, fc, t0:t0 + n] if fc < 4 else KT[:, t0:t0 + n]
                    dreg = QT if fc < 4 else KT
                    if t0 < TC:
                        kb.cp(dst, z[:, 0:n], R=[z], W=[dreg], eng="act")
                    else:
                        pb = kb.bank()
                        kb.mm(pb[:, 0:n], perm[:], z[:, 0:n], True, True, R=[perm, z], W=[pb])
                        a_ = t1[zi % 2]; b_ = t2[zi % 2]
                        kb.tt(a_[:, 0:n], z[:, 0:n], C_[:, 0:n], ALU.mult, R=[z, C_], W=[a_])
                        kb.tt(b_[:, 0:n], pb[:, 0:n], S_[:, 0:n], ALU.mult, R=[pb, S_], W=[b_])
                        kb.tt(dst, a_[:, 0:n], b_[:, 0:n], ALU.add, R=[a_, b_], W=[dreg])
            PT = [[ph.sb("PT%d_%d" % (i, k), [128, 512], BF16) for k in range(5)] for i in range(2)]
            den = [ph.sb("den%d" % i, [64, 512], F32) for i in range(2)]
            ot = [ph.sb("ot%d" % i, [64, 512], BF16) for i in range(2)]
            blocks = []
            if need_ctx:
                blocks += [(0, [(0, None), (128, None)]), (128, [(0, None), (128, None)])]
            for n_ in range(T // 128):
                t0 = TC + 128 * n_
                kts = []
                if n_ > 0:
                    kts.append((t0 - 128, 0))
                kts.append((t0, None))
                if n_ < T // 128 - 1:
                    kts.append((t0 + 128, 1))
                kts += [(0, None), (128, None)]
                blocks.append((t0, kts))
            bi = 0
            for (t0, kts) in blocks:
                for g in range(2):
                    pr = slice(g * 64, (g + 1) * 64)
                    pts = PT[bi % 2]
                    for ki, (tk, mk) in enumerate(kts):
                        pb = kb.bank()
                        kb.mm(pb[:], KT[pr, tk:tk + 128], QT[pr, :, t0:t0 + 128], True, mk is None, R=[KT, QT], W=[pb])
                        if mk is not None:
                            kb.mm(pb[:], self.identb[:], mb[mk][:], False, True, R=[self.identb, mb[mk]], W=[pb])
                        kb.act(pts[ki][:], pb[:], AF.Exp, R=[pb], W=[pts[ki]], scale=0.125)
                    po = kb.bank()
                    for ki, (tk, mk) in enumerate(kts):
                        kb.mm(po[0:64, :], VK[:, tk // 128, g * 64:(g + 1) * 64], pts[ki][:], ki == 0, ki == len(kts) - 1, R=[VK, pts[ki]], W=[po])
                    pd = kb.bank()
                    for ki, (tk, mk) in enumerate(kts):
                        kb.mm(pd[0:64, :], self.onesb[:, 0:64], pts[ki][:], ki == 0, ki == len(kts) - 1, R=[self.onesb, pts[ki]], W=[pd])
                    dn = den[bi % 2]; o_ = ot[bi % 2]
                    kb.tt(dn[:], pd[0:64, :], sinkE[:, g, :, :].rearrange("p j t -> p (j t)"), ALU.add, R=[pd, sinkE], W=[dn])
                    kb.recip(dn[:], dn[:], R=[dn], W=[dn])
                    kb.tt(o_[:], po[0:64, :], dn[:], ALU.mult, R=[po, dn], W=[o_])
                    kb.dma("sp", self.MT[512 + g * 256:512 + (g + 1) * 256, t0:t0 + 128].rearrange("(j d) t -> d j t", d=64),
                           o_[:].rearrange("p (j t) -> p j t", t=128), R=[o_], W=[self.MT])
                    bi += 1

    def p3(self, l, wout, units, moe):
        kb = self.kb
        I = self.inp
        need_ctx = l < DEPTH - 1
        j = l // 2
        with kb.phase() as ph:
            Wo = ph.sb("Wo", [128, 8, D], BF16)
            kb.dma("pool", Wo[:], wout.rearrange("(k p) n -> p k n", p=128), W=[Wo])
            x1 = ph.sb("x1", [128, 8, 1024], F32)
            x1r = [TT(x1.t, Reg()) for _ in range(8)]
            mt = [ph.sb("mt%d" % i, [128, 8, 512], BF16) for i in range(2)]
            h2 = ph.sb("h2", [128, 8, 1024], BF16)
            actb = ph.sb("actb", [128, 11, 1024], BF16)
            W2e = ph.sb("W2e", [128, 11, 1024], BF16)
            NR = 4
            ring = [ph.sb("ring%d" % i, [128, 2048], BF16) for i in range(NR)]
            tmp = {"sq": ph.sb("sq", [128, 8, 512], BF16), "rs": ph.sb("rs", [128, 512], F32), "hf": ph.sb("hf", [128, 8, 512], F32)}
            sa = [ph.sb("sa%d" % i, [128, 512], F32) for i in range(2)]
            if moe is not None:
                WE = ph.sb("WE", [128, 8, 1024], BF16)
                rt = ph.sb("rt", [128, 8, 8], F32)
                kb.dma("sp", rt[:], I["moe_router"][j].rearrange("(k p) e -> p k e", p=128), W=[rt])
                sel = ph.sb("sel", [8, 8 * 128], F32)
                kb.dma("sp", sel[:], I["k_sel"], W=[sel])
                selb = ph.sb("selb", [8, 8 * 128], BF16)
                kb.cp(selb[:], sel[:], R=[sel], W=[selb])
                lg = ph.sb("lg", [128, 8], F32)
                m8 = ph.sb("m8", [128, 8], F32)
                gt = ph.sb("gt", [128, 4], F32)
                wtk = ph.sb("wtk", [128, 8], F32)
                wtk2 = ph.sb("wtk2", [128, 8], F32)
                wT = ph.sb("wT", [8, 512], BF16)
                rreg = TT(None)
                lg4 = ph.sb("lg4", [128, 4, 8], F32); lg2 = ph.sb("lg2", [128, 4, 8], F32)
                eq1 = ph.sb("eq1", [128, 4, 8], F32); eq2 = ph.sb("eq2", [128, 4, 8], F32)
                m1 = ph.sb("m1", [128, 4], F32); m2 = ph.sb("m2", [128, 4], F32); gd = ph.sb("gd", [128, 4], F32)
                ge = ph.sb("ge", [128, 4], F32); g1 = ph.sb("g1", [128, 4], F32); g2 = ph.sb("g2", [128, 4], F32)
            XTv = self.XT.t.rearrange("(k p) t -> p k t", p=128)
            MTv = self.MT.t.rearrange("(k p) t -> p k t", p=128)
            supers = [(0, TC)] if need_ctx else []
            supers += [(TC + 1024 * i, 1024) for i in range(T // 1024)]
            ri = 0
            mi = 0
            si = 0
            for (T0, SN) in supers:
                s = 1 if T0 < TC else 0
                subs = [(o, min(512, SN - o)) for o in range(0, SN, 512)]
                kb.dma("sp", x1[:, :, 0:SN], XTv[:, :, T0:T0 + SN], R=[self.XT], W=x1r)
                for (o, n) in subs:
                    m_ = mt[mi % 2]
                    mi += 1
                    kb.dma("sp", m_[:, :, 0:n], MTv[:, :, T0 + o:T0 + o + n], R=[self.MT], W=[m_])
                    for oc in range(8):
                        pb = kb.bank()
                        for k in range(8):
                            kb.mm(pb[:, 0:n], Wo[:, k, oc * 128:(oc + 1) * 128], m_[:, k, 0:n], k == 0, k == 7, R=[Wo, m_], W=[pb])
                        kb.stt(x1[:, oc, o:o + n], pb[:, 0:n], self.MOD[:, l, 16 + oc, s:s + 1], x1[:, oc, o:o + n], ALU.mult, ALU.add,
                               R=[pb, self.MOD, x1r[oc]], W=[x1r[oc]])
                    xv = TT(x1.t[:, :, o:o + n], None)
                    xv.regs = x1r
                    hv = TT(h2.t[:, :, o:o + n], h2.g)
                    self.normmod(ph, xv, n, lambda k: self.G4[:, l, k, s:s + 1], lambda k: self.MOD[:, l, 24 + k, s:s + 1], s, hv, tmp, fp32out=(moe is not None))
                    if moe is not None:
                        hf = tmp["hf"]
                        nt = n // 128
                        pbL = kb.bank()
                        for tl in range(nt):
                            for k in range(8):
                                kb.mm(pbL[:, tl * 8:(tl + 1) * 8], hf[:, k, tl * 128:(tl + 1) * 128], rt[:, k, :], k == 0, k == 7, R=[hf, rt], W=[pbL])
                        RG = [rreg]
                        lgv = lg4[:, 0:nt, :]
                        kb.cp(lgv.rearrange("p a b -> p (a b)"), pbL[:, 0:nt * 8], R=[pbL], W=RG)
                        kb.op("dve", lambda e: e.tensor_reduce(out=m1[:, 0:nt], in_=lgv, axis=mybir.AxisListType.X, op=ALU.max), R=RG, W=RG)
                        kb.tt(eq1[:, 0:nt, :], lgv, m1[:, 0:nt].unsqueeze(2).to_broadcast([128, nt, 8]), ALU.is_equal, R=RG, W=RG)
                        kb.stt(lg2[:, 0:nt, :], eq1[:, 0:nt, :], -1e30, lgv, ALU.mult, ALU.add, R=RG, W=RG)
                        kb.op("dve", lambda e: e.tensor_reduce(out=m2[:, 0:nt], in_=lg2[:, 0:nt, :], axis=mybir.AxisListType.X, op=ALU.max), R=RG, W=RG)
                        kb.tt(gd[:, 0:nt], m2[:, 0:nt], m1[:, 0:nt], ALU.subtract, R=RG, W=RG)
                        kb.act(ge[:, 0:nt], gd[:, 0:nt], AF.Exp, R=RG, W=RG)
                        kb.ts(g1[:, 0:nt], ge[:, 0:nt], 1.0, None, ALU.add, None, R=RG, W=RG)
                        kb.recip(g1[:, 0:nt], g1[:, 0:nt], R=RG, W=RG)
                        kb.tt(g2[:, 0:nt], ge[:, 0:nt], g1[:, 0:nt], ALU.mult, R=RG, W=RG)
                        kb.tt(eq2[:, 0:nt, :], lg2[:, 0:nt, :], m2[:, 0:nt].unsqueeze(2).to_broadcast([128, nt, 8]), ALU.is_equal, R=RG, W=RG)
                        kb.tt(eq1[:, 0:nt, :], eq1[:, 0:nt, :], g1[:, 0:nt].unsqueeze(2).to_broadcast([128, nt, 8]), ALU.mult, R=RG, W=RG)
                        kb.tt(eq2[:, 0:nt, :], eq2[:, 0:nt, :], g2[:, 0:nt].unsqueeze(2).to_broadcast([128, nt, 8]), ALU.mult, R=RG, W=RG)
                        kb.tt(eq1[:, 0:nt, :], eq1[:, 0:nt, :], eq2[:, 0:nt, :], ALU.add, R=RG, W=RG)
                        pt_ = kb.bank()
                        for tl in range(nt):
                            kb.op("pe", lambda e, pt_=pt_, tl=tl: e.transpose(pt_[0:8, tl * 128:(tl + 1) * 128], eq1[:, tl, :], self.ident[:]), R=[rreg, self.ident], W=[pt_])
                        kb.cp(wT[:, 0:n], pt_[0:8, 0:n], R=[pt_], W=[wT])
                        for e_ in range(NEXP):
                            pb = kb.bank()
                            kb.mm(pb[:, 0:n], selb[:, e_ * 128:(e_ + 1) * 128], wT[:, 0:n], True, True, R=[selb, wT], W=[pb])
                            kb.cp(WE[:, e_, o:o + n], pb[:, 0:n], R=[pb], W=[WE], eng="act")
                for ui, u in enumerate(units):
                    for fc in range(11):
                        rg = ring[ri % NR]
                        ri += 1
                        kb.dma("sp", rg[:], self.U[u, :, fc, 0:2048], R=[self.U], W=[rg])
                        for (o, n) in subs:
                            pa = kb.bank()
                            for k in range(8):
                                kb.mm(pa[:, 0:n], rg[:, k * 128:(k + 1) * 128], h2[:, k, o:o + n], k == 0, k == 7, R=[rg, h2], W=[pa])
                            pb = kb.bank()
                            for k in range(8):
                                kb.mm(pb[:, 0:n], rg[:, 1024 + k * 128:1024 + (k + 1) * 128], h2[:, k, o:o + n], k == 0, k == 7, R=[rg, h2], W=[pb])
                            s_ = sa[si % 2]
                            si += 1
                            kb.act(s_[:, 0:n], pa[:, 0:n], AF.Silu, R=[pa], W=[s_])
                            kb.tt(actb[:, fc, o:o + n], s_[:, 0:n], pb[:, 0:n], ALU.mult, R=[s_, pb], W=[actb])
                            if moe is not None:
                                kb.tt(actb[:, fc, o:o + n], actb[:, fc, o:o + n], WE[:, ui, o:o + n], ALU.mult, R=[actb, WE], W=[actb], eng="pool")
                    kb.dma("sp", W2e[:], self.U[u, :, :, 2048:3072], R=[self.U], W=[W2e])
                    for oc in range(8):
                        for (o, n) in subs:
                            pb = kb.bank()
                            for fc in range(11):
                                kb.mm(pb[:, 0:n], W2e[:, fc, oc * 128:(oc + 1) * 128], actb[:, fc, o:o + n], fc == 0, fc == 10, R=[W2e, actb], W=[pb])
                            kb.stt(x1[:, oc, o:o + n], pb[:, 0:n], self.MOD[:, l, 40 + oc, s:s + 1], x1[:, oc, o:o + n], ALU.mult, ALU.add,
                                   R=[pb, self.MOD, x1r[oc]], W=[x1r[oc]])
                        if ui == len(units) - 1:
                            kb.dma("sp", self.XT[oc * 128:(oc + 1) * 128, T0:T0 + SN], x1[:, oc, 0:SN], R=[x1r[oc]], W=[self.XT])

    def layer_cd(self, l):
        kb = self.kb
        I = self.inp
        j = l // 2
        with kb.phase() as ph:
            W = ph.sb("Wcd", [128, 8, 3072], BF16)
            wv = I["w_in_cd"][j].rearrange("(k p) n -> p k n", p=128)
            for h in range(2):
                kb.dma("pool", W[:, :, h * 1536:(h + 1) * 1536], wv[:, :, h * 1536:(h + 1) * 1536], W=[W])
            self.p1_generic(ph, l, W, 24, lambda oc: (oc * 128, oc * 128))
        if "s5" not in self.skip:
            self.s5(l)
            self.s5_glu(l)
        if "hg" not in self.skip:
            self.hgrn(l)
        if "moe" not in self.skip:
            self.p3(l, I["w_out_cd"][j], [4 + j * 8 + e for e in range(NEXP)], moe=True)

    def s5(self, l):
        kb = self.kb
        I = self.inp
        j = l // 2
        NJ = TA // 16
        HJ = NJ // 2
        MAGIC = 12582912.0
        TWO_PI = 2.0 * math.pi
        with kb.phase() as ph:
            smr = Reg()
            SM = TT(None, smr)

            def T_(name, shape, dt=F32):
                t = ph.sb(name, shape, dt)
                t.g = smr
                return t

            def tt_(o, a, b, op):
                kb.tt(o, a, b, op, R=[SM], W=[SM])

            def ts_(o, a, s1, s2, op0, op1=None):
                kb.ts(o, a, s1, s2, op0, op1, R=[SM], W=[SM])

            def act_(o, a, f, **kw):
                kb.act(o, a, f, R=[SM], W=[SM], **kw)

            def frac_(o, x, tmp):
                ts_(tmp, x, MAGIC, None, ALU.add)
                ts_(tmp, tmp, -MAGIC, None, ALU.add)
                tt_(o, x, tmp, ALU.subtract)

            PBpad = ph.sb("PBpad", [128, 4, 16, 2, 128], BF16)
            CPpad = ph.sb("CPpad", [128, 4, 17, 2, 128], BF16)
            kb.memset(PBpad, 0.0)
            kb.memset(CPpad, 0.0)
            J1 = ph.sb("J1", [128, NJ], F32)
            kb.dma("sp", J1[:], I["k_j1"], W=[J1])
            Dv = T_("Dv", [128, 4])
            kb.dma("sp", Dv[:], I["s5_d"][j].rearrange("(q p) -> p q", p=128), W=[SM])
            Cnat = ph.sb("Cnat", [16, 32, 64], F32)
            P_ = []
            for d in range(2):
                p = {}
                are = T_("are%d" % d, [128, 16]); aim = T_("aim%d" % d, [128, 16]); ls = T_("ls%d" % d, [128, 16])
                Br = T_("Br%d" % d, [128, 16, 16]); Bi = T_("Bi%d" % d, [128, 16, 16])
                Cr = T_("Cr%d" % d, [128, 16, 16]); Ci = T_("Ci%d" % d, [128, 16, 16])
                for a in range(2):
                    pr = slice(a * 64, (a + 1) * 64)
                    kb.dma("sp", are[pr, :], I["s5_a_re"][j, d].rearrange("(P a) n -> a n P", a=2)[a], W=[SM])
                    kb.dma("sp", aim[pr, :], I["s5_a_im"][j, d].rearrange("(P a) n -> a n P", a=2)[a], W=[SM])
                    kb.dma("sp", ls[pr, :], I["s5_log_step"][j, d].rearrange("(P a) -> a P", a=2)[a].partition_broadcast(64), W=[SM])
                    kb.dma("sp", Br[pr, :, :], I["s5_b_re"][j, d].rearrange("(P a) n c -> a n P c", a=2)[a], W=[SM])
                    kb.dma("sp", Bi[pr, :, :], I["s5_b_im"][j, d].rearrange("(P a) n c -> a n P c", a=2)[a], W=[SM])
                for (nm, dst) in (("s5_c_re", Cr), ("s5_c_im", Ci)):
                    kb.dma("sp", Cnat[:], I[nm][j, d].rearrange("g c n -> c g n"), W=[Cnat])
                    pb = kb.bank()
                    for P in range(16):
                        kb.op("pe", lambda e, pb=pb, P=P: e.transpose(pb[:, P * 16:(P + 1) * 16],
                              Cnat[:, 2 * P:2 * P + 2, :].rearrange("c a n -> c (a n)"), self.ident[0:16, 0:16]),
                              R=[Cnat, self.ident], W=[pb])
                    kb.cp(dst[:].rearrange("p a c -> p (a c)"), pb[:, 0:256], R=[pb], W=[SM])
                lr = T_("lr%d" % d, [128, 16]); dt = T_("dt%d" % d, [128, 16]); tq = T_("tq%d" % d, [128, 16])
                mag = T_("mag%d" % d, [128, 16]); mg16 = T_("mg16%d" % d, [128, 16]); trn = T_("trn%d" % d, [128, 16])
                tmp = T_("tmp%d" % d, [128, 16]); fx = T_("fx%d" % d, [128, 16]); sv = T_("sv%d" % d, [128, 16]); cv = T_("cv%d" % d, [128, 16])
                lbr = T_("lbr%d" % d, [128, 16]); lbi = T_("lbi%d" % d, [128, 16]); zr = T_("zr%d" % d, [128, 16])
                den = T_("den%d" % d, [128, 16]); fr = T_("fr%d" % d, [128, 16]); fi = T_("fi%d" % d, [128, 16]); t16 = T_("t16%d" % d, [128, 16])
                u1 = T_("u1%d" % d, [128, 16]); u2 = T_("u2%d" % d, [128, 16])
                ts_(lr[:], are[:], -1e-4, None, ALU.min)
                act_(dt[:], ls[:], AF.Exp)
                tt_(tq[:], lr[:], dt[:], ALU.mult)
                act_(mag[:], tq[:], AF.Exp)
                act_(mg16[:], tq[:], AF.Exp, scale=16.0)
                tt_(trn[:], aim[:], dt[:], ALU.mult)
                ts_(trn[:], trn[:], 1.0 / TWO_PI, None, ALU.mult)
                frac_(fx[:], trn[:], tmp[:])
                act_(sv[:], fx[:], AF.Sin, scale=TWO_PI)
                ts_(u1[:], trn[:], 0.25, None, ALU.add)
                frac_(fx[:], u1[:], tmp[:])
                act_(cv[:], fx[:], AF.Sin, scale=TWO_PI)
                tt_(lbr[:], mag[:], cv[:], ALU.mult)
                tt_(lbi[:], mag[:], sv[:], ALU.mult)
                ts_(zr[:], lbr[:], -1.0, None, ALU.add)
                tt_(den[:], lr[:], lr[:], ALU.mult)
                tt_(u1[:], aim[:], aim[:], ALU.mult)
                tt_(den[:], den[:], u1[:], ALU.add)
                kb.recip(den[:], den[:], R=[SM], W=[SM])
                tt_(u1[:], zr[:], lr[:], ALU.mult)
                tt_(u2[:], lbi[:], aim[:], ALU.mult)
                tt_(u1[:], u1[:], u2[:], ALU.add)
                tt_(fr[:], u1[:], den[:], ALU.mult)
                tt_(u1[:], lbi[:], lr[:], ALU.mult)
                tt_(u2[:], zr[:], aim[:], ALU.mult)
                tt_(u1[:], u1[:], u2[:], ALU.subtract)
                tt_(fi[:], u1[:], den[:], ALU.mult)
                ts_(u1[:], trn[:], 16.0, None, ALU.mult)
                frac_(t16[:], u1[:], tmp[:])
                bbr = T_("bbr%d" % d, [128, 16, 16]); bbi = T_("bbi%d" % d, [128, 16, 16])
                w1 = T_("w1%d" % d, [128, 16, 16]); w2 = T_("w2%d" % d, [128, 16, 16])
                frb = fr[:].unsqueeze(2).to_broadcast([128, 16, 16])
                fib = fi[:].unsqueeze(2).to_broadcast([128, 16, 16])
                tt_(w1[:], Br[:], frb, ALU.mult)
                tt_(w2[:], Bi[:], fib, ALU.mult)
                tt_(bbr[:], w1[:], w2[:], ALU.subtract)
                tt_(w1[:], Bi[:], frb, ALU.mult)
                tt_(w2[:], Br[:], fib, ALU.mult)
                tt_(bbi[:], w1[:], w2[:], ALU.add)
                pwr = T_("pwr%d" % d, [128, 17, 16]); pwi = T_("pwi%d" % d, [128, 17, 16])
                kb.memset(TT(pwr.t[:, 0, :], smr), 1.0, eng="dve") if False else kb.op("dve", lambda e: e.memset(pwr[:, 0, :], 1.0), W=[SM])
                kb.op("dve", lambda e: e.memset(pwi[:, 0, :], 0.0), W=[SM])
                for tau in range(16):
                    tt_(u1[:], pwr[:, tau, :], lbr[:], ALU.mult)
                    tt_(u2[:], pwi[:, tau, :], lbi[:], ALU.mult)
                    tt_(pwr[:, tau + 1, :], u1[:], u2[:], ALU.subtract)
                    tt_(u1[:], pwr[:, tau, :], lbi[:], ALU.mult)
                    tt_(u2[:], pwi[:, tau, :], lbr[:], ALU.mult)
                    tt_(pwi[:, tau + 1, :], u1[:], u2[:], ALU.add)
                p.update(bbr=bbr, bbi=bbi, Cr=Cr, Ci=Ci, pwr=pwr, pwi=pwi, mg16=mg16, t16=t16)
                P_.append(p)
            Y = ph.sb("Y", [128, TA], F32)
            up = [ph.sb("up%d" % d, [128, 16, NJ], BF16) for d in range(2)]
            Kt = ph.sb("Kt", [128, 16, 128], BF16)
            WGT = ph.sb("WGT", [128, 16, 2, 128], BF16)
            Hr = ph.sb("Hr", [128, 4, NJ], BF16)
            Hi = ph.sb("Hi", [128, 4, NJ], BF16)
            Ec = ph.sb("Ec", [128, NJ], F32)
            Es = ph.sb("Es", [128, NJ], F32)
            tA, tB, tC, tD, tE, tF = [ph.sb("s5t%d" % i, [128, NJ], F32) for i in range(6)]
            x1 = T_("x1t", [128, 17, 16]); x2 = T_("x2t", [128, 17, 16])
            Yv = Y.t[:].rearrange("p (j t) -> p j t", t=16)
            Ycv = Y.t[:, 0:TC].rearrange("p (j t) -> p j t", t=16)
            Yxv = Y.t[:, TC:TA].rearrange("p (j t) -> p j t", t=16)
            for q in range(4):
                kb.dma("sp", Y[:], self.ZT[q * 128:(q + 1) * 128, :], R=[self.ZT], W=[Y])
                upf0 = up[0].t[:].rearrange("p s j -> p (s j)")
                upv0 = up[0].t[:].rearrange("p s j -> p j s")
                upv1 = up[1].t[:].rearrange("p s j -> p j s")
                kb.cp(upv0, Yv, R=[Y], W=[up[0]], eng="act")
                kb.cp(upv1[:, 0:16, :], Ycv[:, ::-1, ::-1], R=[Y], W=[up[1]])
                kb.cp(upv1[:, 16:NJ, :], Yxv[:, ::-1, ::-1], R=[Y], W=[up[1]])
                kb.ts(Y[:], Y[:], Dv[:, q:q + 1], None, ALU.mult, None, R=[Y, SM], W=[Y])
                for d in range(2):
                    p = P_[d]
                    for pi in range(4):
                        P = q * 4 + pi
                        oA = 32 * pi
                        for (src_r, src_i, pad, nt, t0_, negim) in ((p["bbr"], p["bbi"], PBpad, 16, 0, False), (p["Cr"], p["Ci"], CPpad, 17, 0, True)):
                            pr_b = p["pwr"][:, 0:nt, P].unsqueeze(2).to_broadcast([128, nt, 16])
                            pi_b = p["pwi"][:, 0:nt, P].unsqueeze(2).to_broadcast([128, nt, 16])
                            sr_b = src_r[:, P, :].unsqueeze(1).to_broadcast([128, nt, 16])
                            si_b = src_i[:, P, :].unsqueeze(1).to_broadcast([128, nt, 16])
                            tt_(x1[:, 0:nt, :], pr_b, sr_b, ALU.mult)
                            tt_(x2[:, 0:nt, :], pi_b, si_b, ALU.mult)
                            for a in range(2):
                                prt = slice(a * 64, (a + 1) * 64)
                                kb.tt(pad[prt, pi, :, 0, oA + 16 * a:oA + 16 * a + 16], x1[prt, 0:nt, :], x2[prt, 0:nt, :], ALU.subtract, R=[SM], W=[pad])
                            tt_(x1[:, 0:nt, :], pr_b, si_b, ALU.mult)
                            tt_(x2[:, 0:nt, :], pi_b, sr_b, ALU.mult)
                            for a in range(2):
                                prt = slice(a * 64, (a + 1) * 64)
                                if negim:
                                    kb.stt(pad[prt, pi, :, 1, oA + 16 * a:oA + 16 * a + 16], x1[prt, 0:nt, :], -1.0, x2[prt, 0:nt, :], ALU.mult, ALU.subtract, R=[SM], W=[pad])
                                else:
                                    kb.tt(pad[prt, pi, :, 1, oA + 16 * a:oA + 16 * a + 16], x1[prt, 0:nt, :], x2[prt, 0:nt, :], ALU.add, R=[SM], W=[pad])
                    for tau in range(16):
                        pb = kb.bank()
                        for pi in range(4):
                            kb.mm(pb[:, 0:128], PBpad[:, pi, tau, 0, :], CPpad[:, pi, 0, 0, :], pi == 0, False, R=[PBpad, CPpad], W=[pb])
                            kb.mm(pb[:, 0:128], PBpad[:, pi, tau, 1, :], CPpad[:, pi, 0, 1, :], False, pi == 3, R=[PBpad, CPpad], W=[pb])
                        kb.cp(Kt[:, tau, :], pb[:, 0:128], R=[pb], W=[Kt], eng=("act" if tau % 2 else "dve"))
                    for pi in range(4):
                        P = q * 4 + pi
                        for tb in range(4):
                            pb = kb.bank()
                            pbv = pb.t[:].bitcast(BF16)
                            for k8 in range(8):
                                tau = tb * 4 + k8 // 2
                                ri = k8 % 2
                                kb.op("pe", lambda e, pbv=pbv, k8=k8, tau=tau, ri=ri, pi=pi: e.transpose(pbv[:, k8 * 128:(k8 + 1) * 128], PBpad[:, pi, tau, ri, :], self.identb[:]),
                                      R=[PBpad, self.identb], W=[pb])
                            kb.cp(WGT[:, tb * 4:(tb + 1) * 4, :, :].rearrange("p a b c -> p (a b c)"), pbv[:, 0:1024], R=[pb], W=[WGT], eng=("act" if tb % 2 else "dve"))
                        kb.ts(tA[:], J1[:], p["t16"][:, P:P + 1], None, ALU.mult, None, R=[J1, SM], W=[tA])
                        kb.ts(tB[:], tA[:], MAGIC, None, ALU.add, None, R=[tA], W=[tB])
                        kb.ts(tB[:], tB[:], -MAGIC, None, ALU.add, None, R=[tB], W=[tB])
                        kb.tt(tB[:], tA[:], tB[:], ALU.subtract, R=[tA, tB], W=[tB])
                        kb.act(Es[:], tB[:], AF.Sin, R=[tB], W=[Es], scale=TWO_PI)
                        kb.ts(tA[:], tA[:], 0.25, None, ALU.add, None, R=[tA], W=[tA])
                        kb.ts(tB[:], tA[:], MAGIC, None, ALU.add, None, R=[tA], W=[tB])
                        kb.ts(tB[:], tB[:], -MAGIC, None, ALU.add, None, R=[tB], W=[tB])
                        kb.tt(tB[:], tA[:], tB[:], ALU.subtract, R=[tA, tB], W=[tB])
                        kb.act(Ec[:], tB[:], AF.Sin, R=[tB], W=[Ec], scale=TWO_PI)
                        G = [[None, None], [None, None]]
                        for ri in range(2):
                            for hh in range(2):
                                pb = kb.bank()
                                for s in range(16):
                                    kb.mm(pb[:, 0:HJ], WGT[:, 15 - s, ri, :], up[d][:, s, hh * HJ:(hh + 1) * HJ], s == 0, s == 15, R=[WGT, up[d]], W=[pb])
                                G[ri][hh] = pb
                        for hh in range(2):
                            cs = slice(hh * HJ, (hh + 1) * HJ)
                            Gr = G[0][hh]; Gi = G[1][hh]
                            kb.tt(tA[:, cs], Gr[:, 0:HJ], Ec[:, cs], ALU.mult, R=[Gr, Ec], W=[tA])
                            kb.tt(tB[:, cs], Gi[:, 0:HJ], Es[:, cs], ALU.mult, R=[Gi, Es], W=[tB])
                            kb.tt(tC[:, cs], tA[:, cs], tB[:, cs], ALU.add, R=[tA, tB], W=[tC])
                            kb.tt(tA[:, cs], Gi[:, 0:HJ], Ec[:, cs], ALU.mult, R=[Gi, Ec], W=[tA])
                            kb.tt(tB[:, cs], Gr[:, 0:HJ], Es[:, cs], ALU.mult, R=[Gr, Es], W=[tB])
                            kb.tt(tD[:, cs], tA[:, cs], tB[:, cs], ALU.subtract, R=[tA, tB], W=[tD])
                        mgb = p["mg16"][:, P:P + 1].to_broadcast([128, NJ])
                        kb.op("dve", lambda e, mgb=mgb: e.tensor_tensor_scan(out=tE[:], data0=mgb, data1=tC[:], initial=0.0, op0=ALU.mult, op1=ALU.add), R=[tC, SM], W=[tE])
                        kb.op("dve", lambda e, mgb=mgb: e.tensor_tensor_scan(out=tF[:], data0=mgb, data1=tD[:], initial=0.0, op0=ALU.mult, op1=ALU.add), R=[tD, SM], W=[tF])
                        kb.tt(tA[:], tE[:], Ec[:], ALU.mult, R=[tE, Ec], W=[tA])
                        kb.tt(tB[:], tF[:], Es[:], ALU.mult, R=[tF, Es], W=[tB])
                        kb.tt(Hr[:, pi, :], tA[:], tB[:], ALU.subtract, R=[tA, tB], W=[Hr])
                        kb.tt(tA[:], tF[:], Ec[:], ALU.mult, R=[tF, Ec], W=[tA])
                        kb.tt(tB[:], tE[:], Es[:], ALU.mult, R=[tE, Es], W=[tB])
                        kb.tt(Hi[:, pi, :], tA[:], tB[:], ALU.add, R=[tA, tB], W=[Hi])
                    for t in range(16):
                        for hh in range(2):
                            c0 = hh * HJ
                            pb = kb.bank()
                            for s in range(t + 1):
                                kb.mm(pb[:, 0:HJ], Kt[:, t - s, :], up[d][:, s, c0:c0 + HJ], s == 0, False, R=[Kt, up[d]], W=[pb])
                            lo = 1 if hh == 0 else 0
                            for pi in range(4):
                                kb.mm(pb[:, lo:HJ], CPpad[:, pi, t + 1, 0, :], Hr[:, pi, c0 + lo - 1:c0 + HJ - 1], False, False, R=[CPpad, Hr], W=[pb])
                                kb.mm(pb[:, lo:HJ], CPpad[:, pi, t + 1, 1, :], Hi[:, pi, c0 + lo - 1:c0 + HJ - 1], False, pi == 3, R=[CPpad, Hi], W=[pb])
                            if d == 0:
                                kb.tt(Yv[:, c0:c0 + HJ, t], pb[:, 0:HJ], Yv[:, c0:c0 + HJ, t], ALU.add, R=[pb, Y], W=[Y])
                            else:
                                tm = 15 - t
                                if hh == 0:
                                    kb.tt(Ycv[:, :, tm][:, ::-1], pb[:, 0:16], Ycv[:, :, tm][:, ::-1], ALU.add, R=[pb, Y], W=[Y])
                                    kb.tt(Yxv[:, 264:512, tm][:, ::-1], pb[:, 16:HJ], Yxv[:, 264:512, tm][:, ::-1], ALU.add, R=[pb, Y], W=[Y])
                                else:
                                    kb.tt(Yxv[:, 0:264, tm][:, ::-1], pb[:, 0:HJ], Yxv[:, 0:264, tm][:, ::-1], ALU.add, R=[pb, Y], W=[Y])
                kb.act(upf0, Y[:], AF.Gelu_apprx_tanh, R=[Y], W=[up[0]])
                kb.dma("sp", self.YG[q * 128:(q + 1) * 128, :], upf0, R=[up[0]], W=[self.YG])

    def s5_glu(self, l):
        kb = self.kb
        I = self.inp
        j = l // 2
        with kb.phase() as ph:
            Wg = ph.sb("Wg", [128, 4, 512], BF16)
            kb.dma("pool", Wg[:], I["s5_w_glu"][j].rearrange("(k p) n -> p k n", p=128), W=[Wg])
            bg = ph.sb("bg", [128, 4], F32)
            kb.dma("sp", bg[:], I["s5_b_glu"][j].rearrange("(q p) -> p q", p=128), W=[bg])
            yg = [ph.sb("yg%d" % i, [128, 4, 512], BF16) for i in range(2)]
            sg = [ph.sb("sg%d" % i, [128, 512], F32) for i in range(2)]
            ob = [ph.sb("gob%d" % i, [128, 512], BF16) for i in range(2)]
            YGv = self.YG.t.rearrange("(k p) t -> p k t", p=128)
            it = 0
            for ci, (t0, n) in enumerate(tok_chunks()):
                y_ = yg[ci % 2]
                kb.dma("sp", y_[:, :, 0:n], YGv[:, :, t0:t0 + n], R=[self.YG], W=[y_])
                for oc in range(4):
                    pb = kb.bank()
                    for k in range(4):
                        kb.mm(pb[:, 0:n], Wg[:, k, oc * 128:(oc + 1) * 128], y_[:, k, 0:n], k == 0, k == 3, R=[Wg, y_], W=[pb])
                    s_ = sg[it % 2]; o_ = ob[it % 2]
                    it += 1
                    kb.act(s_[:, 0:n], pb[:, 0:n], AF.Sigmoid, R=[pb, bg], W=[s_], bias=bg[:, oc:oc + 1])
                    kb.tt(o_[:, 0:n], y_[:, oc, 0:n], s_[:, 0:n], ALU.mult, R=[y_, s_], W=[o_])
                    kb.dma("sp", self.MT[oc * 128:(oc + 1) * 128, t0:t0 + n], o_[:, 0:n], R=[o_], W=[self.MT])

    def hgrn(self, l):
        kb = self.kb
        I = self.inp
        j = l // 2
        NBX = 1024
        blocks_mem = [(0, TC)] + [(TC + NBX * i, NBX) for i in range(T // NBX)]
        orders = [blocks_mem, [blocks_mem[0]] + blocks_mem[:0:-1]]
        with kb.phase() as ph:
            smr = Reg()
            SM = TT(None, smr)

            def T_(name, shape, dt=F32):
                t = ph.sb(name, shape, dt)
                t.g = smr
                return t
            raw = T_("raw", [128, 2, 4])
            for ly in range(2):
                kb.dma("sp", raw[:, ly, :], I["hg_lb_raw"][ly].rearrange("(h p) -> p h", p=128), W=[SM])
            lb = T_("lb", [128, 4]); oml = T_("oml", [128, 4])
            if j == 0:
                kb.op("dve", lambda e: e.memset(lb[:], 0.0), W=[SM])
                kb.op("dve", lambda e: e.memset(oml[:], 1.0), W=[SM])
            else:
                mx = T_("mx", [128, 4]); e0 = T_("e0", [128, 4]); e1 = T_("e1", [128, 4])
                kb.tt(mx[:], raw[:, 0, :], raw[:, 1, :], ALU.max, R=[SM], W=[SM])
                kb.tt(e0[:], raw[:, 0, :], mx[:], ALU.subtract, R=[SM], W=[SM])
                kb.tt(e1[:], raw[:, 1, :], mx[:], ALU.subtract, R=[SM], W=[SM])
                kb.act(e0[:], e0[:], AF.Exp, R=[SM], W=[SM])
                kb.act(e1[:], e1[:], AF.Exp, R=[SM], W=[SM])
                kb.tt(e0[:], e0[:], e1[:], ALU.add, R=[SM], W=[SM])
                kb.recip(e0[:], e0[:], R=[SM], W=[SM])
                kb.tt(lb[:], e1[:], e0[:], ALU.mult, R=[SM], W=[SM])
                kb.ts(oml[:], lb[:], -1.0, 1.0, ALU.mult, ALU.add, R=[SM], W=[SM])
            hgn = T_("hgn", [128, 1])
            kb.dma("sp", hgn[:], I["hg_norm"][j].rearrange("(p o) -> p o", o=1), W=[SM])
            M01 = ph.sb("M01", [128, NBX], F32)
            kb.dma("sp", M01[:], I["k_m01"], W=[M01])
            mk = ph.sb("mk128", [128, 128], F32)
            kb.dma("sp", mk[:], I["k_mask128"], W=[mk])
            C_ = []
            for d in range(4):
                c = {}
                c["Ost"] = ph.sb("Ost%d" % d, [128, NBX], F32)
                for nm in ("A", "B", "C", "Dd", "Ee", "Q", "V32"):
                    c[nm] = ph.sb("h%s%d" % (nm, d), [128, NBX], F32)
                for nm in ("q1b", "qmb", "kmb"):
                    c[nm] = ph.sb("h%s%d" % (nm, d), [128, NBX], BF16)
                c["khT"] = ph.sb("khT%d" % d, [128, NBX // 128, 128], BF16)
                c["vT"] = ph.sb("vT%d" % d, [128, NBX // 128, 128], BF16)
                c["S32"] = ph.sb("S32%d" % d, [128, 128], F32)
                c["As"] = ph.sb("As%d" % d, [128, 128], F32)
                c["Sb"] = ph.sb("Sb%d" % d, [128, 128], BF16)
                c["eb"] = ph.sb("eb%d" % d, [128, NBX // 64], F32)
                c["AT"] = [ph.sb("AT%d%d" % (d, i), [128, 128], BF16) for i in range(2)]
                C_.append(c)
            os_ = ph.sb("os_", [128, 512], F32); sqh = ph.sb("sqh", [128, 512], BF16); rsh = ph.sb("rsh", [128, 512], F32)
            gq = ph.sb("gq", [128, 512], F32); ohb = ph.sb("ohb", [128, 512], BF16)
            oa = ph.sb("oa", [128, 512], F32); obb = ph.sb("obb", [128, 512], F32)
            hbk = [0]

            def lbank():
                p = kb.ps[4 + hbk[0] % 4]
                hbk[0] += 1
                return p

            def prepass(ci, hd, m0, NB):
                c = C_[ci]
                d = ci % 2
                rv = (lambda ap: ap[:, ::-1]) if d else (lambda ap: ap)
                nch = NB // 64
                A, B, C, Dd, Ee, Q = (c[nm] for nm in ("A", "B", "C", "Dd", "Ee", "Q"))
                zrow = (1024 if d == 0 else 1536) + hd * 128
                kb.dma("sp", A[:, 0:NB], self.ZT[zrow:zrow + 128, m0:m0 + NB], R=[self.ZT], W=[A])
                kb.dma("sp", Q[:, 0:NB], self.ZT[512 + hd * 128:512 + (hd + 1) * 128, m0:m0 + NB], R=[self.ZT], W=[Q])
                kb.dma("sp", Ee[:, 0:NB], self.ZT[2048 + hd * 128:2048 + (hd + 1) * 128, m0:m0 + NB], R=[self.ZT], W=[Ee])
                kb.cp(c["V32"][:, 0:NB], rv(Ee[:, 0:NB]), R=[Ee], W=[c["V32"]])
                if d:
                    kb.cp(Dd[:, 0:NB], rv(A[:, 0:NB]), R=[A], W=[Dd])
                    kb.act(B[:, 0:NB], Dd[:, 0:NB], AF.Sigmoid, R=[Dd], W=[B])
                else:
                    kb.act(B[:, 0:NB], A[:, 0:NB], AF.Sigmoid, R=[A], W=[B])
                kb.ts(B[:, 0:NB], B[:, 0:NB], oml[:, hd:hd + 1], lb[:, hd:hd + 1], ALU.mult, ALU.add, R=[B, SM], W=[B])
                kb.act(C[:, 0:NB], B[:, 0:NB], AF.Ln, R=[B], W=[C])
                kb.ts(B[:, 0:NB], B[:, 0:NB], -1.0, 1.0, ALU.mult, ALU.add, R=[B], W=[B])
                kb.op("dve", lambda e: e.tensor_tensor_scan(out=A[:, 0:NB], data0=M01[:, 0:NB], data1=C[:, 0:NB], initial=0.0, op0=ALU.mult, op1=ALU.add),
                      R=[M01, C], W=[A])
                Av = A.t[:, 0:NB].rearrange("p (n c) -> p n c", c=64)
                Dv3 = Dd.t[:, 0:NB].rearrange("p (n c) -> p n c", c=64)
                kb.act(c["eb"][:, 0:nch], Av[:, :, 63], AF.Exp, R=[A], W=[c["eb"]])
                if d:
                    kb.cp(C[:, 0:NB], rv(Q[:, 0:NB]), R=[Q], W=[C])
                    kb.act(C[:, 0:NB], C[:, 0:NB], AF.Silu, R=[C], W=[C])
                else:
                    kb.act(C[:, 0:NB], Q[:, 0:NB], AF.Silu, R=[Q], W=[C])
                kb.act(Ee[:, 0:NB], A[:, 0:NB], AF.Exp, R=[A], W=[Ee])
                kb.tt(c["q1b"][:, 0:NB], C[:, 0:NB], Ee[:, 0:NB], ALU.mult, R=[C, Ee], W=[c["q1b"]])
                kb.tt(Dv3, Av, Av[:, :, 31:32].to_broadcast([128, nch, 64]), ALU.subtract, R=[A], W=[Dd])
                kb.ts(Dd[:, 0:NB], Dd[:, 0:NB], -80.0, 80.0, ALU.max, ALU.min, R=[Dd], W=[Dd])
                kb.act(Ee[:, 0:NB], Dd[:, 0:NB], AF.Exp, R=[Dd], W=[Ee])
                kb.tt(c["qmb"][:, 0:NB], C[:, 0:NB], Ee[:, 0:NB], ALU.mult, R=[C, Ee], W=[c["qmb"]])
                kb.act(Ee[:, 0:NB], Dd[:, 0:NB], AF.Exp, R=[Dd], W=[Ee], scale=-1.0)
                kb.tt(c["kmb"][:, 0:NB], B[:, 0:NB], Ee[:, 0:NB], ALU.mult, R=[B, Ee], W=[c["kmb"]])
                kb.tt(Dv3, Av[:, :, 63:64].to_broadcast([128, nch, 64]), Av, ALU.subtract, R=[A], W=[Dd])
                kb.act(Ee[:, 0:NB], Dd[:, 0:NB], AF.Exp, R=[Dd], W=[Ee])
                kb.tt(Dd[:, 0:NB], B[:, 0:NB], Ee[:, 0:NB], ALU.mult, R=[B, Ee, Dd], W=[Dd])
                for dc in range(NB // 128):
                    if "hgT" in self.skip:
                        break
                    pb = lbank()
                    pb2 = lbank()
                    kb.op("pe", lambda e, pb=pb, dc=dc: e.transpose(pb[:, 0:128], Dd[:, dc * 128:(dc + 1) * 128], self.ident[:]), R=[Dd, self.ident], W=[pb])
                    kb.op("pe", lambda e, pb2=pb2, dc=dc: e.transpose(pb2[:, 0:128], c["V32"][:, dc * 128:(dc + 1) * 128], self.ident[:]), R=[c["V32"], self.ident], W=[pb2])
                    kb.cp(c["khT"][:, dc, :], pb[:, 0:128], R=[pb], W=[c["khT"]], eng="act")
                    kb.cp(c["vT"][:, dc, :], pb2[:, 0:128], R=[pb2], W=[c["vT"]], eng="dve")

            st = {}

            def stepA(ci, dc):
                c = C_[ci]
                d = ci
                cols = slice(dc * 128, (dc + 1) * 128)
                pA = lbank()
                kb.mm(pA[:, 0:128], c["kmb"][:, cols], c["qmb"][:, cols], True, True, R=[c["kmb"], c["qmb"]], W=[pA])
                AT = c["AT"][dc % 2]
                As = c["As"]
                kb.ts(As[:], pA[:, 0:128], -1e30, 1e30, ALU.max, ALU.min, R=[pA], W=[As])
                kb.tt(AT[:], As[:], mk[:], ALU.mult, R=[As, mk], W=[AT])
                pO = kb.ps[ci]
                kb.mm(pO[:, 0:128], c["vT"][:, dc, :], AT[:], True, False, R=[c["vT"], AT], W=[pO])
                st[ci] = pO
                half(ci, dc, 0, pO)

            def half(ci, dc, hf, pO):
                c = C_[ci]
                ch = 2 * dc + hf
                c64 = slice(dc * 128 + hf * 64, dc * 128 + hf * 64 + 64)
                prt = slice(hf * 64, hf * 64 + 64)
                kb.mm(pO[:, hf * 64:(hf + 1) * 64], c["Sb"][:], c["q1b"][:, c64], False, hf == 1, R=[c["Sb"], c["q1b"]], W=[pO])
                pU = lbank()
                kb.mm(pU[:, 0:128], c["khT"][prt, dc, :], c["vT"][prt, dc, :], True, True, R=[c["khT"], c["vT"]], W=[pU])
                kb.stt(c["Sb"][:], c["S32"][:], c["eb"][:, ch:ch + 1], pU[:, 0:128], ALU.mult, ALU.add, R=[c["S32"], c["eb"], pU], W=[c["Sb"]])
                kb.stt(c["S32"][:], c["S32"][:], c["eb"][:, ch:ch + 1], pU[:, 0:128], ALU.mult, ALU.add, R=[c["S32"], c["eb"], pU], W=[c["S32"]])

            def stepB(ci, dc, NB):
                pO = st[ci]
                c = C_[ci]
                half(ci, dc, 1, pO)
                if ci % 2 == 0:
                    kb.cp(c["Ost"][:, dc * 128:(dc + 1) * 128], pO[:, 0:128], R=[pO], W=[c["Ost"]], eng="act")
                else:
                    a = NB - 128 * (dc + 1)
                    kb.cp(c["Ost"][:, a:a + 128][:, ::-1], pO[:, 0:128], R=[pO], W=[c["Ost"]], eng="dve")

            for hp in range(2):
                for ci in range(4):
                    kb.memset(C_[ci]["S32"], 0.0)
                    kb.memset(C_[ci]["Sb"], 0.0)
                for bi in range(len(blocks_mem)):
                    NB = orders[0][bi][1]
                    for ci in range(4):
                        prepass(ci, 2 * hp + ci // 2, orders[ci % 2][bi][0], NB)
                    for dc in range(NB // 128):
                        for ci in range(4):
                            stepA(ci, dc)
                        for ci in range(4):
                            stepB(ci, dc, NB)
                    for ci in range(4):
                        hd = 2 * hp + ci // 2
                        m0 = orders[ci % 2][bi][0]
                        kb.dma("sp", self.OD[ci % 2, hd * 128:(hd + 1) * 128, m0:m0 + NB], C_[ci]["Ost"][:, 0:NB], R=[C_[ci]["Ost"]], W=[self.ODr])
            kb.barrier()
            for hd in range(4):
                for (t0, n) in tok_chunks():
                    kb.dma("sp", oa[:, 0:n], self.OD[0, hd * 128:(hd + 1) * 128, t0:t0 + n], R=[self.ODr], W=[oa])
                    kb.dma("sp", obb[:, 0:n], self.OD[1, hd * 128:(hd + 1) * 128, t0:t0 + n], R=[self.ODr], W=[obb])
                    kb.tt(os_[:, 0:n], oa[:, 0:n], obb[:, 0:n], ALU.add, R=[oa, obb], W=[os_])
                    kb.act(sqh[:, 0:n], os_[:, 0:n], AF.Square, R=[os_], W=[sqh])
                    pb = kb.bank()
                    kb.mm(pb[:, 0:n], self.onesb[:], sqh[:, 0:n], True, True, R=[self.onesb, sqh], W=[pb])
                    kb.act(rsh[:, 0:n], pb[:, 0:n], AF.Sqrt, R=[pb], W=[rsh], bias=self.epsb[:, 0:1], scale=1.0 / 128.0)
                    kb.recip(rsh[:, 0:n], rsh[:, 0:n], R=[rsh], W=[rsh])
                    kb.dma("sp", gq[:, 0:n], self.ZT[2560 + hd * 128:2560 + (hd + 1) * 128, t0:t0 + n], R=[self.ZT], W=[gq])
                    kb.act(gq[:, 0:n], gq[:, 0:n], AF.Silu, R=[gq], W=[gq])
                    kb.stt(os_[:, 0:n], os_[:, 0:n], hgn[:, 0:1], rsh[:, 0:n], ALU.mult, ALU.mult, R=[os_, SM, rsh], W=[os_])
                    kb.tt(ohb[:, 0:n], os_[:, 0:n], gq[:, 0:n], ALU.mult, R=[os_, gq], W=[ohb])
                    kb.dma("sp", self.MT[512 + hd * 128:512 + (hd + 1) * 128, t0:t0 + n], ohb[:, 0:n], R=[ohb], W=[self.MT])

    def dump_xt(self):
        kb = self.kb
        with kb.phase() as ph:
            for k in range(8):
                if "nodbgm" in self.skip:
                    break
                kb.dma("sp", self.dbgm[k * 128:(k + 1) * 128, :], self.MT[k * 128:(k + 1) * 128, :], R=[self.MT])
            st = [ph.sb("dm%d" % i, [128, 8, 512], F32) for i in range(2)]
            XTv = self.XT.t.rearrange("(k p) t -> p k t", p=128)
            Dv = self.dbg.rearrange("(k p) t -> p k t", p=128)
            for ci, (t0, n) in enumerate(tok_chunks()):
                s = st[ci % 2]
                kb.dma("sp", s[:, :, 0:n], XTv[:, :, t0:t0 + n], R=[self.XT], W=[s])
                kb.dma("sp", Dv[:, :, t0:t0 + n], s[:, :, 0:n], R=[s])

    def final(self):
        kb = self.kb
        with kb.phase() as ph:
            xs = [ph.sb("fx%d" % i, [128, 8, 512], F32) for i in range(2)]
            sq = ph.sb("fsq", [128, 8, 512], BF16)
            rs = ph.sb("frs", [128, 512], F32)
            yf = [ph.sb("fy%d" % i, [128, 8, 512], F32) for i in range(2)]
            yo = [ph.sb("fo%d" % i, [128, D], F32) for i in range(3)]
            XTv = self.XT.t.rearrange("(k p) t -> p k t", p=128)
            oi = 0
            for ci, (t0, n) in enumerate(tok_chunks()[1:]):
                xin = xs[ci % 2]
                y_ = yf[ci % 2]
                kb.dma("sp", xin[:], XTv[:, :, t0:t0 + n], R=[self.XT], W=[xin])
                kb.act(sq[:], xin[:], AF.Square, R=[xin], W=[sq])
                pb = kb.bank()
                for k in range(8):
                    kb.mm(pb[:], self.onesb[:], sq[:, k, :], k == 0, k == 7, R=[sq, self.onesb], W=[pb])
                kb.act(rs[:], pb[:], AF.Sqrt, R=[pb], W=[rs], bias=self.epsb[:, 0:1], scale=1.0 / D)
                kb.recip(rs[:], rs[:], R=[rs], W=[rs])
                for k in range(8):
                    kb.stt(y_[:, k, :], xin[:, k, :], self.fn[:, k:k + 1], rs[:], ALU.mult, ALU.mult, R=[xin, rs, self.fn], W=[y_])
                for tl in range(4):
                    o_ = yo[oi % 3]
                    oi += 1
                    for h in range(2):
                        pb = kb.bank()
                        for q in range(4):
                            k = h * 4 + q
                            kb.op("pe", lambda e, pb=pb, q=q, k=k, y_=y_, tl=tl: e.transpose(pb[:, q * 128:(q + 1) * 128], y_[:, k, tl * 128:(tl + 1) * 128], self.ident[:]),
                                  R=[y_, self.ident], W=[pb])
                        kb.cp(o_[:, h * 512:(h + 1) * 512], pb[:], R=[pb], W=[o_], eng=("act" if h else "dve"))
                    r0 = t0 - TC + tl * 128
                    kb.dma("sp", self.y[r0:r0 + 128, :], o_[:], R=[o_])


def host_consts():
    c = {}
    c["k_ident"] = np.eye(128, dtype=np.float32)
    pm = np.zeros((128, 128), np.float32)
    for m in range(128):
        pm[m ^ 16, m] = 1.0
    c["k_perm"] = pm
    rows = T // 64
    t = np.arange(T)
    row = (t // 64).astype(np.float32)
    col = (t % 64).astype(np.float32)
    inv = (10000.0 ** (-np.arange(16, dtype=np.float32) / 16.0)).astype(np.float32)
    C = np.zeros((128, T), np.float32)
    S = np.zeros((128, T), np.float32)
    for m in range(128):
        i = m % 16
        axis = (m % 64) // 32
        half = (m % 32) // 16
        pos = row if axis == 0 else col
        ang = (pos * inv[i]).astype(np.float32)
        C[m] = np.cos(ang)
        S[m] = np.sin(ang) * (-1.0 if half == 0 else 1.0)
    c["k_ropeC"] = C
    c["k_ropeS"] = S
    kl = np.arange(128)[:, None]
    ql = np.arange(128)[None, :]
    mp = np.where(kl >= ql, 0.0, -30000.0).astype(np.float32)
    mn = np.where(kl <= ql, 0.0, -30000.0).astype(np.float32)
    c["k_mprev"] = np.tile(mp, (1, 4))
    c["k_mnext"] = np.tile(mn, (1, 4))
    sel = np.zeros((8, 8, 128), np.float32)
    for e in range(8):
        sel[e, e, :] = 1.0
    c["k_sel"] = sel.reshape(8, 8 * 128)
    c["k_j1"] = np.tile(np.arange(1, TA // 16 + 1, dtype=np.float32)[None, :], (128, 1))
    m01 = np.ones((128, 1024), np.float32)
    m01[:, ::64] = 0.0
    c["k_m01"] = m01
    s_ = np.arange(128)[:, None]
    t_ = np.arange(128)[None, :]
    c["k_mask128"] = (((s_ // 64) == (t_ // 64)) & (s_ <= t_)).astype(np.float32)
    return c


_CACHE = {}


def run(inputs, depth_run=DEPTH, debug=False, layers=None, build_only=False, skip=(), ncores=8):
    nc = bass.Bass("TRN2", target_bir_lowering=False)
    prog = Prog(nc, depth_run=depth_run, debug=debug, layers=layers, skip=skip)
    prog.build()
    if build_only:
        return prog
    consts = host_consts()
    shared = {k: np.ascontiguousarray(v) for k, v in inputs.items() if k not in ("x", "c", "ctx")}
    shared.update(consts)
    in_maps = []
    for core in range(ncores):
        b = core % 4
        m = dict(shared)
        m["x"] = np.ascontiguousarray(inputs["x"][b])
        m["c"] = np.ascontiguousarray(inputs["c"][b])
        m["ctx"] = np.ascontiguousarray(inputs["ctx"][b])
        in_maps.append(m)
    res = run_bass_kernel_spmd(nc, in_maps, core_ids=list(range(ncores)))
    return res


def kernel(**inputs):
    inputs = {k: np.asarray(v) for k, v in inputs.items()}
    res = run(inputs)
    out = np.stack([np.asarray(res.results[b]["y"]) for b in range(4)], axis=0)
    return out.astype(np.float32)
```
